# Optimizing a Trainium2 kernel written in Bass

```python
import jax
import jax.numpy as jnp
from jax import lax
import numpy as np

D_MODEL = 1024
BATCH = 8
SEQ = 4096
DEPTH = 2

CONV_WIDTH = D_MODEL
CONV_TAPS = 3
RWKV_HEAD_DIM = 64
RWKV_WIDTH = D_MODEL
RWKV_HEADS = RWKV_WIDTH // RWKV_HEAD_DIM
RWKV_DECAY_LORA = max(32, int(round(1.8 * D_MODEL ** 0.5 / 32)) * 32)
RWKV_AAA_LORA = max(32, int(round(1.8 * D_MODEL ** 0.5 / 32)) * 32)
RWKV_MV_LORA = max(32, int(round(1.3 * D_MODEL ** 0.5 / 32)) * 32)
RWKV_GATE_LORA = max(32, int(round(0.6 * D_MODEL ** 0.8 / 32)) * 32)
RWKV_GN_EPS = 1e-5 * RWKV_HEAD_DIM
RWKV_COLS = 3 * RWKV_WIDTH + RWKV_DECAY_LORA + RWKV_AAA_LORA + RWKV_GATE_LORA
DILATED_GROUPS = ((128, 1), (512, 4), (2048, 16))
N_DIL_GROUPS = len(DILATED_GROUPS)
ATTN_HEADS_PER_GROUP = 8
ATTN_HEAD_DIM = 64
ATTN_WIDTH = N_DIL_GROUPS * ATTN_HEADS_PER_GROUP * ATTN_HEAD_DIM
ATTN_OUT_WIDTH = ATTN_HEADS_PER_GROUP * ATTN_HEAD_DIM
ATTN_BLOCK = 128
ROT_DIM = ATTN_HEAD_DIM // 4
ROPE_THETA = 500000.0
N_IN = 3 * D_MODEL + 3 * CONV_WIDTH + RWKV_COLS + 3 * ATTN_WIDTH
N_EXPERTS = 16
N_EXPERT_GROUPS = 4
EXPERTS_PER_GROUP = N_EXPERTS // N_EXPERT_GROUPS
TOP_K = 2
D_EXPERT = D_MODEL // 2
ALPHA = (2 * DEPTH) ** 0.25
BETA = (8 * DEPTH) ** -0.25
LN_EPS = 1e-5
NEG_INF = -1e30
MAX_POS_OFFSET = 1024

kernel_name = "hybrid_conv_rwkv7_dilated_attn_moe_deepnorm"


def split_cols(z, sizes):
    return jnp.split(z, np.cumsum(sizes)[:-1].tolist(), axis=-1)


def layer_norm(x, g, b):
    xf = x.astype(jnp.float32)
    mu = jnp.mean(xf, axis=-1, keepdims=True)
    var = jnp.mean(jnp.square(xf - mu), axis=-1, keepdims=True)
    return ((xf - mu) * lax.rsqrt(var + LN_EPS) * g + b).astype(x.dtype)


def causal_depthwise_conv(y, w):
    return lax.conv_general_dilated(
        y, w[:, None, :].astype(y.dtype), window_strides=(1,), padding=[(w.shape[0] - 1, 0)],
        dimension_numbers=('NWC', 'WIO', 'NWC'), feature_group_count=y.shape[-1])


def token_shift_mix(z, mu):
    prev = jnp.pad(z[:, :-1], ((0, 0), (1, 0), (0, 0)))
    return z + (prev - z) * mu


def rotary_tables(positions):
    inv_freq = ROPE_THETA ** (-jnp.arange(0, ROT_DIM, 2, dtype=jnp.float32) / ROT_DIM)
    ang = positions.astype(jnp.float32)[..., None] * inv_freq
    return jnp.cos(ang)[:, :, None, None, :], jnp.sin(ang)[:, :, None, None, :]


def apply_partial_rotary(t, cos, sin):
    half = ROT_DIM // 2
    t1 = t[..., :half].astype(jnp.float32)
    t2 = t[..., half:ROT_DIM].astype(jnp.float32)
    rot = jnp.concatenate([t1 * cos - t2 * sin, t2 * cos + t1 * sin], axis=-1).astype(t.dtype)
    return jnp.concatenate([rot, t[..., ROT_DIM:]], axis=-1)


def dilated_window_attention(q, k, v, window, dilation):
    b, s, h, dh = q.shape
    span = window // dilation
    assert span <= ATTN_BLOCK
    L = s // dilation
    nb = -(-L // ATTN_BLOCK)
    Lp = nb * ATTN_BLOCK
    Q = ATTN_BLOCK

    def to_sub(t):
        return t.reshape(b, L, dilation, h, dh).transpose(0, 2, 1, 3, 4)

    qs, ks, vs = to_sub(q), to_sub(k), to_sub(v)
    qb = jnp.pad(qs, ((0, 0), (0, 0), (0, Lp - L), (0, 0), (0, 0))).reshape(b, dilation, nb, Q, h, dh)

    def key_blocks(t):
        tp = jnp.pad(t, ((0, 0), (0, 0), (Q, Lp - L), (0, 0), (0, 0)))
        prev = tp[:, :, :Lp].reshape(b, dilation, nb, Q, h, dh)
        cur = tp[:, :, Q:].reshape(b, dilation, nb, Q, h, dh)
        return jnp.concatenate([prev, cur], axis=3)

    kb, vb = key_blocks(ks), key_blocks(vs)
    scores = jnp.einsum('brnqhc,brnkhc->brnhqk', qb, kb).astype(jnp.float32) * (ATTN_HEAD_DIM ** -0.5)
    qi = jnp.arange(Q)[:, None]
    kj = jnp.arange(2 * Q)[None, :]
    dist = Q + qi - kj
    key_idx = (jnp.arange(nb)[:, None, None] - 1) * Q + kj[None]
    valid = (dist >= 0) & (dist <= span) & (key_idx >= 0)
    scores = jnp.where(valid[:, None], scores, NEG_INF)
    m = jnp.max(scores, axis=-1, keepdims=True)
    p = jnp.exp(scores - m)
    den = jnp.sum(p, axis=-1, keepdims=True)
    out = jnp.einsum('brnhqk,brnkhc->brnqhc', (p / den).astype(v.dtype), vb)
    lse = (m + jnp.log(den))[..., 0]
    out = out.reshape(b, dilation, Lp, h, dh)[:, :, :L].transpose(0, 2, 1, 3, 4).reshape(b, s, h, dh)
    lse = lse.transpose(0, 1, 2, 4, 3).reshape(b, dilation, Lp, h)[:, :, :L]
    lse = lse.transpose(0, 2, 1, 3).reshape(b, s, h)
    return out, lse


def dilated_attention_branch(z, cos, sin):
    b, s, _ = z.shape
    q, k, v = [t.reshape(b, s, N_DIL_GROUPS, ATTN_HEADS_PER_GROUP, ATTN_HEAD_DIM)
               for t in jnp.split(z, 3, axis=-1)]
    q = apply_partial_rotary(q, cos, sin)
    k = apply_partial_rotary(k, cos, sin)
    outs, lses = [], []
    for g, (window, dilation) in enumerate(DILATED_GROUPS):
        o, l = dilated_window_attention(q[:, :, g], k[:, :, g], v[:, :, g], window, dilation)
        outs.append(o)
        lses.append(l)
    wts = jax.nn.softmax(jnp.stack(lses, axis=0), axis=0)
    y = jnp.sum(wts[..., None] * jnp.stack(outs, axis=0).astype(jnp.float32), axis=0)
    return y.reshape(b, s, ATTN_OUT_WIDTH).astype(z.dtype)


def wkv7_scan(r, w, k, v, a, b):
    bsz, _, h, n = r.shape

    def step(state, inp):
        rt, wt, kt, vt, at, bt = inp
        sa = jnp.einsum('bhvk,bhk->bhv', state, at)
        state = state * wt[:, :, None, :] + sa[..., None] * bt[:, :, None, :] + vt[..., None] * kt[:, :, None, :]
        return state, jnp.einsum('bhvk,bhk->bhv', state, rt)

    seq_first = tuple(jnp.moveaxis(t.astype(jnp.float32), 1, 0) for t in (r, w, k, v, a, b))
    _, y = lax.scan(step, jnp.zeros((bsz, h, n, n), jnp.float32), seq_first)
    return jnp.moveaxis(y, 0, 1)


def rwkv7_time_mix(u, z, v_first, params, vres):
    mu, w0, w2, a0, a2, g2, k_k, k_a, r_k, lnx_g, lnx_b = params
    b, s, _ = u.shape
    z = token_shift_mix(z, mu)
    r, k, v, xw, xa, xg = split_cols(z, (RWKV_WIDTH, RWKV_WIDTH, RWKV_WIDTH,
                                          RWKV_DECAY_LORA, RWKV_AAA_LORA, RWKV_GATE_LORA))
    log_w = -jax.nn.softplus(-(w0 + jnp.tanh(xw) @ w2)) - 0.5
    decay = jnp.exp(-jnp.exp(log_w.astype(jnp.float32)))
    if vres is None:
        v_first = v
    else:
        v0, v1, v2 = vres
        v = v + (v_first - v) * jax.nn.sigmoid(v0 + (u @ v1) @ v2)
    a = jax.nn.sigmoid(a0 + xa @ a2)
    g = jax.nn.sigmoid(xg) @ g2

    def heads(t):
        return t.reshape(b, s, RWKV_HEADS, RWKV_HEAD_DIM)

    kk = heads(k * k_k).astype(jnp.float32)
    kk = kk * lax.rsqrt(jnp.maximum(jnp.sum(kk * kk, axis=-1, keepdims=True), 1e-24))
    k = k * (1 + (a - 1) * k_a)
    rh, kh, vh, ah = heads(r), heads(k), heads(v), heads(a)
    y = wkv7_scan(rh, heads(decay), kh, vh, -kk, kk * ah)
    ym = jnp.mean(y, axis=-1, keepdims=True)
    yv = jnp.mean(jnp.square(y - ym), axis=-1, keepdims=True)
    y = ((y - ym) * lax.rsqrt(yv + RWKV_GN_EPS)).reshape(b, s, RWKV_WIDTH) * lnx_g + lnx_b
    bonus = jnp.sum(rh * kh * r_k, axis=-1, keepdims=True) * vh
    y = y + bonus.reshape(b, s, RWKV_WIDTH)
    return (y * g).astype(u.dtype), v_first


def hybrid_mixer(u, cos, sin, v_first, w_in, conv_w, rwkv_params, vres, p_a, p_b, p_c, w_o):
    z = jnp.einsum('bsd,dn->bsn', u, w_in)
    z_gate, z_conv, z_rwkv, z_attn = split_cols(z, (3 * D_MODEL, 3 * CONV_WIDTH, RWKV_COLS, 3 * ATTN_WIDTH))
    gate_a, gate_b, gate_c = jnp.split(jax.nn.sigmoid(z_gate), 3, axis=-1)
    conv_b, conv_c, conv_h = jnp.split(z_conv, 3, axis=-1)
    y_a = (conv_b * causal_depthwise_conv(conv_c * conv_h, conv_w)) @ p_a
    y_b, v_first = rwkv7_time_mix(u, z_rwkv, v_first, rwkv_params, vres)
    y_b = y_b @ p_b
    y_c = dilated_attention_branch(z_attn, cos, sin) @ p_c
    return (gate_a * y_a + gate_b * y_b + gate_c * y_c) @ w_o, v_first


def moe_ffn(u, w_router, router_bias, w_gate, w_up, w_down):
    logits = jnp.einsum('bsd,de->bse', u, w_router).astype(jnp.float32)
    probs = jax.nn.softmax(logits, axis=-1)
    sel = probs + router_bias.astype(jnp.float32)
    grouped = sel.reshape(sel.shape[:-1] + (N_EXPERT_GROUPS, EXPERTS_PER_GROUP))
    group_score = jnp.sum(lax.top_k(grouped, TOP_K)[0], axis=-1)
    best_group = jnp.argmax(group_score, axis=-1)
    in_group = (jnp.arange(N_EXPERTS) // EXPERTS_PER_GROUP) == best_group[..., None]
    _, idx = lax.top_k(jnp.where(in_group, sel, -jnp.inf), TOP_K)
    gate_w = jnp.take_along_axis(probs, idx, axis=-1)
    gate_w = gate_w / jnp.sum(gate_w, axis=-1, keepdims=True)
    combine = jnp.sum(jax.nn.one_hot(idx, N_EXPERTS, dtype=jnp.float32) * gate_w[..., None], axis=-2)
    combine = combine.astype(u.dtype)
    out = jnp.zeros_like(u)
    for e in range(N_EXPERTS):
        hid = jax.nn.silu(u @ w_gate[e]) * (u @ w_up[e])
        out = out + combine[..., e:e + 1] * (hid @ w_down[e])
    return out


def setup_inputs(seed: int = 0) -> dict:
    key = jax.random.key(seed)
    keys = jax.random.split(key, 48)
    counter = iter(range(48))

    def nk():
        return keys[next(counter)]

    def nrm(shape, scale):
        return jax.random.normal(nk(), shape, jnp.float32) * scale

    L = DEPTH
    x = nrm((BATCH, SEQ, D_MODEL), 1.0)
    offset = jax.random.randint(nk(), (BATCH, 1), 0, MAX_POS_OFFSET, dtype=jnp.int32)
    positions = offset + jnp.arange(SEQ, dtype=jnp.int32)[None, :]
    return {
        'x': x,
        'positions': positions,
        'ln_in_g': 1.0 + nrm((D_MODEL,), 0.02),
        'ln_in_b': nrm((D_MODEL,), 0.02),
        'w_in': nrm((L, D_MODEL, N_IN), D_MODEL ** -0.5),
        'conv_w': nrm((L, CONV_TAPS, CONV_WIDTH), CONV_TAPS ** -0.5),
        'rwkv_mu': jax.random.uniform(nk(), (L, RWKV_COLS), jnp.float32, 0.0, 1.0),
        'rwkv_w0': jax.random.uniform(nk(), (L, RWKV_WIDTH), jnp.float32, -6.0, -1.0),
        'rwkv_w2': nrm((L, RWKV_DECAY_LORA, RWKV_WIDTH), 0.1 * RWKV_DECAY_LORA ** -0.5),
        'rwkv_a0': nrm((L, RWKV_WIDTH), 0.1),
        'rwkv_a2': nrm((L, RWKV_AAA_LORA, RWKV_WIDTH), RWKV_AAA_LORA ** -0.5),
        'rwkv_g2': nrm((L, RWKV_GATE_LORA, RWKV_WIDTH), RWKV_GATE_LORA ** -0.5),
        'rwkv_v0': 1.0 + nrm((L - 1, RWKV_WIDTH), 0.1),
        'rwkv_v1': nrm((L - 1, D_MODEL, RWKV_MV_LORA), D_MODEL ** -0.5),
        'rwkv_v2': nrm((L - 1, RWKV_MV_LORA, RWKV_WIDTH), RWKV_MV_LORA ** -0.5),
        'rwkv_k_k': 0.85 + nrm((L, RWKV_WIDTH), 0.05),
        'rwkv_k_a': 1.0 + nrm((L, RWKV_WIDTH), 0.05),
        'rwkv_r_k': nrm((L, RWKV_HEADS, RWKV_HEAD_DIM), 0.1),
        'rwkv_lnx_g': 1.0 + nrm((L, RWKV_WIDTH), 0.02),
        'rwkv_lnx_b': nrm((L, RWKV_WIDTH), 0.02),
        'p_a': nrm((L, CONV_WIDTH, D_MODEL), BETA * CONV_WIDTH ** -0.5),
        'p_b': nrm((L, RWKV_WIDTH, D_MODEL), BETA * RWKV_WIDTH ** -0.5),
        'p_c': nrm((L, ATTN_OUT_WIDTH, D_MODEL), BETA * ATTN_OUT_WIDTH ** -0.5),
        'w_o': nrm((L, D_MODEL, D_MODEL), BETA * D_MODEL ** -0.5),
        'ln1_g': 1.0 + nrm((L, D_MODEL), 0.02),
        'ln1_b': nrm((L, D_MODEL), 0.02),
        'w_router': nrm((D_MODEL, N_EXPERTS), D_MODEL ** -0.5),
        'router_bias': nrm((N_EXPERTS,), 0.01),
        'w_gate': nrm((L, N_EXPERTS, D_MODEL, D_EXPERT), D_MODEL ** -0.5),
        'w_up': nrm((L, N_EXPERTS, D_MODEL, D_EXPERT), D_MODEL ** -0.5),
        'w_down': nrm((L, N_EXPERTS, D_EXPERT, D_MODEL), BETA * D_EXPERT ** -0.5),
        'ln2_g': 1.0 + nrm((L, D_MODEL), 0.02),
        'ln2_b': nrm((L, D_MODEL), 0.02),
    }


def reference(x, positions, ln_in_g, ln_in_b, w_in, conv_w, rwkv_mu, rwkv_w0, rwkv_w2, rwkv_a0,
              rwkv_a2, rwkv_g2, rwkv_v0, rwkv_v1, rwkv_v2, rwkv_k_k, rwkv_k_a, rwkv_r_k, rwkv_lnx_g,
              rwkv_lnx_b, p_a, p_b, p_c, w_o, ln1_g, ln1_b, w_router, router_bias, w_gate, w_up,
              w_down, ln2_g, ln2_b):
    h = layer_norm(x, ln_in_g, ln_in_b)
    cos, sin = rotary_tables(positions)
    v_first = None
    for l in range(DEPTH):
        rwkv_params = (rwkv_mu[l], rwkv_w0[l], rwkv_w2[l], rwkv_a0[l], rwkv_a2[l], rwkv_g2[l],
                       rwkv_k_k[l], rwkv_k_a[l], rwkv_r_k[l], rwkv_lnx_g[l], rwkv_lnx_b[l])
        vres = None if l == 0 else (rwkv_v0[l - 1], rwkv_v1[l - 1], rwkv_v2[l - 1])
        mix, v_first = hybrid_mixer(h, cos, sin, v_first, w_in[l], conv_w[l], rwkv_params, vres,
                                    p_a[l], p_b[l], p_c[l], w_o[l])
        h = layer_norm(ALPHA * h + mix, ln1_g[l], ln1_b[l])
        ffn = moe_ffn(h, w_router, router_bias, w_gate[l], w_up[l], w_down[l])
        h = layer_norm(ALPHA * h + ffn, ln2_g[l], ln2_b[l])
    return h
```

```python
import ml_dtypes
import numpy as np
from contextlib import ExitStack
import concourse.bass as bass
import concourse.mybir as mybir
from concourse.bass_utils import run_bass_kernel_spmd

F32 = mybir.dt.float32
BF16 = mybir.dt.bfloat16
I32 = mybir.dt.int32
AF = mybir.ActivationFunctionType
ALU = mybir.AluOpType
AX = mybir.AxisListType


class Sem:
    def __init__(self, h, is_dma):
        self.h = h
        self.is_dma = is_dma
        self.total = 0


class View:
    def __init__(self, buf, ap):
        self.buf = buf
        self.ap = ap


class Buf:
    def __init__(self, kb, t, dram=False):
        self.kb = kb
        self.t = t
        self.dram = dram
        self.w = {}
        self.r = {}
        self.ds = {}

    def __getitem__(self, idx):
        return View(self, self.t[idx])

    def v(self, ap):
        return View(self, ap)

    def dsem(self, q):
        if q not in self.ds:
            self.ds[q] = self.kb.new_dma_sem(q)
        return self.ds[q]


class KB:
    def __init__(self, nc):
        self.nc = nc
        self.engs = {'pe': nc.tensor, 'act': nc.scalar, 'dve': nc.vector, 'pool': nc.gpsimd, 'sp': nc.sync}
        self.esem = {}
        for k in ['pe', 'act', 'dve', 'pool']:
            self.esem[k] = Sem(nc.alloc_semaphore('es_' + k), False)
        self.seen = {k: {} for k in self.engs}
        self.allsems = list(self.esem.values())
        self.nds = 0
        self.ninst = 0
        self.free_ds = {'sp': [], 'pool': [], 'act': []}

    def new_dma_sem(self, q):
        if self.free_ds[q]:
            return self.free_ds[q].pop()
        s = Sem(self.nc.alloc_semaphore('ds%d' % self.nds), True)
        self.nds += 1
        self.allsems.append(s)
        return s

    def _wait(self, eng, need):
        E = self.engs[eng]
        seen = self.seen[eng]
        for s, c in need.items():
            if s.is_dma:
                c = s.total
            elif eng == 'pe' and s is self.esem['pe']:
                continue
            if seen.get(s, 0) < c:
                E.wait_ge(s.h, c)
                seen[s] = c
                self.ninst += 1

    def _deps(self, reads, writes):
        need = {}

        def add(d):
            for s, c in d.items():
                if need.get(s, 0) < c:
                    need[s] = c
        for b in reads:
            add(b.w)
        for b in writes:
            add(b.w)
            add(b.r)
        return need

    def _stamp(self, reads, writes, s, c):
        for b in reads:
            if b.r.get(s, 0) < c:
                b.r[s] = c
        for b in writes:
            if b.dram:
                if b.w.get(s, 0) < c:
                    b.w[s] = c
            else:
                b.w = {s: c}
                b.r = {}

    def op(self, eng, fn, outs, ins):
        reads = [v.buf for v in ins]
        writes = [v.buf for v in outs]
        self._wait(eng, self._deps(reads, writes))
        ins_ = fn(self.engs[eng], *[v.ap for v in outs], *[v.ap for v in ins])
        s = self.esem[eng]
        s.total += 1
        ins_.then_inc(s.h, 1)
        self.ninst += 1
        self._stamp(reads, writes, s, s.total)
        return ins_

    def dma(self, q, out, in_, **kw):
        reads = [in_.buf]
        writes = [out.buf]
        self._wait(q, self._deps(reads, writes))
        sb = out.buf if not out.buf.dram else in_.buf
        s = sb.dsem(q)
        ins_ = self.engs[q].dma_start(out=out.ap, in_=in_.ap, **kw)
        s.total += 16
        ins_.then_inc(s.h, 16)
        self.ninst += 1
        self._stamp(reads, writes, s, s.total)
        return ins_

    def barrier(self):
        need = {s: s.total for s in self.allsems if s.total > 0}
        for eng in self.engs:
            self._wait(eng, need)

    def mm(self, out, lhsT, rhs, start=True, stop=True):
        return self.op('pe', lambda E, o, a, b: E.matmul(o, lhsT=a, rhs=b, start=start, stop=stop), [out], [lhsT, rhs])

    def transpose(self, out, in_, ident):
        return self.op('pe', lambda E, o, a, b: E.transpose(o, a, b), [out], [in_, ident])

    def act(self, out, in_, func, bias=None, scale=None, accum=None, eng='act'):
        outs = [out] + ([accum] if accum is not None else [])
        ins = [in_]
        kw = {}
        if isinstance(bias, View):
            ins.append(bias)
        if isinstance(scale, View):
            ins.append(scale)

        def fn(E, *aps):
            aps = list(aps)
            o = aps.pop(0)
            if accum is not None:
                kw['accum_out'] = aps.pop(0)
            i = aps.pop(0)
            if isinstance(bias, View):
                kw['bias'] = aps.pop(0)
            elif bias is not None:
                kw['bias'] = bias
            if isinstance(scale, View):
                kw['scale'] = aps.pop(0)
            elif scale is not None:
                kw['scale'] = scale
            return E.activation(out=o, in_=i, func=func, **kw)
        return self.op(eng, fn, outs, ins)

    def tt(self, out, a, b, op, eng='dve'):
        return self.op(eng, lambda E, o, x, y: E.tensor_tensor(out=o, in0=x, in1=y, op=op), [out], [a, b])

    def ts(self, out, a, s1, op0, s2=None, op1=None, eng='dve', accum=None):
        ins = [a]
        outs = [out] + ([accum] if accum is not None else [])
        if isinstance(s1, View):
            ins.append(s1)
        if isinstance(s2, View):
            ins.append(s2)

        def fn(E, *aps):
            aps = list(aps)
            o = aps.pop(0)
            kw = {}
            if accum is not None:
                kw['accum_out'] = aps.pop(0)
            x = aps.pop(0)
            a1 = aps.pop(0) if isinstance(s1, View) else s1
            a2 = aps.pop(0) if isinstance(s2, View) else s2
            if op1 is None:
                return E.tensor_scalar(out=o, in0=x, scalar1=a1, scalar2=None, op0=op0, **kw)
            return E.tensor_scalar(out=o, in0=x, scalar1=a1, scalar2=a2, op0=op0, op1=op1, **kw)
        return self.op(eng, fn, outs, ins)

    def stt(self, out, a, s, b, op0, op1, eng='dve'):
        ins = [a, b]
        if isinstance(s, View):
            ins.append(s)

        def fn(E, o, x, y, *rest):
            sc = rest[0] if rest else s
            return E.scalar_tensor_tensor(out=o, in0=x, scalar=sc, in1=y, op0=op0, op1=op1)
        return self.op(eng, fn, [out], ins)

    def copy(self, out, in_, eng='dve'):
        if eng == 'act':
            return self.op('act', lambda E, o, i: E.copy(out=o, in_=i), [out], [in_])
        return self.op(eng, lambda E, o, i: E.tensor_copy(out=o, in_=i), [out], [in_])

    def memset(self, out, val, eng='pool'):
        return self.op(eng, lambda E, o: E.memset(o, val), [out], [])

    def reduce(self, out, in_, op, axis=AX.X, eng='dve'):
        return self.op(eng, lambda E, o, i: E.tensor_reduce(out=o, in_=i, axis=axis, op=op), [out], [in_])

    def scan(self, out, d0, d1, initial, op0, op1):
        return self.op('dve', lambda E, o, x, y: E.tensor_tensor_scan(out=o, data0=x, data1=y, initial=initial, op0=op0, op1=op1), [out], [d0, d1])


class Phase:
    def __init__(self, kb):
        self.kb = kb
        self.st = ExitStack()
        self.bufs = []

    def __enter__(self):
        self.st.__enter__()
        return self

    def __exit__(self, *a):
        self.kb.barrier()
        for b in self.bufs:
            for q_, s_ in b.ds.items():
                self.kb.free_ds[q_].append(s_)
            b.ds = {}
        return self.st.__exit__(*a)

    def sb(self, name, shape, dt):
        t = self.st.enter_context(self.kb.nc.sbuf_tensor(name, list(shape), dt))
        b = Buf(self.kb, t)
        self.bufs.append(b)
        return b

    def ps(self, name, shape, dt=F32):
        t = self.st.enter_context(self.kb.nc.psum_tensor(name, list(shape), dt))
        return Buf(self.kb, t)

    def bank(self, name, dt=F32):
        n = 512 if dt == F32 else 1024
        return self.st.enter_context(self.kb.nc.psum_tensor(name, [128, n], dt))

    def sub(self, ap):
        return Buf(self.kb, ap)


class Sub:
    def __init__(self, buf, ap):
        self.buf = buf
        self.ap = ap

    def __getitem__(self, idx):
        return View(self.buf, self.ap[idx])


def interleave(items, make_gen, K, nstage):
    it = iter(items)
    slots = [None] * K
    start_at = [(k * nstage) // K for k in range(K)]
    rnd = 0
    exhausted = False
    while True:
        alive = False
        for k in range(K):
            if slots[k] is None and not exhausted and rnd >= start_at[k]:
                try:
                    slots[k] = make_gen(next(it), k)
                except StopIteration:
                    exhausted = True
            if slots[k] is not None:
                alive = True
                try:
                    next(slots[k])
                except StopIteration:
                    slots[k] = None
        if not alive and exhausted:
            break
        rnd += 1

S = 4096; D = 1024; NIN = 14112; L = 2
ALPHA = (2 * L) ** 0.25
LN_EPS = 1e-5
O_GATE = 0; O_CONV = 3072; O_RWKV = 6144; O_ATTN = 9504
O_UV1 = NIN
NZ = NIN + 32


def cdiv(a, b):
    return (a + b - 1) // b


class Net:
    def __init__(self, debug=()):
        nc = self.nc = bass.Bass("TRN2", target_bir_lowering=False)
        self.kb = KB(nc)
        self.debug = debug
        self.inp = {}
        self.dr = {}
        self.uid = 0

    def din(self, name, shape, dt=F32):
        t = self.nc.dram_tensor(name, list(shape), dt, kind="ExternalInput")
        b = Buf(self.kb, t.ap(), dram=True)
        self.inp[name] = b
        return b

    def dscr(self, name, shape, dt):
        kind = "ExternalOutput" if name in self.debug else "Internal"
        t = self.nc.dram_tensor(name, list(shape), dt, kind=kind)
        b = Buf(self.kb, t.ap(), dram=True)
        self.dr[name] = b
        return b

    def n(self, s):
        self.uid += 1
        return "%s_%d" % (s, self.uid)


def layernorm_tile(net, ph, T, xin, gB, bB, out):
    kb = net.kb
    st = T['st']; mv = T['mv']; sd = T['sd']
    for c in range(2):
        kb.op('dve', lambda E, o, i: E.bn_stats(out=o, in_=i), [st[:, c, :]], [View(xin.buf, xin.ap[:, c * 512:(c + 1) * 512])])
    kb.op('dve', lambda E, o, i: E.bn_aggr(out=o, in_=i), [mv[:, :]], [st[:, :, :]])
    kb.ts(sd[:, 0:1], mv[:, 1:2], LN_EPS, ALU.add)
    kb.act(sd[:, 1:2], sd[:, 0:1], AF.Sqrt)
    kb.op('dve', lambda E, o, i: E.reciprocal(out=o, in_=i), [sd[:, 2:3]], [sd[:, 1:2]])
    kb.ts(out, xin, mv[:, 0:1], ALU.subtract, sd[:, 2:3], ALU.mult)
    kb.tt(out, out, gB, ALU.mult, eng='pool')
    kb.tt(out, out, bB, ALU.add, eng='pool')


def ln_scratch(net, ph):
    return {'st': ph.sb(net.n('lnst'), [128, 2, 6], F32), 'mv': ph.sb(net.n('lnmv'), [128, 2], F32),
            'sd': ph.sb(net.n('lnsd'), [128, 4], F32)}


def bcast_load(net, ph, name, src_ap):
    b = ph.sb(net.n(name), [128, 1024], F32)
    net.kb.dma('sp', b[:, :], View(net.cur_in, src_ap.partition_broadcast(128)))
    return b
def emit_h_tile(net, ph, T, hv, ti, hdst, hT, ident_f, router=None):
    kb = net.kb
    if hdst is not None:
        kb.dma('pool', hdst[ti * 128:(ti + 1) * 128, :], hv)
    if hT is None:
        return
    pT = T['pT']
    for kc in range(8):
        kb.transpose(pT[:, kc, :], View(hv.buf, hv.ap[:, kc * 128:(kc + 1) * 128]), ident_f[:, :])
    q, r = divmod(ti, 8)
    kb.copy(hT[q][:, :, r * 128:(r + 1) * 128], pT[:, :, :], eng='act')
    if router is not None:
        router(pT, ti)


def phase0(net, hT, C):
    kb = net.kb
    with Phase(kb) as ph:
        net.cur_in = net.inp['ln_in_g']
        gB = bcast_load(net, ph, 'gB', net.inp['ln_in_g'].t[:])
        net.cur_in = net.inp['ln_in_b']
        bB = bcast_load(net, ph, 'bB', net.inp['ln_in_b'].t[:])
        T = ln_scratch(net, ph)
        T['pT'] = ph.ps(net.n('pT'), [128, 8, 128], F32)
        xs = [ph.sb(net.n('x'), [128, 1024], F32) for _ in range(2)]
        hs = [ph.sb(net.n('h'), [128, 1024], F32) for _ in range(2)]
        x = net.inp['x']
        for ti in range(32):
            xt = xs[ti % 2]; ht = hs[ti % 2]
            kb.dma('sp', xt[:, :], x[ti * 128:(ti + 1) * 128, :])
            layernorm_tile(net, ph, T, xt[:, :], gB[:, :], bB[:, :], ht[:, :])
            emit_h_tile(net, ph, T, ht[:, :], ti, net.dr['hA'], hT, C['ident_f'])


def phase1(net, l, hT):
    kb = net.kb
    w_in = net.inp['w_in']
    zT = net.dr['zT']
    with Phase(kb) as ph:
        wst = [ph.sb(net.n('wst'), [128, 8, 128], F32) for _ in range(2)]
        wb = [ph.sb(net.n('wb'), [128, 8, 128], BF16) for _ in range(2)]
        zs = [ph.sb(net.n('zs'), [128, S], BF16) for _ in range(2)]
        pz = [ph.ps(net.n('pz'), [128, 512], F32) for _ in range(4)]
        blocks = [(c0, min(128, NIN - c0), None) for c0 in range(0, NIN, 128)]
        if l == 1:
            blocks.append((O_UV1, 32, 'v1'))
        wv = w_in.t[l].rearrange("(kc p) n -> p kc n", p=128)
        k = 0
        for bi, (c0, ncol, kind) in enumerate(blocks):
            st = wst[bi % 2]; w = wb[bi % 2]; z = zs[bi % 2]
            if kind is None:
                kb.dma('sp', st[:, :, 0:ncol], View(w_in, wv[:, :, c0:c0 + ncol]))
            else:
                v1 = net.inp['rwkv_v1']
                kb.dma('sp', st[:, :, 0:ncol], View(v1, v1.t[0].rearrange("(kc p) n -> p kc n", p=128)))
            kb.copy(w[:, :, 0:ncol], st[:, :, 0:ncol], eng='pool')
            for tt in range(8):
                p = pz[k % 4]
                for kc in range(8):
                    kb.mm(p[0:ncol, :], w[:, kc, 0:ncol], hT[tt // 2][:, kc, (tt % 2) * 512:(tt % 2 + 1) * 512], start=(kc == 0), stop=(kc == 7))
                kb.copy(z[0:ncol, tt * 512:(tt + 1) * 512], p[0:ncol, :], eng=('act' if k % 2 == 0 else 'dve'))
                k += 1
            kb.dma('pool', zT[c0:c0 + ncol, :], z[0:ncol, :])
NV = 123
V_CONV = 0; V_MUR = 24; V_MUK = 32; V_MUV = 40; V_MUWA = 48; V_MUG = 49; V_W0 = 51; V_A0 = 59; V_V0 = 67
V_KK = 75; V_KA = 83; V_RK = 91; V_LG = 99; V_LB = 107; V_OMKA = 115


def load_w_bf16(net, ph, src, ap, kc, ncol, name):
    kb = net.kb
    w = ph.sb(net.n(name), [128, kc, ncol], BF16)
    v = ap.rearrange("(kc p) n -> p kc n", p=128)
    with Phase(kb) as p2:
        st = [p2.sb(net.n('wstg'), [128, kc, 256], F32) for _ in range(2)]
        for i, c0 in enumerate(range(0, ncol, 256)):
            n = min(256, ncol - c0)
            kb.dma('sp', st[i % 2][:, :, 0:n], View(src, v[:, :, c0:c0 + n]))
            kb.copy(w[:, :, c0:c0 + n], st[i % 2][:, :, 0:n], eng='pool')
    return w


def branch_out(net, T, pw, rhs_list, goff, Gdst, t0, N, kp=128):
    kb = net.kb
    zT = net.dr['zT']
    gt = T['gt']; sg = T['sg']; go = T['go']; pp = T['pp']
    kb.dma('sp', gt[:, :, 0:N], View(zT, zT.t[goff:goff + 1024, t0:t0 + N].rearrange("(m p) t -> p m t", p=128)))
    for m in range(8):
        p = pp[m % len(pp)]
        for kc, r in enumerate(rhs_list):
            kb.mm(p[:, 0:N], pw[0:kp, kc, m * 128:(m + 1) * 128], r, start=(kc == 0), stop=(kc == len(rhs_list) - 1))
        kb.act(sg[:, 0:N], gt[:, m, 0:N], AF.Sigmoid)
        kb.tt(go[:, m, 0:N], p[:, 0:N], sg[:, 0:N], ALU.mult)
    kb.dma('pool', View(Gdst, Gdst.t[:, t0:t0 + N].rearrange("(m p) t -> p m t", p=128)), go[:, :, 0:N])


def bo_scratch(net, ph, N, npp=2):
    return {'gt': ph.sb(net.n('gt'), [128, 8, N], BF16), 'sg': ph.sb(net.n('sg'), [128, N], F32),
            'go': ph.sb(net.n('go'), [128, 8, N], BF16), 'pp': [ph.ps(net.n('pp'), [128, 512], F32) for _ in range(npp)]}


def phase_conv(net, l, vec):
    kb = net.kb
    zT = net.dr['zT']
    with Phase(kb) as ph:
        pa = load_w_bf16(net, ph, net.inp['p_a'], net.inp['p_a'].t[l], 8, 1024, 'pa')
        T = bo_scratch(net, ph, 512)
        cc = ph.sb(net.n('cc'), [128, 8, 514], BF16)
        chh = ph.sb(net.n('chh'), [128, 8, 514], BF16)
        cb = ph.sb(net.n('cb'), [128, 8, 512], BF16)
        yf = [ph.sb(net.n('yf'), [128, 514], F32) for _ in range(2)]
        of = [ph.sb(net.n('of'), [128, 512], F32) for _ in range(2)]
        u = ph.sb(net.n('u'), [128, 8, 512], BF16)

        def rows(off, a, b):
            return View(zT, zT.t[off:off + 1024, a:b].rearrange("(m p) t -> p m t", p=128))
        for tt in range(8):
            t0 = tt * 512
            if tt == 0:
                kb.memset(cc[:, :, 0:2], 0.0)
                kb.memset(chh[:, :, 0:2], 0.0)
                kb.dma('sp', cc[:, :, 2:514], rows(O_CONV + 1024, 0, 512))
                kb.dma('sp', chh[:, :, 2:514], rows(O_CONV + 2048, 0, 512))
            else:
                kb.dma('sp', cc[:, :, :], rows(O_CONV + 1024, t0 - 2, t0 + 512))
                kb.dma('sp', chh[:, :, :], rows(O_CONV + 2048, t0 - 2, t0 + 512))
            kb.dma('sp', cb[:, :, :], rows(O_CONV, t0, t0 + 512))
            for c in range(8):
                y = yf[c % 2]; o = of[c % 2]
                kb.tt(y[:, :], cc[:, c, :], chh[:, c, :], ALU.mult, eng='pool')
                kb.ts(o[:, :], y[:, 2:514], vec[:, V_CONV + 16 + c:V_CONV + 17 + c], ALU.mult)
                kb.stt(o[:, :], y[:, 1:513], vec[:, V_CONV + 8 + c:V_CONV + 9 + c], o[:, :], ALU.mult, ALU.add)
                kb.stt(o[:, :], y[:, 0:512], vec[:, V_CONV + c:V_CONV + 1 + c], o[:, :], ALU.mult, ALU.add)
                kb.tt(u[:, c, :], o[:, :], cb[:, c, :], ALU.mult, eng='pool')
            branch_out(net, T, pa, [u[:, c, :] for c in range(8)], O_GATE, net.dr['GA'], t0, 512)


def phase_mix(net, l, vec, hT, C, hsrc, hdst, CW):
    kb = net.kb
    with Phase(kb) as ph:
        wo = load_w_bf16(net, ph, net.inp['w_o'], net.inp['w_o'].t[l], 8, 1024, 'wo')
        net.cur_in = net.inp['ln1_g']; gB = bcast_load(net, ph, 'g1B', net.inp['ln1_g'].t[l])
        net.cur_in = net.inp['ln1_b']; bB = bcast_load(net, ph, 'b1B', net.inp['ln1_b'].t[l])
        T = ln_scratch(net, ph)
        T['pT'] = ph.ps(net.n('pT'), [128, 8, 128], F32)
        pm = [ph.ps(net.n('pm'), [128, 512], F32) for _ in range(2)]
        pr = ph.ps(net.n('pr'), [128, 128], F32)
        G = [ph.sb(net.n('G'), [128, 8, 512], BF16) for _ in range(3)]
        hres = [ph.sb(net.n('hres'), [128, 1024], F32) for _ in range(2)]
        pre = [ph.sb(net.n('pre'), [128, 1024], F32) for _ in range(2)]
        h1 = [ph.sb(net.n('h1'), [128, 1024], F32) for _ in range(2)]
        hTf = ph.sb(net.n('hTf'), [128, 8, 128], F32)
        wr = ph.sb(net.n('wr'), [128, 8, 128], F32)
        if True:
          kb.dma('sp', wr[:, :, :], net.inp['w_router'][:, :, :])
        whi = ph.sb(net.n('whi'), [128, 8, 128], BF16)
        wlo = ph.sb(net.n('wlo'), [128, 8, 128], BF16)
        hlo = ph.sb(net.n('hlo'), [128, 8, 128], BF16)
        if True:
            kb.copy(whi[:, :, :], wr[:, :, :])
            kb.tt(wr[:, :, :], wr[:, :, :], whi[:, :, :], ALU.subtract)
            kb.copy(wlo[:, :, :], wr[:, :, :])
        rb = ph.sb(net.n('rb'), [128, 16], F32)
        if True:
          kb.dma('sp', rb[:, :], net.inp['router_bias'][:, :])
        R = {k: ph.sb(net.n('r' + k), [128, 16], F32) for k in ['lg', 'e', 'pr', 'sel', 'selm', 'oh1', 'oh2', 'gw']}
        r1 = ph.sb(net.n('r1'), [128, 16], F32)
        ps6 = ph.sb(net.n('ps6'), [128, 4, 6], F32)
        gs = ph.sb(net.n('gs'), [128, 4], F32)
        eq = ph.sb(net.n('eq'), [128, 4], F32)
        pen = ph.sb(net.n('pen'), [128, 4], F32)
        Gsrc = [net.dr['GA'], net.dr['GB'], net.dr['GC']]

        def router(pT, ti):
            kb.copy(hTf[:, :, :], pT[:, :, :], eng='act')
            q_, r_ = divmod(ti, 8)
            hi_v = hT[q_][:, :, r_ * 128:(r_ + 1) * 128]
            kb.tt(hlo[:, :, :], hTf[:, :, :], hi_v, ALU.subtract)
            n_ = 0
            for kc in range(8):
                for a_, b_ in ((hT[q_][:, kc, r_ * 128:(r_ + 1) * 128], whi[:, kc, :]), (hT[q_][:, kc, r_ * 128:(r_ + 1) * 128], wlo[:, kc, :]), (hlo[:, kc, :], whi[:, kc, :])):
                    kb.mm(pr[:, :], a_, b_, start=(n_ == 0), stop=(n_ == 23))
                    n_ += 1
            lg = R['lg']
            kb.copy(lg[:, :], pr[:, 0:16])
            kb.reduce(r1[:, 0:1], lg[:, :], ALU.max)
            kb.ts(r1[:, 1:2], r1[:, 0:1], -1.0, ALU.mult)
            kb.act(R['e'][:, :], lg[:, :], AF.Exp, bias=r1[:, 1:2], accum=r1[:, 2:3])
            kb.op('dve', lambda E, o, i: E.reciprocal(out=o, in_=i), [r1[:, 3:4]], [r1[:, 2:3]])
            kb.ts(R['pr'][:, :], R['e'][:, :], r1[:, 3:4], ALU.mult)
            kb.tt(R['sel'][:, :], R['pr'][:, :], rb[:, :], ALU.add)
            s3 = R['sel'].t[:, :].rearrange("p (g e) -> p g e", e=4)
            k = 0
            for i in range(4):
                for j in range(i + 1, 4):
                    kb.tt(ps6[:, :, k], R['sel'].v(s3[:, :, i]), R['sel'].v(s3[:, :, j]), ALU.add)
                    k += 1
            kb.reduce(gs[:, :], ps6[:, :, :], ALU.max)
            kb.reduce(r1[:, 4:5], gs[:, :], ALU.max)
            kb.ts(eq[:, :], gs[:, :], r1[:, 4:5], ALU.is_ge)
            kb.ts(pen[:, :], eq[:, :], -1.0, ALU.add, 1e30, ALU.mult)
            m3 = R['selm'].t[:, :].rearrange("p (g e) -> p g e", e=4)
            kb.tt(R['selm'].v(m3), R['sel'].v(s3), eq.v(eq.t[:, :].unsqueeze(2).to_broadcast([128, 4, 4])), ALU.mult)
            kb.tt(R['selm'].v(m3), R['selm'].v(m3), pen.v(pen.t[:, :].unsqueeze(2).to_broadcast([128, 4, 4])), ALU.add)
            kb.reduce(r1[:, 5:6], R['selm'][:, :], ALU.max)
            kb.ts(R['oh1'][:, :], R['selm'][:, :], r1[:, 5:6], ALU.is_ge)
            kb.stt(R['selm'][:, :], R['oh1'][:, :], -1e30, R['selm'][:, :], ALU.mult, ALU.add)
            kb.reduce(r1[:, 6:7], R['selm'][:, :], ALU.max)
            kb.ts(R['oh2'][:, :], R['selm'][:, :], r1[:, 6:7], ALU.is_ge)
            kb.tt(R['oh1'][:, :], R['oh1'][:, :], R['oh2'][:, :], ALU.add)
            kb.tt(R['gw'][:, :], R['pr'][:, :], R['oh1'][:, :], ALU.mult)
            kb.reduce(r1[:, 7:8], R['gw'][:, :], ALU.add)
            kb.op('dve', lambda E, o, i: E.reciprocal(out=o, in_=i), [r1[:, 8:9]], [r1[:, 7:8]])
            kb.ts(CW[:, ti, :], R['gw'][:, :], r1[:, 8:9], ALU.mult)

        for tt in range(8):
            for b in range(3):
                kb.dma('sp', G[b][:, :, :], View(Gsrc[b], Gsrc[b].t[:, tt * 512:(tt + 1) * 512].rearrange("(m p) t -> p m t", p=128)))
            for s in range(4):
                ti = tt * 4 + s
                hr = hres[ti % 2]; pv = pre[ti % 2]; ho = h1[ti % 2]
                kb.dma('sp', hr[:, :], hsrc[ti * 128:(ti + 1) * 128, :])
                for dh in range(2):
                    p = pm[dh]
                    n = 0
                    for b in range(3):
                        for kc in range(8):
                            kb.mm(p[:, :], G[b][:, kc, s * 128:(s + 1) * 128], wo[:, kc, dh * 512:(dh + 1) * 512], start=(n == 0), stop=(n == 23))
                            n += 1
                    kb.stt(pv[:, dh * 512:(dh + 1) * 512], hr[:, dh * 512:(dh + 1) * 512], ALPHA, p[:, :], ALU.mult, ALU.add)
                layernorm_tile(net, ph, T, pv[:, :], gB[:, :], bB[:, :], ho[:, :])
                emit_h_tile(net, ph, T, ho[:, :], ti, hdst, hT, C['ident_f'], router=router)


def phase_moe(net, l, hT, C, CW, hsrc, hdst, last, out_dst):
    kb = net.kb
    wg_d = net.inp['w_gate']; wu_d = net.inp['w_up']; wd_d = net.inp['w_down']
    with Phase(kb) as ph:
        net.cur_in = net.inp['ln2_g']; gB = bcast_load(net, ph, 'g2B', net.inp['ln2_g'].t[l])
        net.cur_in = net.inp['ln2_b']; bB = bcast_load(net, ph, 'b2B', net.inp['ln2_b'].t[l])
        T = ln_scratch(net, ph)
        T['pT'] = ph.ps(net.n('pT'), [128, 8, 128], F32)
        pg = [ph.ps(net.n('pg'), [128, 512], F32) for _ in range(2)]
        pu = [ph.ps(net.n('pu'), [128, 512], F32) for _ in range(2)]
        pd = [ph.ps(net.n('pd'), [128, 512], F32) for _ in range(2)]
        acc = ph.sb(net.n('acc'), [128, 8, 1024], F32)
        wgb = [ph.sb(net.n('wgb'), [128, 8, 512], BF16) for _ in range(2)]
        wub = [ph.sb(net.n('wub'), [128, 8, 512], BF16) for _ in range(2)]
        wdb = [ph.sb(net.n('wdb'), [128, 4, 1024], BF16) for _ in range(2)]
        stg = [ph.sb(net.n('stg'), [128, 4, 512], F32) for _ in range(2)]
        hid = [ph.sb(net.n('hid'), [128, 4, 512], BF16) for _ in range(2)]
        sl = [ph.sb(net.n('sl'), [128, 512], F32) for _ in range(2)]
        hres = [ph.sb(net.n('hres'), [128, 1024], F32) for _ in range(1)]
        pre = [ph.sb(net.n('pre'), [128, 1024], F32) for _ in range(1)]
        h2 = [ph.sb(net.n('h2'), [128, 1024], F32) for _ in range(1)]
        ns = 0
        hT_new = hT
        for q in range(4):
            for e in range(16):
                wgt = wgb[e % 2]; wut = wub[e % 2]; wdt = wdb[e % 2]
                gv = wg_d.t[l, e].rearrange("(kc p) n -> p kc n", p=128)
                uv = wu_d.t[l, e].rearrange("(kc p) n -> p kc n", p=128)
                dv = wd_d.t[l, e].rearrange("(kc p) n -> p kc n", p=128)
                for hh in range(2):
                    s_ = stg[ns % 2]; ns += 1
                    kb.dma('sp', s_[:, :, :], View(wg_d, gv[:, hh * 4:(hh + 1) * 4, :]))
                    kb.copy(wgt[:, hh * 4:(hh + 1) * 4, :], s_[:, :, :], eng='pool')
                for hh in range(2):
                    s_ = stg[ns % 2]; ns += 1
                    kb.dma('sp', s_[:, :, :], View(wu_d, uv[:, hh * 4:(hh + 1) * 4, :]))
                    kb.copy(wut[:, hh * 4:(hh + 1) * 4, :], s_[:, :, :], eng='pool')
                for hh in range(2):
                    s_ = stg[ns % 2]; ns += 1
                    kb.dma('sp', s_[:, :, :], View(wd_d, dv[:, :, hh * 512:(hh + 1) * 512]))
                    kb.copy(wdt[:, :, hh * 512:(hh + 1) * 512], s_[:, :, :], eng='pool')
                for tt in range(2):
                    hd = hid[tt % 2]
                    rhs = lambda kc: hT[q][:, kc, tt * 512:(tt + 1) * 512]
                    for f in range(4):
                        g_ = pg[f % 2]; u_ = pu[f % 2]; s2 = sl[f % 2]
                        for kc in range(8):
                            kb.mm(g_[:, :], wgt[:, kc, f * 128:(f + 1) * 128], rhs(kc), start=(kc == 0), stop=(kc == 7))
                        for kc in range(8):
                            kb.mm(u_[:, :], wut[:, kc, f * 128:(f + 1) * 128], rhs(kc), start=(kc == 0), stop=(kc == 7))
                        kb.act(s2[:, :], g_[:, :], AF.Silu)
                        kb.tt(hd[:, f, :], u_[:, :], s2[:, :], ALU.mult)
                    for s in range(4):
                        tl = tt * 4 + s
                        ti = q * 8 + tl
                        for dh in range(2):
                            p = pd[dh]
                            for f in range(4):
                                kb.mm(p[:, :], hd[:, f, s * 128:(s + 1) * 128], wdt[:, f, dh * 512:(dh + 1) * 512], start=(f == 0), stop=(f == 3))
                            a = acc[:, tl, dh * 512:(dh + 1) * 512]
                            if e == 0:
                                kb.ts(a, p[:, :], CW[:, ti, e:e + 1], ALU.mult)
                            else:
                                kb.stt(a, p[:, :], CW[:, ti, e:e + 1], a, ALU.mult, ALU.add)
            for tl in range(8):
                ti = q * 8 + tl
                hr = hres[0]; pv = pre[0]; ho = h2[0]
                kb.dma('sp', hr[:, :], hsrc[ti * 128:(ti + 1) * 128, :])
                kb.stt(pv[:, :], hr[:, :], ALPHA, acc[:, tl, :], ALU.mult, ALU.add)
                layernorm_tile(net, ph, T, pv[:, :], gB[:, :], bB[:, :], ho[:, :])
                if last:
                    kb.dma('pool', out_dst[ti * 128:(ti + 1) * 128, :], ho[:, :])
                else:
                    emit_h_tile(net, ph, T, ho[:, :], ti, hdst, hT, C['ident_f'])
GROUPS = ((128, 1), (512, 4), (2048, 16))


def phase_attn(net, l, C):
    kb = net.kb
    zT = net.dr['zT']; YC = net.dr['YC']
    with Phase(kb) as ph:
        C2 = ph.sb(net.n('C2'), [128, S], BF16)
        S2 = ph.sb(net.n('S2'), [128, S], BF16)
        with Phase(kb) as p2:
            pi_ = p2.sb(net.n('posi'), [128, 512], I32)
            pf = p2.sb(net.n('posf'), [128, 512], F32)
            uf = p2.sb(net.n('uf'), [128, 512], F32)
            ui = p2.sb(net.n('ui'), [128, 512], I32)
            fr = p2.sb(net.n('fr'), [128, 512], F32)
            pos = net.inp['positions']
            for c in range(8):
                kb.dma('sp', pi_[:, :], View(pos, pos.t[c * 512:(c + 1) * 512].partition_broadcast(128)))
                kb.copy(pf[:, :], pi_[:, :])
                for tab, sh in ((S2, 0.0), (C2, 0.25)):
                    kb.ts(uf[:, :], pf[:, :], C['invf'][:, 0:1], ALU.mult, sh, ALU.add)
                    kb.copy(ui[:, :], uf[:, :])
                    kb.copy(fr[:, :], ui[:, :])
                    kb.tt(fr[:, :], uf[:, :], fr[:, :], ALU.subtract)
                    kb.act(tab[:, c * 512:(c + 1) * 512], fr[:, :], AF.Sin, scale=6.28318)
        q = ph.sb(net.n('q'), [128, S], BF16)
        k = ph.sb(net.n('k'), [128, S], BF16)
        v = ph.sb(net.n('v'), [128, S], BF16)
        qr = q; kr = k
        VT = ph.sb(net.n('VT'), [128, 32, 128], BF16)
        OG = [[ph.sb(net.n('OG'), [65, S], F32) for _ in range(2)] for _ in range(3)]
        t1 = ph.sb(net.n('t1'), [128, 512], F32)
        t2 = ph.sb(net.n('t2'), [128, 512], F32)
        KA = 4
        lrow = ph.sb(net.n('lrow'), [65, 6, 512], F32)
        wrow = ph.sb(net.n('wrow'), [65, 3, 512], BF16)
        ycs = ph.sb(net.n('ycs'), [64, 512], F32)
        ycb = ph.sb(net.n('ycb'), [64, 512], BF16)
        pf_ = [ph.ps(net.n('pf'), [128, 512], F32) for _ in range(KA)]
        ps_pt_ = [ph.ps(net.n('ps_pt'), [128, 2, 128], BF16) for _ in range(KA)]
        ps_s_ = [Sub(b, b.t[:, 0:256]) for b in pf_]
        ps_o_ = [Sub(b, b.t[:, 256:320]) for b in pf_]
        ps_t_ = [Sub(b, b.t[0:65, 320:448]) for b in pf_]
        ps_rot = pf_[0]
        ps_bc = Sub(pf_[1], pf_[1].t[0:64, :])
        ps_vt = Sub(ps_pt_[0], ps_pt_[0].t[:, 0, :])
        sm_ = [ph.sb(net.n('sm'), [128, 256], F32) for _ in range(KA)]
        pb_ = [ph.sb(net.n('pb'), [128, 256], BF16) for _ in range(KA)]
        PT_ = [ph.sb(net.n('PT'), [128, 2, 128], BF16) for _ in range(KA)]
        aug_ = [ph.sb(net.n('aug'), [128, 65], F32) for _ in range(KA)]
        r1_ = [ph.sb(net.n('ar1'), [128, 8], F32) for _ in range(KA)]
        for hp in range(4):
            for g, (window, d) in enumerate(GROUPS):
                base = O_ATTN + g * 512 + hp * 128
                kb.dma('sp', q[:, :], zT[base:base + 128, :])
                kb.dma('sp', k[:, :], zT[base + 1536:base + 1536 + 128, :])
                kb.dma('sp', v[:, :], zT[base + 3072:base + 3072 + 128, :])
                for src, dst in ((q, qr), (k, kr)):
                    for c in range(8):
                        sl_ = slice(c * 512, (c + 1) * 512)
                        kb.mm(ps_rot[:, :], C['PT'][:, :], src[:, sl_])
                        kb.tt(t1[:, :], ps_rot[:, :], S2[:, sl_], ALU.mult)
                        kb.tt(t2[:, :], src[:, sl_], C2[:, sl_], ALU.mult, eng='pool')
                        kb.tt(dst[:, sl_], t1[:, :], t2[:, :], ALU.add)
                Lr = S // d
                nb = Lr // 128

                def toks(r, n0, cnt):
                    a = r + d * n0 * 128
                    return slice(a, a + d * 128 * cnt - (d - 1), d)
                for r in range(d):
                    for n in range(nb):
                        kb.transpose(ps_vt[:, :], v[:, toks(r, n, 1)], C['ident_b'][:, :])
                        kb.copy(VT[:, r * nb + n, :], ps_vt[:, :], eng='act')
                def block_gen(item, slot, g=g, d=d, nb=nb, toks=toks):
                    hd, r, n = item
                    rows = slice(hd * 64, hd * 64 + 64)
                    og = OG[g][hd]
                    sm = sm_[slot]; pb = pb_[slot]; PT = PT_[slot]; aug = aug_[slot]; r1 = r1_[slot]
                    ps_s = ps_s_[slot]; ps_pt = ps_pt_[slot]; ps_o = ps_o_[slot]; ps_t = ps_t_[slot]
                    bi = r * nb + n
                    if n == 0:
                        kb.mm(ps_s[:, 128:256], qr[rows, toks(r, n, 1)], kr[rows, toks(r, n, 1)])
                        kb.stt(sm[:, 128:256], ps_s[:, 128:256], 0.125, C['amask'][:, 128:256], ALU.mult, ALU.add)
                        kb.memset(sm[:, 0:128], -1e30)
                    else:
                        kb.mm(ps_s[:, :], qr[rows, toks(r, n, 1)], kr[rows, toks(r, n - 1, 2)])
                        kb.stt(sm[:, :], ps_s[:, :], 0.125, C['amask'][:, :], ALU.mult, ALU.add)
                    yield
                    kb.reduce(r1[:, 0:1], sm[:, :], ALU.max)
                    kb.ts(r1[:, 1:2], r1[:, 0:1], -1.0, ALU.mult)
                    kb.act(pb[:, :], sm[:, :], AF.Exp, bias=r1[:, 1:2], accum=r1[:, 2:3])
                    yield
                    for j in range(2):
                        kb.transpose(ps_pt[:, j, :], pb[:, j * 128:(j + 1) * 128], C['ident_b'][:, :])
                    kb.copy(PT[:, :, :], ps_pt[:, :, :])
                    yield
                    if n == 0:
                        kb.mm(ps_o[:, :], PT[:, 1, :], VT[:, bi, rows])
                    else:
                        kb.mm(ps_o[:, :], PT[:, 0, :], VT[:, bi - 1, rows], start=True, stop=False)
                        kb.mm(ps_o[:, :], PT[:, 1, :], VT[:, bi, rows], start=False, stop=True)
                    kb.op('dve', lambda E, o, i: E.reciprocal(out=o, in_=i), [r1[:, 3:4]], [r1[:, 2:3]])
                    kb.ts(aug[:, 0:64], ps_o[:, :], r1[:, 3:4], ALU.mult)
                    kb.act(r1[:, 4:5], r1[:, 2:3], AF.Ln)
                    kb.tt(aug[:, 64:65], r1[:, 4:5], r1[:, 0:1], ALU.add)
                    yield
                    kb.transpose(ps_t[:, :], aug[:, :], C['ident_f'][:, :])
                    kb.copy(og[:, toks(r, n, 1)], ps_t[:, :], eng='act')
                interleave([(hd, r, n) for hd in range(2) for r in range(d) for n in range(nb)], block_gen, KA, 5)
            for hd in range(2):
                for c in range(8):
                    sl_ = slice(c * 512, (c + 1) * 512)
                    L0 = OG[0][hd][64:65, sl_]; L1 = OG[1][hd][64:65, sl_]; L2 = OG[2][hd][64:65, sl_]
                    m = lrow[64:65, 0, :]
                    kb.tt(m, L0, L1, ALU.max)
                    kb.tt(m, m, L2, ALU.max)
                    for g, Lg in enumerate((L0, L1, L2)):
                        kb.tt(lrow[64:65, 1 + g, :], Lg, m, ALU.subtract)
                        kb.act(lrow[64:65, 1 + g, :], lrow[64:65, 1 + g, :], AF.Exp)
                    den = lrow[64:65, 4, :]
                    kb.tt(den, lrow[64:65, 1, :], lrow[64:65, 2, :], ALU.add)
                    kb.tt(den, den, lrow[64:65, 3, :], ALU.add)
                    kb.op('dve', lambda E, o, i: E.reciprocal(out=o, in_=i), [lrow[64:65, 5, :]], [den])
                    for g in range(3):
                        kb.tt(wrow[64:65, g, :], lrow[64:65, 1 + g, :], lrow[64:65, 5, :], ALU.mult)
                    for g in range(3):
                        kb.mm(ps_bc[:, :], C['ones_b'][64:65, 0:64], wrow[64:65, g, :])
                        if g == 0:
                            kb.tt(ycs[:, :], ps_bc[:, :], OG[g][hd][0:64, sl_], ALU.mult)
                        else:
                            kb.tt(t1[0:64, :], ps_bc[:, :], OG[g][hd][0:64, sl_], ALU.mult)
                            kb.tt(ycs[:, :], ycs[:, :], t1[0:64, :], ALU.add)
                    kb.copy(ycb[:, :], ycs[:, :], eng='act')
                    kb.dma('pool', YC[hp * 128 + hd * 64:hp * 128 + hd * 64 + 64, sl_], ycb[:, :])
    with Phase(kb) as ph:
        pc = load_w_bf16(net, ph, net.inp['p_c'], net.inp['p_c'].t[l], 4, 1024, 'pc')
        T = bo_scratch(net, ph, 512, npp=1)
        yc = ph.sb(net.n('yc'), [128, 4, 512], BF16)
        for tt in range(8):
            kb.dma('sp', yc[:, :, :], View(YC, YC.t[:, tt * 512:(tt + 1) * 512].rearrange("(m p) t -> p m t", p=128)))
            branch_out(net, T, pc, [yc[:, kc, :] for kc in range(4)], O_GATE + 2048, net.dr['GC'], tt * 512, 512)
C0 = float(np.exp(-0.5))
GN_EPS = 1e-5 * 64
TT = 256


def phase_rwkv(net, l, vec, C):
    kb = net.kb
    zT = net.dr['zT']; VF = net.dr['VF']
    with Phase(kb) as ph:
        pbw = load_w_bf16(net, ph, net.inp['p_b'], net.inp['p_b'].t[l], 8, 1024, 'pbw')
        wa2 = ph.sb(net.n('wa2'), [128, 1024], BF16)
        g2a = ph.sb(net.n('g2a'), [128, 1024], BF16)
        g2b = ph.sb(net.n('g2b'), [32, 1024], BF16)
        v2 = ph.sb(net.n('v2'), [32, 1024], BF16)
        with Phase(kb) as p2:
            st = p2.sb(net.n('lst'), [128, 1024], F32)
            kb.dma('sp', st[0:64, :], net.inp['rwkv_w2'][l])
            kb.dma('sp', st[64:128, :], net.inp['rwkv_a2'][l])
            kb.copy(wa2[:, :], st[:, :])
            st2 = p2.sb(net.n('lst2'), [128, 1024], F32)
            kb.dma('sp', st2[:, :], net.inp['rwkv_g2'][l, 0:128, :])
            kb.copy(g2a[:, :], st2[:, :])
            st3 = p2.sb(net.n('lst3'), [32, 1024], F32)
            kb.dma('sp', st3[:, :], net.inp['rwkv_g2'][l, 128:160, :])
            kb.copy(g2b[:, :], st3[:, :])
            if l == 1:
                st4 = p2.sb(net.n('lst4'), [32, 1024], F32)
                kb.dma('sp', st4[:, :], net.inp['rwkv_v2'][0])
                kb.copy(v2[:, :], st4[:, :])
        KR = 2
        W = TT + 1
        zin = {k_: ph.sb(net.n('z' + k_), [128, 8, W], BF16) for k_ in 'rkv'}
        zwa = ph.sb(net.n('zwa'), [128, W], BF16)
        zg1 = ph.sb(net.n('zg1'), [128, W], BF16)
        zg2 = ph.sb(net.n('zg2'), [32, W], BF16)
        uv1 = ph.sb(net.n('uv1'), [32, TT], BF16)
        ft = {k_: ph.sb(net.n('ft' + k_), [128, TT], F32) for k_ in ['d', 'wa', 'g1', 'g2']}
        bt16 = {k_: ph.sb(net.n('bt' + k_), [128, TT], BF16) for k_ in ['wa', 'g1']}
        bg2 = ph.sb(net.n('bg2'), [32, TT], BF16)
        yb = ph.sb(net.n('yb'), [128, 8, TT], BF16)
        STf = [ph.sb(net.n('STf'), [128, 128], F32) for _ in range(8)]
        STb = [ph.sb(net.n('STb'), [128, 128], BF16) for _ in range(8)]
        for j in range(8):
            kb.memset(STf[j][:, :], 0.0)
            kb.memset(STb[j][:, :], 0.0)

        def mkset():
            X = {}
            X['vf_t'] = ph.sb(net.n('vf_t'), [128, TT], BF16)
            X['f'] = {k_: ph.sb(net.n('f' + k_), [128, TT], F32) for k_ in
                      ['d', 'r', 'k', 'v', 'sg', 'a', 'g', 's', 'kk', 'kkn', 'k2', 'tmp', 'cs', 'e', 'ka', 'y', 'yc', 'bon']}
            X['b16'] = {k_: ph.sb(net.n('b' + k_), [128, TT], BF16) for k_ in ['sq', 'rk']}
            X['ARt'] = ph.sb(net.n('ARt'), [128, 4, 192], BF16)
            for k_ in ('Bt', 'Kt', 'Bb', 'Kb', 'Vb'):
                X[k_] = ph.sb(net.n(k_), [128, 4, 128], BF16)
            for k_ in ('ARt', 'Bt', 'Kt', 'Bb', 'Kb', 'Vb'):
                kb.memset(X[k_][:, :, :], 0.0)
            X['AB'] = ph.sb(net.n('AB'), [128, 4, 192], BF16)
            X['AK'] = ph.sb(net.n('AK'), [128, 4, 192], BF16)
            for k_ in ('Ui', 'Li', 'Gi'):
                X[k_] = [ph.sb(net.n(k_), [128, 4, 128], BF16) for _ in range(2)]
            for k_ in ('Vtm', 'Bbtm', 'Kbtm'):
                X[k_] = ph.sb(net.n(k_), [128, 4, 128], BF16)
            X['RHS'] = ph.sb(net.n('RHS'), [128, 128], BF16)
            X['SA'] = ph.sb(net.n('SA'), [128, 128], BF16)
            X['Wc'] = ph.sb(net.n('Wc'), [128, 4], F32)
            P = [ph.ps(net.n('P'), [128, 512], F32) for _ in range(4)]
            X['P'] = P
            X['pA'] = lambda c: View(P[c // 2], P[c // 2].t[:, (c % 2) * 256:(c % 2) * 256 + 192])
            X['pA2'] = lambda h2: View(P[h2], P[h2].t[:, :].rearrange("p (c t) -> p c t", t=256)[:, :, 0:192])
            X['pB'] = Sub(P[0], P[0].t[:, :].rearrange("p (c t) -> p c t", t=128))
            X['pC'] = Sub(P[1], P[1].t[:, :].rearrange("p (c t) -> p c t", t=128))
            X['pD'] = Sub(P[2], P[2].t[:, :].rearrange("p (c t) -> p c t", t=128))
            X['pS'] = Sub(P[3], P[3].t[:, 0:256])
            X['pTr'] = Sub(P[3], P[3].t[:, 256:512].bitcast(BF16).rearrange("p (c t) -> p c t", t=128))
            return X
        sets = [mkset() for _ in range(KR)]
        T = {'gt': ph.sb(net.n('gt'), [128, 8, TT], BF16), 'sg': ph.sb(net.n('sg'), [128, TT], F32),
             'go': ph.sb(net.n('go'), [128, 8, TT], BF16), 'pp': [sets[0]['P'][2]]}

        def rows(off, a, b):
            return View(zT, zT.t[off:off + 1024, a:b].rearrange("(m p) t -> p m t", p=128))

        def vc(col, j=0):
            return vec[:, col + j:col + j + 1]

        def shift(dst, src, mu, d_):
            P_ = src.ap.shape[0]
            dd = d_[0:P_, :]
            kb.tt(dd, View(src.buf, src.ap[:, 0:TT]), View(src.buf, src.ap[:, 1:W]), ALU.subtract, eng='pool')
            kb.stt(dst, dd, mu, View(src.buf, src.ap[:, 1:W]), ALU.mult, ALU.add)

        def bd_write(dst, c_lo, src_fn):
            for hd in range(2):
                rs_ = slice(hd * 64, hd * 64 + 64)
                src_fn(rs_, dst[rs_, :, c_lo + hd * 64:c_lo + hd * 64 + 64])

        def v3(b, rs_=slice(0, 128)):
            return b.v(b.t[rs_, :].rearrange("p (c t) -> p c t", t=64))

        def body(ti, j, X):
            t0 = ti * TT
            f = X['f']; b16 = X['b16']; vf_t = X['vf_t']
            ARt = X['ARt']; Bt = X['Bt']; Kt = X['Kt']; Bb = X['Bb']; Kb = X['Kb']; Vb = X['Vb']
            AB = X['AB']; AK = X['AK']; Ui = X['Ui']; Li = X['Li']; Gi = X['Gi']
            Vtm = X['Vtm']; Bbtm = X['Bbtm']; Kbtm = X['Kbtm']; RHS = X['RHS']; SA = X['SA']; Wc = X['Wc']
            pA = X['pA']; pA2 = X['pA2']; pB = X['pB']; pC = X['pC']; pD = X['pD']; pS = X['pS']; pTr = X['pTr']
            cs_ = slice(j * 128, (j + 1) * 128)
            shift(f['r'][:, :], zin['r'][:, j, :], vc(V_MUR, j), f['d'])
            shift(f['k'][:, :], zin['k'][:, j, :], vc(V_MUK, j), f['d'])
            shift(f['v'][:, :], zin['v'][:, j, :], vc(V_MUV, j), f['d'])
            yield
            kb.mm(pS[:, :], wa2[0:64, cs_], bt16['wa'][0:64, :])
            kb.act(f['sg'][:, :], pS[:, :], AF.Sigmoid, bias=vc(V_W0, j))
            kb.mm(pS[:, :], wa2[64:128, cs_], bt16['wa'][64:128, :])
            kb.act(f['a'][:, :], pS[:, :], AF.Sigmoid, bias=vc(V_A0, j))
            yield
            kb.mm(pS[:, :], g2a[:, cs_], bt16['g1'][:, :], start=True, stop=False)
            kb.mm(pS[:, :], g2b[:, cs_], bg2[:, :], start=False, stop=True)
            kb.copy(f['g'][:, :], pS[:, :], eng='act')
            if l == 1:
                kb.mm(pS[:, :], v2[:, cs_], uv1[:, :])
                kb.act(f['s'][:, :], pS[:, :], AF.Sigmoid, bias=vc(V_V0, j))
                kb.dma('sp', vf_t[:, :], VF[cs_, t0:t0 + TT])
                kb.tt(f['tmp'][:, :], vf_t[:, :], f['v'][:, :], ALU.subtract)
                kb.tt(f['tmp'][:, :], f['tmp'][:, :], f['s'][:, :], ALU.mult)
                kb.tt(f['v'][:, :], f['v'][:, :], f['tmp'][:, :], ALU.add)
            else:
                kb.copy(vf_t[:, :], f['v'][:, :], eng='act')
                kb.dma('pool', VF[cs_, t0:t0 + TT], vf_t[:, :])
            yield
            kb.ts(f['kk'][:, :], f['k'][:, :], vc(V_KK, j), ALU.mult)
            kb.tt(b16['sq'][:, :], f['kk'][:, :], f['kk'][:, :], ALU.mult)
            kb.mm(pS[:, :], C['bones_b'][:, :], b16['sq'][:, :])
            kb.ts(f['tmp'][:, :], pS[:, :], 1e-24, ALU.max)
            kb.act(f['tmp'][:, :], f['tmp'][:, :], AF.Sqrt)
            kb.op('dve', lambda E, o, i: E.reciprocal(out=o, in_=i), [f['tmp'][:, :]], [f['tmp'][:, :]])
            kb.tt(f['kkn'][:, :], f['kk'][:, :], f['tmp'][:, :], ALU.mult)
            yield
            kb.ts(f['tmp'][:, :], f['a'][:, :], vc(V_KA, j), ALU.mult, vc(V_OMKA, j), ALU.add)
            kb.tt(f['k2'][:, :], f['k'][:, :], f['tmp'][:, :], ALU.mult)
            kb.tt(f['ka'][:, :], f['kkn'][:, :], f['a'][:, :], ALU.mult)
            kb.scan(f['cs'][:, :], C['cmask'][:, :], f['sg'][:, :], 0.0, ALU.mult, ALU.add)
            cs3 = v3(f['cs'])
            yield
            kb.tt(f['tmp'][:, :], f['cs'][:, :], f['sg'][:, :], ALU.subtract)
            kb.act(f['e'][:, :], f['tmp'][:, :], AF.Exp, scale=-C0)
            kb.tt(f['tmp'][:, :], f['kkn'][:, :], f['e'][:, :], ALU.mult)
            bd_write(ARt, 0, lambda rs_, o: kb.ts(o, v3(f['tmp'], rs_), -1.0, ALU.mult))
            kb.act(f['e'][:, :], f['cs'][:, :], AF.Exp, scale=-C0)
            kb.tt(ARt.v(ARt.t[:, :, 128:192]), v3(f['r']), v3(f['e']), ALU.mult)
            yield
            kb.act(f['e'][:, :], f['cs'][:, :], AF.Exp, scale=C0)
            bd_write(Bt, 0, lambda rs_, o: kb.tt(o, v3(f['ka'], rs_), v3(f['e'], rs_), ALU.mult))
            bd_write(Kt, 0, lambda rs_, o: kb.tt(o, v3(f['k2'], rs_), v3(f['e'], rs_), ALU.mult, eng='pool'))
            yield
            kb.tt(v3(f['tmp']), f['cs'].v(cs3.ap[:, :, 63:64].to_broadcast([128, 4, 64])), cs3, ALU.subtract)
            kb.act(f['e'][:, :], f['tmp'][:, :], AF.Exp, scale=-C0)
            kb.act(Wc[:, :], f['cs'].v(cs3.ap[:, :, 63]), AF.Exp, scale=-C0)
            bd_write(Bb, 0, lambda rs_, o: kb.tt(o, v3(f['ka'], rs_), v3(f['e'], rs_), ALU.mult))
            bd_write(Kb, 0, lambda rs_, o: kb.tt(o, v3(f['k2'], rs_), v3(f['e'], rs_), ALU.mult, eng='pool'))
            bd_write(Vb, 0, lambda rs_, o: kb.copy(o, v3(f['v'], rs_), eng='act'))
            yield
            mab2 = C['m_ab'].v(C['m_ab'].t[:, :].unsqueeze(1).to_broadcast([128, 2, 192]))
            for c in range(4):
                kb.mm(pA(c), Bt[:, c, :], ARt[:, c, :])
            for h2 in range(2):
                kb.tt(AB[:, 2 * h2:2 * h2 + 2, :], pA2(h2), mab2, ALU.mult)
            yield
            for c in range(4):
                kb.mm(pA(c), Kt[:, c, :], ARt[:, c, :])
            for h2 in range(2):
                kb.tt(AK[:, 2 * h2:2 * h2 + 2, :], pA2(h2), mab2, ALU.mult)
            yield
            for c in range(4):
                kb.mm(pB[:, c, :], ARt[:, c, 0:128], Bt[:, c, :])
            kb.tt(Li[0][:, :, :], pB[:, :, :], C['m_l'].v(C['m_l'].t[:, :].unsqueeze(1).to_broadcast([128, 4, 128])), ALU.mult)
            kb.copy(Ui[0][:, :, :], AB[:, :, 0:128], eng='pool')
            kb.tt(Gi[0][:, :, :], AB[:, :, 0:128], C['ident_b'].v(C['ident_b'].t[:, :].unsqueeze(1).to_broadcast([128, 4, 128])), ALU.add, eng='pool')
            yield
            cu, cl_, cg = 0, 0, 0
            for lev in range(5):
                Uo, Lo, Go = Ui[cu], Li[cl_], Gi[cg]
                Un, Ln, Gn = Ui[1 - cu], Li[1 - cl_], Gi[1 - cg]
                for c in range(4):
                    kb.mm(pB[:, c, :], Uo[:, c, :], Lo[:, c, :])
                if lev < 4:
                    for c in range(4):
                        kb.mm(pC[:, c, :], Lo[:, c, :], Uo[:, c, :])
                kb.copy(Ln[:, :, :], pB[:, :, :], eng='act')
                if lev < 4:
                    kb.copy(Un[:, :, :], pC[:, :, :], eng='dve')
                yield
                for c in range(4):
                    kb.mm(pD[:, c, :], Ln[:, c, :], Go[:, c, :])
                kb.tt(Gn[:, :, :], pD[:, :, :], Go[:, :, :], ALU.add)
                cu, cl_, cg = 1 - cu, 1 - cl_, 1 - cg
                yield
            G = Gi[cg]
            for src, dst in ((Vb, Vtm), (Bb, Bbtm), (Kb, Kbtm)):
                for c in range(4):
                    kb.transpose(pTr[:, c, :], src[:, c, :], C['ident_b'][:, :])
                kb.copy(dst[:, :, :], pTr[:, :, :], eng='act')
            yield
            for c in range(4):
                kb.mm(pD[:, 0, :], ARt[:, c, 0:128], STb[j][:, :], start=True, stop=False)
                kb.mm(pD[:, 0, :], AK[:, c, 0:128], Vtm[:, c, :], start=False, stop=True)
                kb.copy(RHS[:, :], pD[:, 0, :], eng='act')
                yield
                kb.mm(pD[:, 1, :], G[:, c, :], RHS[:, :])
                kb.copy(SA[:, :], pD[:, 1, :], eng='act')
                yield
                kb.mm(pS[:, c * 64:(c + 1) * 64], STb[j][:, :], ARt[:, c, 128:192], start=True, stop=False)
                kb.mm(pS[:, c * 64:(c + 1) * 64], SA[:, :], AB[:, c, 128:192], start=False, stop=False)
                kb.mm(pS[:, c * 64:(c + 1) * 64], Vtm[:, c, :], AK[:, c, 128:192], start=False, stop=True)
                kb.mm(pD[:, 2, :], Bbtm[:, c, :], SA[:, :], start=True, stop=False)
                kb.mm(pD[:, 2, :], Kbtm[:, c, :], Vtm[:, c, :], start=False, stop=True)
                kb.stt(STf[j][:, :], STf[j][:, :], Wc[:, c:c + 1], pD[:, 2, :], ALU.mult, ALU.add)
                kb.copy(STb[j][:, :], STf[j][:, :], eng='act')
                yield
            kb.copy(f['y'][:, :], pS[:, :], eng='act')
            kb.mm(pS[:, :], C['bmean_f'][:, :], f['y'][:, :])
            kb.tt(f['yc'][:, :], f['y'][:, :], pS[:, :], ALU.subtract)
            kb.tt(f['tmp'][:, :], f['yc'][:, :], f['yc'][:, :], ALU.mult)
            yield
            kb.mm(pS[:, :], C['bmean_f'][:, :], f['tmp'][:, :])
            kb.ts(f['tmp'][:, :], pS[:, :], GN_EPS, ALU.add)
            kb.act(f['tmp'][:, :], f['tmp'][:, :], AF.Sqrt)
            kb.op('dve', lambda E, o, i: E.reciprocal(out=o, in_=i), [f['tmp'][:, :]], [f['tmp'][:, :]])
            kb.tt(f['yc'][:, :], f['yc'][:, :], f['tmp'][:, :], ALU.mult)
            kb.ts(f['yc'][:, :], f['yc'][:, :], vc(V_LG, j), ALU.mult, vc(V_LB, j), ALU.add)
            yield
            kb.tt(f['tmp'][:, :], f['r'][:, :], f['k2'][:, :], ALU.mult)
            kb.ts(b16['rk'][:, :], f['tmp'][:, :], vc(V_RK, j), ALU.mult)
            kb.mm(pS[:, :], C['bones_b'][:, :], b16['rk'][:, :])
            kb.tt(f['bon'][:, :], pS[:, :], f['v'][:, :], ALU.mult)
            kb.tt(f['yc'][:, :], f['yc'][:, :], f['bon'][:, :], ALU.add)
            kb.tt(yb[:, j, :], f['yc'][:, :], f['g'][:, :], ALU.mult)
        NST = 38

        for ti in range(S // TT):
            t0 = ti * TT
            for k_, off in (('r', O_RWKV), ('k', O_RWKV + 1024), ('v', O_RWKV + 2048)):
                if ti == 0:
                    kb.memset(zin[k_][:, :, 0:1], 0.0)
                    kb.dma('sp', zin[k_][:, :, 1:W], rows(off, 0, TT))
                else:
                    kb.dma('sp', zin[k_][:, :, :], rows(off, t0 - 1, t0 + TT))
            o2 = O_RWKV + 3072
            for tl, a_, n_ in ((zwa, o2, 128), (zg1, o2 + 128, 128), (zg2, o2 + 256, 32)):
                if ti == 0:
                    kb.memset(tl[0:n_, 0:1], 0.0)
                    kb.dma('sp', tl[0:n_, 1:W], zT[a_:a_ + n_, 0:TT])
                else:
                    kb.dma('sp', tl[0:n_, :], zT[a_:a_ + n_, t0 - 1:t0 + TT])
            shift(ft['wa'][:, :], zwa[:, :], vc(V_MUWA), ft['d'])
            shift(ft['g1'][:, :], zg1[:, :], vc(V_MUG), ft['d'])
            shift(ft['g2'][0:32, :], zg2[0:32, :], vec[0:32, V_MUG + 1:V_MUG + 2], ft['d'])
            kb.act(bt16['wa'][0:64, :], ft['wa'][0:64, :], AF.Tanh)
            kb.copy(bt16['wa'][64:128, :], ft['wa'][64:128, :])
            kb.act(bt16['g1'][:, :], ft['g1'][:, :], AF.Sigmoid)
            kb.act(bg2[:, :], ft['g2'][0:32, :], AF.Sigmoid)
            if l == 1:
                kb.dma('sp', uv1[:, :], zT[O_UV1:O_UV1 + 32, t0:t0 + TT])
            interleave(range(8), lambda j, slot, ti=ti: body(ti, j, sets[slot]), KR, NST)
            branch_out(net, T, pbw, [yb[:, kc, :] for kc in range(8)], O_GATE + 1024, net.dr['GB'], t0, TT)
def host_consts():
    cf = {}
    cf['ident_f'] = np.eye(128, dtype=np.float32)
    p = np.arange(128)
    inv_freq = 500000.0 ** (-np.arange(0, 16, 2, dtype=np.float32) / 16)
    invf = np.where((p % 64) < 16, inv_freq[(p % 64) % 8] / (2 * np.pi), 0.0).astype(np.float32)
    cf['invf'] = invf[:, None]
    qi = np.arange(128)[:, None]; kj = np.arange(256)[None, :]
    cf['amask'] = np.where((kj >= qi) & (kj <= qi + 128), 0.0, -1e30).astype(np.float32)
    blk = (p[:, None] // 64) == (p[None, :] // 64)
    cf['bmean_f'] = (blk / 64.0).astype(np.float32)
    cm = np.ones((128, 256), np.float32); cm[:, ::64] = 0.0
    cf['cmask'] = cm
    s_ = (p % 64)[:, None]
    mab = np.zeros((128, 192), np.float32)
    mab[:, :128] = blk & (s_ < (p % 64)[None, :])
    mab[:, 128:] = (s_ <= np.arange(64)[None, :])
    cf['m_ab'] = mab
    cf['m_l'] = (blk & (s_ > (p % 64)[None, :])).astype(np.float32)
    cb = {}
    cb['ident_b'] = np.eye(128, dtype=np.float32)
    PT = np.zeros((128, 128), np.float32)
    for h in range(2):
        for i in range(8):
            PT[h * 64 + i + 8, h * 64 + i] = -1.0
            PT[h * 64 + i, h * 64 + i + 8] = 1.0
    cb['PT'] = PT
    cb['ones_b'] = np.ones((128, 128), np.float32)
    cb['bones_b'] = blk.astype(np.float32)
    return cf, cb


CF_KEYS = ['ident_f', 'invf', 'amask', 'bmean_f', 'cmask', 'm_ab', 'm_l']
CB_KEYS = ['ident_b', 'PT', 'ones_b', 'bones_b']


def host_vecs(I):
    out = np.zeros((L, 128, NV), np.float32)

    def pc(v):
        return np.ascontiguousarray(v.reshape(-1, 128).T)
    for l in range(L):
        o = out[l]
        for j in range(3):
            o[:, V_CONV + j * 8:V_CONV + j * 8 + 8] = pc(I['conv_w'][l, j])
        mu = I['rwkv_mu'][l]
        o[:, V_MUR:V_MUR + 8] = pc(mu[0:1024]); o[:, V_MUK:V_MUK + 8] = pc(mu[1024:2048]); o[:, V_MUV:V_MUV + 8] = pc(mu[2048:3072])
        o[:, V_MUWA] = mu[3072:3200]
        o[:, V_MUG] = mu[3200:3328]
        o[0:32, V_MUG + 1] = mu[3328:3360]
        o[:, V_W0:V_W0 + 8] = pc(I['rwkv_w0'][l]); o[:, V_A0:V_A0 + 8] = pc(I['rwkv_a0'][l])
        if l >= 1:
            o[:, V_V0:V_V0 + 8] = pc(I['rwkv_v0'][l - 1])
        o[:, V_KK:V_KK + 8] = pc(I['rwkv_k_k'][l]); o[:, V_KA:V_KA + 8] = pc(I['rwkv_k_a'][l])
        o[:, V_RK:V_RK + 8] = pc(I['rwkv_r_k'][l].reshape(-1))
        o[:, V_LG:V_LG + 8] = pc(I['rwkv_lnx_g'][l]); o[:, V_LB:V_LB + 8] = pc(I['rwkv_lnx_b'][l])
    return out


IN_SHAPES = {'x': [S, D], 'positions': [S], 'ln_in_g': [D], 'ln_in_b': [D], 'w_in': [L, D, NIN], 'rwkv_w2': [L, 64, D], 'rwkv_a2': [L, 64, D],
             'rwkv_g2': [L, 160, D], 'rwkv_v1': [1, D, 32], 'rwkv_v2': [1, 32, D], 'p_a': [L, D, D], 'p_b': [L, D, D], 'p_c': [L, 512, D],
             'w_o': [L, D, D], 'ln1_g': [L, D], 'ln1_b': [L, D], 'w_router': [128, 8, 128], 'router_bias': [128, 16], 'w_gate': [L, 16, D, 512],
             'w_up': [L, 16, D, 512], 'w_down': [L, 16, 512, D], 'ln2_g': [L, D], 'ln2_b': [L, D], 'vecs': [L, 128, NV]}


def build(debug=(), stop_after=None, skip=()):
    net = Net(debug=debug)
    kb = net.kb
    for k_, sh in IN_SHAPES.items():
        net.din(k_, sh, I32 if k_ == 'positions' else F32)
    cfh, cbh = host_consts()
    for k_ in CF_KEYS:
        net.din('c_' + k_, list(cfh[k_].shape), F32)
    for k_ in CB_KEYS:
        net.din('c_' + k_, list(cbh[k_].shape), BF16)
    out = net.nc.dram_tensor('out', [S, D], F32, kind="ExternalOutput")
    out = Buf(kb, out.ap(), dram=True)
    net.dscr('hA', [S, D], F32); net.dscr('hB', [S, D], F32); net.dscr('zT', [NZ, S], BF16)
    for k_ in ('GA', 'GB', 'GC', 'VF'):
        net.dscr(k_, [D, S], BF16)
    net.dscr('YC', [512, S], BF16)
    with Phase(kb) as g:
        C = {}
        for k_ in CF_KEYS:
            C[k_] = g.sb(net.n('k' + k_), list(cfh[k_].shape), F32)
            kb.dma('sp', C[k_][:, :], net.inp['c_' + k_][:, :])
        for k_ in CB_KEYS:
            C[k_] = g.sb(net.n('k' + k_), list(cbh[k_].shape), BF16)
            kb.dma('sp', C[k_][:, :], net.inp['c_' + k_][:, :])
        vec = []
        for l in range(L):
            v_ = g.sb(net.n('vec'), [128, NV], F32)
            kb.dma('sp', v_[:, :], net.inp['vecs'][l])
            kb.ts(v_[:, V_OMKA:V_OMKA + 8], v_[:, V_KA:V_KA + 8], -1.0, ALU.mult, 1.0, ALU.add)
            vec.append(v_)
        CW = g.sb(net.n('CW'), [128, 32, 16], F32)

        def mk_hT(ph):
            return [ph.sb(net.n('hT'), [128, 8, 1024], BF16) for _ in range(4)]
        with Phase(kb) as A:
            hT = mk_hT(A)
            phase0(net, hT, C)
            phase1(net, 0, hT)
        for l in range(L):
            if stop_after == ('p1', l):
                return net
            phase_conv(net, l, vec[l])
            if stop_after == ('conv', l):
                return net
            if 'rwkv' not in skip:
                phase_rwkv(net, l, vec[l], C)
            if stop_after == ('rwkv', l):
                return net
            if 'attn' not in skip:
                phase_attn(net, l, C)
            if stop_after == ('attn', l):
                return net
            with Phase(kb) as B:
                hT = mk_hT(B)
                phase_mix(net, l, vec[l], hT, C, net.dr['hA'], net.dr['hB'], CW)
                if stop_after == ('mix', l):
                    return net
                phase_moe(net, l, hT, C, CW, net.dr['hB'], net.dr['hA'], l == L - 1, out)
                if stop_after == ('moe', l):
                    return net
                if l < L - 1:
                    phase1(net, l + 1, hT)
    return net


def make_inputs(I, b):
    cfh, cbh = host_consts()
    m = {}
    for k_ in IN_SHAPES:
        if k_ == 'vecs':
            continue
        a = I[k_]
        if k_ in ('x', 'positions'):
            a = a[b]
        if k_ == 'w_router':
            a = np.concatenate([a.reshape(8, 128, 16).transpose(1, 0, 2), np.zeros((128, 8, 112), np.float32)], axis=2)
        if k_ == 'router_bias':
            a = np.broadcast_to(a[None, :], (128, 16))
        m[k_] = np.ascontiguousarray(a)
    m['vecs'] = host_vecs(I)
    for k_ in CF_KEYS:
        m['c_' + k_] = cfh[k_]
    for k_ in CB_KEYS:
        m['c_' + k_] = cbh[k_].astype(ml_dtypes.bfloat16)
    return m


def kernel(**inputs):
    I = {k_: np.asarray(v_) for k_, v_ in inputs.items()}
    net = build()
    in_maps = [make_inputs(I, b) for b in range(8)]
    res = run_bass_kernel_spmd(net.nc, in_maps, core_ids=list(range(8)))
    return np.stack([np.asarray(r["out"], dtype=np.float32) for r in res.results], axis=0)
```

```python
import ml_dtypes
import numpy as np
from contextlib import ExitStack
import concourse.bass as bass
import concourse.mybir as mybir
from concourse.bass_utils import run_bass_kernel_spmd

F32 = mybir.dt.float32
BF16 = mybir.dt.bfloat16
I32 = mybir.dt.int32
AF = mybir.ActivationFunctionType
ALU = mybir.AluOpType
AX = mybir.AxisListType


class Sem:
    def __init__(self, h, is_dma):
        self.h = h
        self.is_dma = is_dma
        self.total = 0


class View:
    def __init__(self, buf, ap):
        self.buf = buf
        self.ap = ap


class Buf:
    def __init__(self, kb, t, dram=False):
        self.kb = kb
        self.t = t
        self.dram = dram
        self.w = {}
        self.r = {}
        self.ds = {}

    def __getitem__(self, idx):
        return View(self, self.t[idx])

    def v(self, ap):
        return View(self, ap)

    def dsem(self, q):
        if q not in self.ds:
            self.ds[q] = self.kb.new_dma_sem(q)
        return self.ds[q]


class KB:
    def __init__(self, nc):
        self.nc = nc
        self.engs = {'pe': nc.tensor, 'act': nc.scalar, 'dve': nc.vector, 'pool': nc.gpsimd, 'sp': nc.sync}
        self.esem = {}
        for k in ['pe', 'act', 'dve', 'pool']:
            self.esem[k] = Sem(nc.alloc_semaphore('es_' + k), False)
        self.seen = {k: {} for k in self.engs}
        self.allsems = list(self.esem.values())
        self.nds = 0
        self.ninst = 0
        self.free_ds = {'sp': [], 'pool': [], 'act': []}

    def new_dma_sem(self, q):
        if self.free_ds[q]:
            return self.free_ds[q].pop()
        s = Sem(self.nc.alloc_semaphore('ds%d' % self.nds), True)
        self.nds += 1
        self.allsems.append(s)
        return s

    def _wait(self, eng, need):
        E = self.engs[eng]
        seen = self.seen[eng]
        for s, c in need.items():
            if s.is_dma:
                c = s.total
            elif eng == 'pe' and s is self.esem['pe']:
                continue
            if seen.get(s, 0) < c:
                E.wait_ge(s.h, c)
                seen[s] = c
                self.ninst += 1

    def _deps(self, reads, writes):
        need = {}

        def add(d):
            for s, c in d.items():
                if need.get(s, 0) < c:
                    need[s] = c
        for b in reads:
            add(b.w)
        for b in writes:
            add(b.w)
            add(b.r)
        return need

    def _stamp(self, reads, writes, s, c):
        for b in reads:
            if b.r.get(s, 0) < c:
                b.r[s] = c
        for b in writes:
            if b.dram:
                if b.w.get(s, 0) < c:
                    b.w[s] = c
            else:
                b.w = {s: c}
                b.r = {}

    def op(self, eng, fn, outs, ins):
        reads = [v.buf for v in ins]
        writes = [v.buf for v in outs]
        self._wait(eng, self._deps(reads, writes))
        ins_ = fn(self.engs[eng], *[v.ap for v in outs], *[v.ap for v in ins])
        s = self.esem[eng]
        s.total += 1
        ins_.then_inc(s.h, 1)
        self.ninst += 1
        self._stamp(reads, writes, s, s.total)
        return ins_

    def dma(self, q, out, in_, **kw):
        reads = [in_.buf]
        writes = [out.buf]
        self._wait(q, self._deps(reads, writes))
        sb = out.buf if not out.buf.dram else in_.buf
        s = sb.dsem(q)
        ins_ = self.engs[q].dma_start(out=out.ap, in_=in_.ap, **kw)
        s.total += 16
        ins_.then_inc(s.h, 16)
        self.ninst += 1
        self._stamp(reads, writes, s, s.total)
        return ins_

    def barrier(self):
        need = {s: s.total for s in self.allsems if s.total > 0}
        for eng in self.engs:
            self._wait(eng, need)

    def mm(self, out, lhsT, rhs, start=True, stop=True):
        return self.op('pe', lambda E, o, a, b: E.matmul(o, lhsT=a, rhs=b, start=start, stop=stop), [out], [lhsT, rhs])

    def transpose(self, out, in_, ident):
        return self.op('pe', lambda E, o, a, b: E.transpose(o, a, b), [out], [in_, ident])

    def act(self, out, in_, func, bias=None, scale=None, accum=None, eng='act'):
        outs = [out] + ([accum] if accum is not None else [])
        ins = [in_]
        kw = {}
        if isinstance(bias, View):
            ins.append(bias)
        if isinstance(scale, View):
            ins.append(scale)

        def fn(E, *aps):
            aps = list(aps)
            o = aps.pop(0)
            if accum is not None:
                kw['accum_out'] = aps.pop(0)
            i = aps.pop(0)
            if isinstance(bias, View):
                kw['bias'] = aps.pop(0)
            elif bias is not None:
                kw['bias'] = bias
            if isinstance(scale, View):
                kw['scale'] = aps.pop(0)
            elif scale is not None:
                kw['scale'] = scale
            return E.activation(out=o, in_=i, func=func, **kw)
        return self.op(eng, fn, outs, ins)

    def tt(self, out, a, b, op, eng='dve'):
        return self.op(eng, lambda E, o, x, y: E.tensor_tensor(out=o, in0=x, in1=y, op=op), [out], [a, b])

    def ts(self, out, a, s1, op0, s2=None, op1=None, eng='dve', accum=None):
        ins = [a]
        outs = [out] + ([accum] if accum is not None else [])
        if isinstance(s1, View):
            ins.append(s1)
        if isinstance(s2, View):
            ins.append(s2)

        def fn(E, *aps):
            aps = list(aps)
            o = aps.pop(0)
            kw = {}
            if accum is not None:
                kw['accum_out'] = aps.pop(0)
            x = aps.pop(0)
            a1 = aps.pop(0) if isinstance(s1, View) else s1
            a2 = aps.pop(0) if isinstance(s2, View) else s2
            if op1 is None:
                return E.tensor_scalar(out=o, in0=x, scalar1=a1, scalar2=None, op0=op0, **kw)
            return E.tensor_scalar(out=o, in0=x, scalar1=a1, scalar2=a2, op0=op0, op1=op1, **kw)
        return self.op(eng, fn, outs, ins)

    def stt(self, out, a, s, b, op0, op1, eng='dve'):
        ins = [a, b]
        if isinstance(s, View):
            ins.append(s)

        def fn(E, o, x, y, *rest):
            sc = rest[0] if rest else s
            return E.scalar_tensor_tensor(out=o, in0=x, scalar=sc, in1=y, op0=op0, op1=op1)
        return self.op(eng, fn, [out], ins)

    def copy(self, out, in_, eng='dve'):
        if eng == 'act':
            return self.op('act', lambda E, o, i: E.copy(out=o, in_=i), [out], [in_])
        return self.op(eng, lambda E, o, i: E.tensor_copy(out=o, in_=i), [out], [in_])

    def memset(self, out, val, eng='pool'):
        return self.op(eng, lambda E, o: E.memset(o, val), [out], [])

    def reduce(self, out, in_, op, axis=AX.X, eng='dve'):
        return self.op(eng, lambda E, o, i: E.tensor_reduce(out=o, in_=i, axis=axis, op=op), [out], [in_])

    def scan(self, out, d0, d1, initial, op0, op1):
        return self.op('dve', lambda E, o, x, y: E.tensor_tensor_scan(out=o, data0=x, data1=y, initial=initial, op0=op0, op1=op1), [out], [d0, d1])


class Phase:
    def __init__(self, kb):
        self.kb = kb
        self.st = ExitStack()
        self.bufs = []

    def __enter__(self):
        self.st.__enter__()
        return self

    def __exit__(self, *a):
        self.kb.barrier()
        for b in self.bufs:
            for q_, s_ in b.ds.items():
                self.kb.free_ds[q_].append(s_)
            b.ds = {}
        return self.st.__exit__(*a)

    def sb(self, name, shape, dt):
        t = self.st.enter_context(self.kb.nc.sbuf_tensor(name, list(shape), dt))
        b = Buf(self.kb, t)
        self.bufs.append(b)
        return b

    def ps(self, name, shape, dt=F32):
        t = self.st.enter_context(self.kb.nc.psum_tensor(name, list(shape), dt))
        return Buf(self.kb, t)

    def bank(self, name, dt=F32):
        n = 512 if dt == F32 else 1024
        return self.st.enter_context(self.kb.nc.psum_tensor(name, [128, n], dt))

    def sub(self, ap):
        return Buf(self.kb, ap)


class Sub:
    def __init__(self, buf, ap):
        self.buf = buf
        self.ap = ap

    def __getitem__(self, idx):
        return View(self.buf, self.ap[idx])


def interleave(items, make_gen, K, nstage):
    it = iter(items)
    slots = [None] * K
    start_at = [(k * nstage) // K for k in range(K)]
    rnd = 0
    exhausted = False
    while True:
        alive = False
        for k in range(K):
            if slots[k] is None and not exhausted and rnd >= start_at[k]:
                try:
                    slots[k] = make_gen(next(it), k)
                except StopIteration:
                    exhausted = True
            if slots[k] is not None:
                alive = True
                try:
                    next(slots[k])
                except StopIteration:
                    slots[k] = None
        if not alive and exhausted:
            break
        rnd += 1

S = 4096; D = 1024; NIN = 14112; L = 2
ALPHA = (2 * L) ** 0.25
LN_EPS = 1e-5
O_GATE = 0; O_CONV = 3072; O_RWKV = 6144; O_ATTN = 9504
O_UV1 = NIN
NZ = NIN + 32


def cdiv(a, b):
    return (a + b - 1) // b


class Net:
    def __init__(self, debug=()):
        nc = self.nc = bass.Bass("TRN2", target_bir_lowering=False)
        self.kb = KB(nc)
        self.debug = debug
        self.inp = {}
        self.dr = {}
        self.uid = 0

    def din(self, name, shape, dt=F32):
        t = self.nc.dram_tensor(name, list(shape), dt, kind="ExternalInput")
        b = Buf(self.kb, t.ap(), dram=True)
        self.inp[name] = b
        return b

    def dscr(self, name, shape, dt):
        kind = "ExternalOutput" if name in self.debug else "Internal"
        t = self.nc.dram_tensor(name, list(shape), dt, kind=kind)
        b = Buf(self.kb, t.ap(), dram=True)
        self.dr[name] = b
        return b

    def n(self, s):
        self.uid += 1
        return "%s_%d" % (s, self.uid)


def layernorm_tile(net, ph, T, xin, gB, bB, out):
    kb = net.kb
    st = T['st']; mv = T['mv']; sd = T['sd']
    for c in range(2):
        kb.op('dve', lambda E, o, i: E.bn_stats(out=o, in_=i), [st[:, c, :]], [View(xin.buf, xin.ap[:, c * 512:(c + 1) * 512])])
    kb.op('dve', lambda E, o, i: E.bn_aggr(out=o, in_=i), [mv[:, :]], [st[:, :, :]])
    kb.ts(sd[:, 0:1], mv[:, 1:2], LN_EPS, ALU.add)
    kb.act(sd[:, 1:2], sd[:, 0:1], AF.Sqrt)
    kb.op('dve', lambda E, o, i: E.reciprocal(out=o, in_=i), [sd[:, 2:3]], [sd[:, 1:2]])
    kb.ts(out, xin, mv[:, 0:1], ALU.subtract, sd[:, 2:3], ALU.mult)
    kb.tt(out, out, gB, ALU.mult, eng='pool')
    kb.tt(out, out, bB, ALU.add, eng='pool')


def ln_scratch(net, ph):
    return {'st': ph.sb(net.n('lnst'), [128, 2, 6], F32), 'mv': ph.sb(net.n('lnmv'), [128, 2], F32),
            'sd': ph.sb(net.n('lnsd'), [128, 4], F32)}


def bcast_load(net, ph, name, src_ap):
    b = ph.sb(net.n(name), [128, 1024], F32)
    net.kb.dma('sp', b[:, :], View(net.cur_in, src_ap.partition_broadcast(128)))
    return b
def emit_h_tile(net, ph, T, hv, ti, hdst, hT, ident_f, router=None):
    kb = net.kb
    if hdst is not None:
        kb.dma('pool', hdst[ti * 128:(ti + 1) * 128, :], hv)
    if hT is None:
        return
    pT = T['pT']
    for kc in range(8):
        kb.transpose(pT[:, kc, :], View(hv.buf, hv.ap[:, kc * 128:(kc + 1) * 128]), ident_f[:, :])
    q, r = divmod(ti, 8)
    kb.copy(hT[q][:, :, r * 128:(r + 1) * 128], pT[:, :, :], eng='act')
    if router is not None:
        router(pT, ti)


def phase0(net, hT, C):
    kb = net.kb
    with Phase(kb) as ph:
        net.cur_in = net.inp['ln_in_g']
        gB = bcast_load(net, ph, 'gB', net.inp['ln_in_g'].t[:])
        net.cur_in = net.inp['ln_in_b']
        bB = bcast_load(net, ph, 'bB', net.inp['ln_in_b'].t[:])
        T = ln_scratch(net, ph)
        T['pT'] = ph.ps(net.n('pT'), [128, 8, 128], F32)
        xs = [ph.sb(net.n('x'), [128, 1024], F32) for _ in range(2)]
        hs = [ph.sb(net.n('h'), [128, 1024], F32) for _ in range(2)]
        x = net.inp['x']
        for ti in range(32):
            xt = xs[ti % 2]; ht = hs[ti % 2]
            kb.dma('sp', xt[:, :], x[ti * 128:(ti + 1) * 128, :])
            layernorm_tile(net, ph, T, xt[:, :], gB[:, :], bB[:, :], ht[:, :])
            emit_h_tile(net, ph, T, ht[:, :], ti, net.dr['hA'], hT, C['ident_f'])


def phase1(net, l, hT):
    kb = net.kb
    w_in = net.inp['w_in']
    zT = net.dr['zT']
    with Phase(kb) as ph:
        wst = [ph.sb(net.n('wst'), [128, 8, 128], F32) for _ in range(2)]
        wb = [ph.sb(net.n('wb'), [128, 8, 128], BF16) for _ in range(2)]
        zs = [ph.sb(net.n('zs'), [128, S], BF16) for _ in range(2)]
        pz = [ph.ps(net.n('pz'), [128, 512], F32) for _ in range(4)]
        blocks = [(c0, min(128, NIN - c0), None) for c0 in range(0, NIN, 128)]
        if l == 1:
            blocks.append((O_UV1, 32, 'v1'))
        wv = w_in.t[l].rearrange("(kc p) n -> p kc n", p=128)
        k = 0

        def prefetch(bi):
            c0, ncol, kind = blocks[bi]
            st = wst[bi % 2]; w = wb[bi % 2]
            if kind is None:
                kb.dma('sp', st[:, :, 0:ncol], View(w_in, wv[:, :, c0:c0 + ncol]))
            else:
                v1 = net.inp['rwkv_v1']
                kb.dma('sp', st[:, :, 0:ncol], View(v1, v1.t[0].rearrange("(kc p) n -> p kc n", p=128)))
            kb.copy(w[:, :, 0:ncol], st[:, :, 0:ncol], eng='pool')
        prefetch(0)
        for bi, (c0, ncol, kind) in enumerate(blocks):
            w = wb[bi % 2]; z = zs[bi % 2]
            if bi + 1 < len(blocks):
                prefetch(bi + 1)
            for tt in range(8):
                p = pz[k % 4]
                for kc in range(8):
                    kb.mm(p[0:ncol, :], w[:, kc, 0:ncol], hT[tt // 2][:, kc, (tt % 2) * 512:(tt % 2 + 1) * 512], start=(kc == 0), stop=(kc == 7))
                kb.copy(z[0:ncol, tt * 512:(tt + 1) * 512], p[0:ncol, :], eng=('act' if k % 2 == 0 else 'dve'))
                k += 1
            kb.dma('pool' if bi % 2 else 'sp', zT[c0:c0 + ncol, :], z[0:ncol, :])
NV = 123
V_CONV = 0; V_MUR = 24; V_MUK = 32; V_MUV = 40; V_MUWA = 48; V_MUG = 49; V_W0 = 51; V_A0 = 59; V_V0 = 67
V_KK = 75; V_KA = 83; V_RK = 91; V_LG = 99; V_LB = 107; V_OMKA = 115


def load_w_bf16(net, ph, src, ap, kc, ncol, name):
    kb = net.kb
    w = ph.sb(net.n(name), [128, kc, ncol], BF16)
    v = ap.rearrange("(kc p) n -> p kc n", p=128)
    with Phase(kb) as p2:
        st = [p2.sb(net.n('wstg'), [128, kc, 256], F32) for _ in range(2)]
        for i, c0 in enumerate(range(0, ncol, 256)):
            n = min(256, ncol - c0)
            kb.dma('sp', st[i % 2][:, :, 0:n], View(src, v[:, :, c0:c0 + n]))
            kb.copy(w[:, :, c0:c0 + n], st[i % 2][:, :, 0:n], eng='pool')
    return w


def branch_out(net, T, pw, rhs_list, goff, Gdst, t0, N, kp=128):
    kb = net.kb
    zT = net.dr['zT']
    gt = T['gt']; sg = T['sg']; go = T['go']; pp = T['pp']
    kb.dma('sp', gt[:, :, 0:N], View(zT, zT.t[goff:goff + 1024, t0:t0 + N].rearrange("(m p) t -> p m t", p=128)))
    for m in range(8):
        p = pp[m % len(pp)]
        for kc, r in enumerate(rhs_list):
            kb.mm(p[:, 0:N], pw[0:kp, kc, m * 128:(m + 1) * 128], r, start=(kc == 0), stop=(kc == len(rhs_list) - 1))
        kb.act(sg[:, 0:N], gt[:, m, 0:N], AF.Sigmoid)
        kb.tt(go[:, m, 0:N], p[:, 0:N], sg[:, 0:N], ALU.mult)
    kb.dma('pool', View(Gdst, Gdst.t[:, t0:t0 + N].rearrange("(m p) t -> p m t", p=128)), go[:, :, 0:N])


def bo_scratch(net, ph, N, npp=2):
    return {'gt': ph.sb(net.n('gt'), [128, 8, N], BF16), 'sg': ph.sb(net.n('sg'), [128, N], F32),
            'go': ph.sb(net.n('go'), [128, 8, N], BF16), 'pp': [ph.ps(net.n('pp'), [128, 512], F32) for _ in range(npp)]}


def phase_conv(net, l, vec):
    kb = net.kb
    zT = net.dr['zT']
    with Phase(kb) as ph:
        pa = load_w_bf16(net, ph, net.inp['p_a'], net.inp['p_a'].t[l], 8, 1024, 'pa')
        T = bo_scratch(net, ph, 512)
        cc = ph.sb(net.n('cc'), [128, 8, 514], BF16)
        chh = ph.sb(net.n('chh'), [128, 8, 514], BF16)
        cb = ph.sb(net.n('cb'), [128, 8, 512], BF16)
        yf = [ph.sb(net.n('yf'), [128, 514], F32) for _ in range(2)]
        of = [ph.sb(net.n('of'), [128, 512], F32) for _ in range(2)]
        u = ph.sb(net.n('u'), [128, 8, 512], BF16)

        def rows(off, a, b):
            return View(zT, zT.t[off:off + 1024, a:b].rearrange("(m p) t -> p m t", p=128))
        for tt in range(8):
            t0 = tt * 512
            if tt == 0:
                kb.memset(cc[:, :, 0:2], 0.0)
                kb.memset(chh[:, :, 0:2], 0.0)
                kb.dma('sp', cc[:, :, 2:514], rows(O_CONV + 1024, 0, 512))
                kb.dma('sp', chh[:, :, 2:514], rows(O_CONV + 2048, 0, 512))
            else:
                kb.dma('sp', cc[:, :, :], rows(O_CONV + 1024, t0 - 2, t0 + 512))
                kb.dma('sp', chh[:, :, :], rows(O_CONV + 2048, t0 - 2, t0 + 512))
            kb.dma('sp', cb[:, :, :], rows(O_CONV, t0, t0 + 512))
            for c in range(8):
                y = yf[c % 2]; o = of[c % 2]
                kb.tt(y[:, :], cc[:, c, :], chh[:, c, :], ALU.mult, eng='pool')
                kb.ts(o[:, :], y[:, 2:514], vec[:, V_CONV + 16 + c:V_CONV + 17 + c], ALU.mult)
                kb.stt(o[:, :], y[:, 1:513], vec[:, V_CONV + 8 + c:V_CONV + 9 + c], o[:, :], ALU.mult, ALU.add)
                kb.stt(o[:, :], y[:, 0:512], vec[:, V_CONV + c:V_CONV + 1 + c], o[:, :], ALU.mult, ALU.add)
                kb.tt(u[:, c, :], o[:, :], cb[:, c, :], ALU.mult, eng='pool')
            branch_out(net, T, pa, [u[:, c, :] for c in range(8)], O_GATE, net.dr['GA'], t0, 512)


def phase_mix(net, l, vec, hT, C, hsrc, hdst, CW):
    kb = net.kb
    with Phase(kb) as ph:
        wo = load_w_bf16(net, ph, net.inp['w_o'], net.inp['w_o'].t[l], 8, 1024, 'wo')
        net.cur_in = net.inp['ln1_g']; gB = bcast_load(net, ph, 'g1B', net.inp['ln1_g'].t[l])
        net.cur_in = net.inp['ln1_b']; bB = bcast_load(net, ph, 'b1B', net.inp['ln1_b'].t[l])
        T = ln_scratch(net, ph)
        T['pT'] = ph.ps(net.n('pT'), [128, 8, 128], F32)
        pm = [ph.ps(net.n('pm'), [128, 512], F32) for _ in range(2)]
        pr = ph.ps(net.n('pr'), [128, 128], F32)
        G = [ph.sb(net.n('G'), [128, 8, 512], BF16) for _ in range(3)]
        hres = [ph.sb(net.n('hres'), [128, 1024], F32) for _ in range(2)]
        pre = [ph.sb(net.n('pre'), [128, 1024], F32) for _ in range(2)]
        h1 = [ph.sb(net.n('h1'), [128, 1024], F32) for _ in range(2)]
        hTf = ph.sb(net.n('hTf'), [128, 8, 128], F32)
        wr = ph.sb(net.n('wr'), [128, 8, 128], F32)
        if True:
          kb.dma('sp', wr[:, :, :], net.inp['w_router'][:, :, :])
        whi = ph.sb(net.n('whi'), [128, 8, 128], BF16)
        wlo = ph.sb(net.n('wlo'), [128, 8, 128], BF16)
        hlo = ph.sb(net.n('hlo'), [128, 8, 128], BF16)
        if True:
            kb.copy(whi[:, :, :], wr[:, :, :])
            kb.tt(wr[:, :, :], wr[:, :, :], whi[:, :, :], ALU.subtract)
            kb.copy(wlo[:, :, :], wr[:, :, :])
        rb = ph.sb(net.n('rb'), [128, 16], F32)
        if True:
          kb.dma('sp', rb[:, :], net.inp['router_bias'][:, :])
        R = {k: ph.sb(net.n('r' + k), [128, 16], F32) for k in ['lg', 'e', 'pr', 'sel', 'selm', 'oh1', 'oh2', 'gw']}
        r1 = ph.sb(net.n('r1'), [128, 16], F32)
        ps6 = ph.sb(net.n('ps6'), [128, 4, 6], F32)
        gs = ph.sb(net.n('gs'), [128, 4], F32)
        eq = ph.sb(net.n('eq'), [128, 4], F32)
        pen = ph.sb(net.n('pen'), [128, 4], F32)
        Gsrc = [net.dr['GA'], net.dr['GB'], net.dr['GC']]

        def router(pT, ti):
            kb.copy(hTf[:, :, :], pT[:, :, :], eng='act')
            q_, r_ = divmod(ti, 8)
            hi_v = hT[q_][:, :, r_ * 128:(r_ + 1) * 128]
            kb.tt(hlo[:, :, :], hTf[:, :, :], hi_v, ALU.subtract)
            n_ = 0
            for kc in range(8):
                for a_, b_ in ((hT[q_][:, kc, r_ * 128:(r_ + 1) * 128], whi[:, kc, :]), (hT[q_][:, kc, r_ * 128:(r_ + 1) * 128], wlo[:, kc, :]), (hlo[:, kc, :], whi[:, kc, :])):
                    kb.mm(pr[:, :], a_, b_, start=(n_ == 0), stop=(n_ == 23))
                    n_ += 1
            lg = R['lg']
            kb.copy(lg[:, :], pr[:, 0:16])
            kb.reduce(r1[:, 0:1], lg[:, :], ALU.max)
            kb.ts(r1[:, 1:2], r1[:, 0:1], -1.0, ALU.mult)
            kb.act(R['e'][:, :], lg[:, :], AF.Exp, bias=r1[:, 1:2], accum=r1[:, 2:3])
            kb.op('dve', lambda E, o, i: E.reciprocal(out=o, in_=i), [r1[:, 3:4]], [r1[:, 2:3]])
            kb.ts(R['pr'][:, :], R['e'][:, :], r1[:, 3:4], ALU.mult)
            kb.tt(R['sel'][:, :], R['pr'][:, :], rb[:, :], ALU.add)
            s3 = R['sel'].t[:, :].rearrange("p (g e) -> p g e", e=4)
            k = 0
            for i in range(4):
                for j in range(i + 1, 4):
                    kb.tt(ps6[:, :, k], R['sel'].v(s3[:, :, i]), R['sel'].v(s3[:, :, j]), ALU.add)
                    k += 1
            kb.reduce(gs[:, :], ps6[:, :, :], ALU.max)
            kb.reduce(r1[:, 4:5], gs[:, :], ALU.max)
            kb.ts(eq[:, :], gs[:, :], r1[:, 4:5], ALU.is_ge)
            kb.ts(pen[:, :], eq[:, :], -1.0, ALU.add, 1e30, ALU.mult)
            m3 = R['selm'].t[:, :].rearrange("p (g e) -> p g e", e=4)
            kb.tt(R['selm'].v(m3), R['sel'].v(s3), eq.v(eq.t[:, :].unsqueeze(2).to_broadcast([128, 4, 4])), ALU.mult)
            kb.tt(R['selm'].v(m3), R['selm'].v(m3), pen.v(pen.t[:, :].unsqueeze(2).to_broadcast([128, 4, 4])), ALU.add)
            kb.reduce(r1[:, 5:6], R['selm'][:, :], ALU.max)
            kb.ts(R['oh1'][:, :], R['selm'][:, :], r1[:, 5:6], ALU.is_ge)
            kb.stt(R['selm'][:, :], R['oh1'][:, :], -1e30, R['selm'][:, :], ALU.mult, ALU.add)
            kb.reduce(r1[:, 6:7], R['selm'][:, :], ALU.max)
            kb.ts(R['oh2'][:, :], R['selm'][:, :], r1[:, 6:7], ALU.is_ge)
            kb.tt(R['oh1'][:, :], R['oh1'][:, :], R['oh2'][:, :], ALU.add)
            kb.tt(R['gw'][:, :], R['pr'][:, :], R['oh1'][:, :], ALU.mult)
            kb.reduce(r1[:, 7:8], R['gw'][:, :], ALU.add)
            kb.op('dve', lambda E, o, i: E.reciprocal(out=o, in_=i), [r1[:, 8:9]], [r1[:, 7:8]])
            kb.ts(CW[:, ti, :], R['gw'][:, :], r1[:, 8:9], ALU.mult)

        for tt in range(8):
            for b in range(3):
                kb.dma('sp', G[b][:, :, :], View(Gsrc[b], Gsrc[b].t[:, tt * 512:(tt + 1) * 512].rearrange("(m p) t -> p m t", p=128)))
            for s in range(4):
                ti = tt * 4 + s
                hr = hres[ti % 2]; pv = pre[ti % 2]; ho = h1[ti % 2]
                kb.dma('sp', hr[:, :], hsrc[ti * 128:(ti + 1) * 128, :])
                for dh in range(2):
                    p = pm[dh]
                    n = 0
                    for b in range(3):
                        for kc in range(8):
                            kb.mm(p[:, :], G[b][:, kc, s * 128:(s + 1) * 128], wo[:, kc, dh * 512:(dh + 1) * 512], start=(n == 0), stop=(n == 23))
                            n += 1
                    kb.stt(pv[:, dh * 512:(dh + 1) * 512], hr[:, dh * 512:(dh + 1) * 512], ALPHA, p[:, :], ALU.mult, ALU.add)
                layernorm_tile(net, ph, T, pv[:, :], gB[:, :], bB[:, :], ho[:, :])
                emit_h_tile(net, ph, T, ho[:, :], ti, hdst, hT, C['ident_f'], router=router)


def phase_moe(net, l, hT, C, CW, hsrc, hdst, last, out_dst):
    kb = net.kb
    wg_d = net.inp['w_gate']; wu_d = net.inp['w_up']; wd_d = net.inp['w_down']
    with Phase(kb) as ph:
        net.cur_in = net.inp['ln2_g']; gB = bcast_load(net, ph, 'g2B', net.inp['ln2_g'].t[l])
        net.cur_in = net.inp['ln2_b']; bB = bcast_load(net, ph, 'b2B', net.inp['ln2_b'].t[l])
        T = ln_scratch(net, ph)
        T['pT'] = ph.ps(net.n('pT'), [128, 8, 128], F32)
        pg = [ph.ps(net.n('pg'), [128, 512], F32) for _ in range(2)]
        pu = [ph.ps(net.n('pu'), [128, 512], F32) for _ in range(2)]
        pd = [ph.ps(net.n('pd'), [128, 512], F32) for _ in range(2)]
        acc = ph.sb(net.n('acc'), [128, 8, 1024], F32)
        wgb = [ph.sb(net.n('wgb'), [128, 8, 512], BF16) for _ in range(2)]
        wub = [ph.sb(net.n('wub'), [128, 8, 512], BF16) for _ in range(2)]
        wdb = [ph.sb(net.n('wdb'), [128, 4, 1024], BF16) for _ in range(2)]
        stg = [ph.sb(net.n('stg'), [128, 4, 512], F32) for _ in range(3)]
        hid = [ph.sb(net.n('hid'), [128, 4, 512], BF16) for _ in range(2)]
        sl = [ph.sb(net.n('sl'), [128, 512], F32) for _ in range(2)]
        hres = [ph.sb(net.n('hres'), [128, 1024], F32) for _ in range(1)]
        h2 = [ph.sb(net.n('h2'), [128, 1024], F32) for _ in range(1)]
        ns = 0
        hT_new = hT
        def load_expert(e):
            nonlocal ns
            wgt = wgb[e % 2]; wut = wub[e % 2]; wdt = wdb[e % 2]
            gv = wg_d.t[l, e].rearrange("(kc p) n -> p kc n", p=128)
            uv = wu_d.t[l, e].rearrange("(kc p) n -> p kc n", p=128)
            dv = wd_d.t[l, e].rearrange("(kc p) n -> p kc n", p=128)
            for hh in range(2):
                s_ = stg[ns % 3]; ns += 1
                kb.dma('sp', s_[:, :, :], View(wg_d, gv[:, hh * 4:(hh + 1) * 4, :]))
                kb.copy(wgt[:, hh * 4:(hh + 1) * 4, :], s_[:, :, :], eng='pool')
            for hh in range(2):
                s_ = stg[ns % 3]; ns += 1
                kb.dma('sp', s_[:, :, :], View(wu_d, uv[:, hh * 4:(hh + 1) * 4, :]))
                kb.copy(wut[:, hh * 4:(hh + 1) * 4, :], s_[:, :, :], eng='pool')
            pend = []
            for hh in range(2):
                s_ = stg[ns % 3]; ns += 1
                kb.dma('sp', s_[:, :, :], View(wd_d, dv[:, :, hh * 512:(hh + 1) * 512]))
                pend.append((wdt, hh, s_))
            return pend

        def cast_d(pend):
            for wdt, hh, s_ in pend:
                kb.copy(wdt[:, :, hh * 512:(hh + 1) * 512], s_[:, :, :], eng='act')
        seq = [(q, e) for q in range(4) for e in range(16)]
        cast_d(load_expert(0))
        for si, (q, e) in enumerate(seq):
            if True:
                wgt = wgb[e % 2]; wut = wub[e % 2]; wdt = wdb[e % 2]
                pend = load_expert(seq[si + 1][1]) if si + 1 < len(seq) else []
                for tt in range(2):
                    hd = hid[tt % 2]
                    rhs = lambda kc: hT[q][:, kc, tt * 512:(tt + 1) * 512]
                    for f in range(4):
                        g_ = pg[f % 2]; u_ = pu[f % 2]; s2 = sl[f % 2]
                        for kc in range(8):
                            kb.mm(g_[:, :], wgt[:, kc, f * 128:(f + 1) * 128], rhs(kc), start=(kc == 0), stop=(kc == 7))
                        for kc in range(8):
                            kb.mm(u_[:, :], wut[:, kc, f * 128:(f + 1) * 128], rhs(kc), start=(kc == 0), stop=(kc == 7))
                        kb.act(s2[:, :], g_[:, :], AF.Silu)
                        kb.tt(hd[:, f, :], u_[:, :], s2[:, :], ALU.mult)
                    if tt == 1:
                        cast_d(pend)
                    for s in range(4):
                        tl = tt * 4 + s
                        ti = q * 8 + tl
                        for dh in range(2):
                            p = pd[dh]
                            for f in range(4):
                                kb.mm(p[:, :], hd[:, f, s * 128:(s + 1) * 128], wdt[:, f, dh * 512:(dh + 1) * 512], start=(f == 0), stop=(f == 3))
                            a = acc[:, tl, dh * 512:(dh + 1) * 512]
                            if e == 0:
                                kb.ts(a, p[:, :], CW[:, ti, e:e + 1], ALU.mult)
                            else:
                                kb.stt(a, p[:, :], CW[:, ti, e:e + 1], a, ALU.mult, ALU.add)
            for tl in (range(8) if e == 15 else ()):
                ti = q * 8 + tl
                hr = hres[0]; pv = hres[0]; ho = h2[0]
                kb.dma('sp', hr[:, :], hsrc[ti * 128:(ti + 1) * 128, :])
                kb.stt(pv[:, :], hr[:, :], ALPHA, acc[:, tl, :], ALU.mult, ALU.add)
                layernorm_tile(net, ph, T, pv[:, :], gB[:, :], bB[:, :], ho[:, :])
                if last:
                    kb.dma('pool', out_dst[ti * 128:(ti + 1) * 128, :], ho[:, :])
                else:
                    emit_h_tile(net, ph, T, ho[:, :], ti, hdst, hT, C['ident_f'])
GROUPS = ((128, 1), (512, 4), (2048, 16))


def phase_attn(net, l, C):
    kb = net.kb
    zT = net.dr['zT']; YC = net.dr['YC']
    with Phase(kb) as ph:
        C2 = ph.sb(net.n('C2'), [128, S], BF16)
        S2 = ph.sb(net.n('S2'), [128, S], BF16)
        with Phase(kb) as p2:
            pi_ = p2.sb(net.n('posi'), [128, 512], I32)
            pf = p2.sb(net.n('posf'), [128, 512], F32)
            uf = p2.sb(net.n('uf'), [128, 512], F32)
            ui = p2.sb(net.n('ui'), [128, 512], I32)
            fr = p2.sb(net.n('fr'), [128, 512], F32)
            pos = net.inp['positions']
            for c in range(8):
                kb.dma('sp', pi_[:, :], View(pos, pos.t[c * 512:(c + 1) * 512].partition_broadcast(128)))
                kb.copy(pf[:, :], pi_[:, :])
                for tab, sh in ((S2, 0.0), (C2, 0.25)):
                    kb.ts(uf[:, :], pf[:, :], C['invf'][:, 0:1], ALU.mult, sh, ALU.add)
                    kb.copy(ui[:, :], uf[:, :])
                    kb.copy(fr[:, :], ui[:, :])
                    kb.tt(fr[:, :], uf[:, :], fr[:, :], ALU.subtract)
                    kb.act(tab[:, c * 512:(c + 1) * 512], fr[:, :], AF.Sin, scale=6.28318)
        q = ph.sb(net.n('q'), [128, S], BF16)
        k = ph.sb(net.n('k'), [128, S], BF16)
        v = ph.sb(net.n('v'), [128, S], BF16)
        qr = q; kr = k
        VT = ph.sb(net.n('VT'), [128, 32, 128], BF16)
        OG = [[ph.sb(net.n('OG'), [65, S], F32) for _ in range(2)] for _ in range(3)]
        t1 = ph.sb(net.n('t1'), [128, 512], F32)
        t2 = ph.sb(net.n('t2'), [128, 512], F32)
        KA = 4
        lrow = ph.sb(net.n('lrow'), [65, 6, 512], F32)
        wrow = ph.sb(net.n('wrow'), [65, 3, 512], BF16)
        ycs = ph.sb(net.n('ycs'), [64, 512], F32)
        ycb = ph.sb(net.n('ycb'), [64, 512], BF16)
        pf_ = [ph.ps(net.n('pf'), [128, 512], F32) for _ in range(KA)]
        ps_pt_ = [ph.ps(net.n('ps_pt'), [128, 2, 128], BF16) for _ in range(KA)]
        ps_s_ = [Sub(b, b.t[:, 0:256]) for b in pf_]
        ps_o_ = [Sub(b, b.t[:, 256:320]) for b in pf_]
        ps_t_ = [Sub(b, b.t[0:65, 320:448]) for b in pf_]
        ps_rot = pf_[0]
        ps_bc = Sub(pf_[1], pf_[1].t[0:64, :])
        ps_vt = Sub(ps_pt_[0], ps_pt_[0].t[:, 0, :])
        sm_ = [ph.sb(net.n('sm'), [128, 256], F32) for _ in range(KA)]
        pb_ = [ph.sb(net.n('pb'), [128, 256], BF16) for _ in range(KA)]
        PT_ = [ph.sb(net.n('PT'), [128, 2, 128], BF16) for _ in range(KA)]
        aug_ = [ph.sb(net.n('aug'), [128, 65], F32) for _ in range(KA)]
        r1_ = [ph.sb(net.n('ar1'), [128, 8], F32) for _ in range(KA)]
        for hp in range(4):
            for g, (window, d) in enumerate(GROUPS):
                base = O_ATTN + g * 512 + hp * 128
                kb.dma('sp', q[:, :], zT[base:base + 128, :])
                kb.dma('sp', k[:, :], zT[base + 1536:base + 1536 + 128, :])
                kb.dma('sp', v[:, :], zT[base + 3072:base + 3072 + 128, :])
                for src, dst in ((q, qr), (k, kr)):
                    for c in range(8):
                        sl_ = slice(c * 512, (c + 1) * 512)
                        kb.mm(ps_rot[:, :], C['PT'][:, :], src[:, sl_])
                        kb.tt(t1[:, :], ps_rot[:, :], S2[:, sl_], ALU.mult)
                        kb.tt(t2[:, :], src[:, sl_], C2[:, sl_], ALU.mult, eng='pool')
                        kb.tt(dst[:, sl_], t1[:, :], t2[:, :], ALU.add)
                Lr = S // d
                nb = Lr // 128

                def toks(r, n0, cnt):
                    a = r + d * n0 * 128
                    return slice(a, a + d * 128 * cnt - (d - 1), d)
                for r in range(d):
                    for n in range(nb):
                        kb.transpose(ps_vt[:, :], v[:, toks(r, n, 1)], C['ident_b'][:, :])
                        kb.copy(VT[:, r * nb + n, :], ps_vt[:, :], eng='act')
                def block_gen(item, slot, g=g, d=d, nb=nb, toks=toks):
                    hd, r, n = item
                    rows = slice(hd * 64, hd * 64 + 64)
                    og = OG[g][hd]
                    sm = sm_[slot]; pb = pb_[slot]; PT = PT_[slot]; aug = aug_[slot]; r1 = r1_[slot]
                    ps_s = ps_s_[slot]; ps_pt = ps_pt_[slot]; ps_o = ps_o_[slot]; ps_t = ps_t_[slot]
                    bi = r * nb + n
                    if n == 0:
                        kb.mm(ps_s[:, 128:256], qr[rows, toks(r, n, 1)], kr[rows, toks(r, n, 1)])
                        kb.stt(sm[:, 128:256], ps_s[:, 128:256], 0.125, C['amask'][:, 128:256], ALU.mult, ALU.add)
                        kb.memset(sm[:, 0:128], -1e30)
                    else:
                        kb.mm(ps_s[:, :], qr[rows, toks(r, n, 1)], kr[rows, toks(r, n - 1, 2)])
                        kb.stt(sm[:, :], ps_s[:, :], 0.125, C['amask'][:, :], ALU.mult, ALU.add)
                    yield
                    kb.reduce(r1[:, 0:1], sm[:, :], ALU.max)
                    kb.ts(r1[:, 1:2], r1[:, 0:1], -1.0, ALU.mult)
                    kb.act(pb[:, :], sm[:, :], AF.Exp, bias=r1[:, 1:2], accum=r1[:, 2:3])
                    yield
                    for j in range(2):
                        kb.transpose(ps_pt[:, j, :], pb[:, j * 128:(j + 1) * 128], C['ident_b'][:, :])
                    kb.copy(PT[:, :, :], ps_pt[:, :, :])
                    yield
                    if n == 0:
                        kb.mm(ps_o[:, :], PT[:, 1, :], VT[:, bi, rows])
                    else:
                        kb.mm(ps_o[:, :], PT[:, 0, :], VT[:, bi - 1, rows], start=True, stop=False)
                        kb.mm(ps_o[:, :], PT[:, 1, :], VT[:, bi, rows], start=False, stop=True)
                    kb.op('dve', lambda E, o, i: E.reciprocal(out=o, in_=i), [r1[:, 3:4]], [r1[:, 2:3]])
                    kb.ts(aug[:, 0:64], ps_o[:, :], r1[:, 3:4], ALU.mult)
                    kb.act(r1[:, 4:5], r1[:, 2:3], AF.Ln)
                    kb.tt(aug[:, 64:65], r1[:, 4:5], r1[:, 0:1], ALU.add)
                    yield
                    kb.transpose(ps_t[:, :], aug[:, :], C['ident_f'][:, :])
                    kb.copy(og[:, toks(r, n, 1)], ps_t[:, :], eng='act')
                interleave([(hd, r, n) for hd in range(2) for r in range(d) for n in range(nb)], block_gen, KA, 5)
            for hd in range(2):
                for c in range(8):
                    sl_ = slice(c * 512, (c + 1) * 512)
                    L0 = OG[0][hd][64:65, sl_]; L1 = OG[1][hd][64:65, sl_]; L2 = OG[2][hd][64:65, sl_]
                    m = lrow[64:65, 0, :]
                    kb.tt(m, L0, L1, ALU.max)
                    kb.tt(m, m, L2, ALU.max)
                    for g, Lg in enumerate((L0, L1, L2)):
                        kb.tt(lrow[64:65, 1 + g, :], Lg, m, ALU.subtract)
                        kb.act(lrow[64:65, 1 + g, :], lrow[64:65, 1 + g, :], AF.Exp)
                    den = lrow[64:65, 4, :]
                    kb.tt(den, lrow[64:65, 1, :], lrow[64:65, 2, :], ALU.add)
                    kb.tt(den, den, lrow[64:65, 3, :], ALU.add)
                    kb.op('dve', lambda E, o, i: E.reciprocal(out=o, in_=i), [lrow[64:65, 5, :]], [den])
                    for g in range(3):
                        kb.tt(wrow[64:65, g, :], lrow[64:65, 1 + g, :], lrow[64:65, 5, :], ALU.mult)
                    for g in range(3):
                        kb.mm(ps_bc[:, :], C['ones_b'][64:65, 0:64], wrow[64:65, g, :])
                        if g == 0:
                            kb.tt(ycs[:, :], ps_bc[:, :], OG[g][hd][0:64, sl_], ALU.mult)
                        else:
                            kb.tt(t1[0:64, :], ps_bc[:, :], OG[g][hd][0:64, sl_], ALU.mult)
                            kb.tt(ycs[:, :], ycs[:, :], t1[0:64, :], ALU.add)
                    kb.copy(ycb[:, :], ycs[:, :], eng='act')
                    kb.dma('pool', YC[hp * 128 + hd * 64:hp * 128 + hd * 64 + 64, sl_], ycb[:, :])
    with Phase(kb) as ph:
        pc = load_w_bf16(net, ph, net.inp['p_c'], net.inp['p_c'].t[l], 4, 1024, 'pc')
        T = bo_scratch(net, ph, 512, npp=1)
        yc = ph.sb(net.n('yc'), [128, 4, 512], BF16)
        for tt in range(8):
            kb.dma('sp', yc[:, :, :], View(YC, YC.t[:, tt * 512:(tt + 1) * 512].rearrange("(m p) t -> p m t", p=128)))
            branch_out(net, T, pc, [yc[:, kc, :] for kc in range(4)], O_GATE + 2048, net.dr['GC'], tt * 512, 512)
C0 = float(np.exp(-0.5))
GN_EPS = 1e-5 * 64
TT = 256


def phase_rwkv(net, l, vec, C):
    kb = net.kb
    zT = net.dr['zT']; VF = net.dr['VF']
    with Phase(kb) as ph:
        pbw = load_w_bf16(net, ph, net.inp['p_b'], net.inp['p_b'].t[l], 8, 1024, 'pbw')
        wa2 = ph.sb(net.n('wa2'), [128, 1024], BF16)
        g2a = ph.sb(net.n('g2a'), [128, 1024], BF16)
        g2b = ph.sb(net.n('g2b'), [32, 1024], BF16)
        v2 = ph.sb(net.n('v2'), [32, 1024], BF16)
        with Phase(kb) as p2:
            st = p2.sb(net.n('lst'), [128, 1024], F32)
            kb.dma('sp', st[0:64, :], net.inp['rwkv_w2'][l])
            kb.dma('sp', st[64:128, :], net.inp['rwkv_a2'][l])
            kb.copy(wa2[:, :], st[:, :])
            st2 = p2.sb(net.n('lst2'), [128, 1024], F32)
            kb.dma('sp', st2[:, :], net.inp['rwkv_g2'][l, 0:128, :])
            kb.copy(g2a[:, :], st2[:, :])
            st3 = p2.sb(net.n('lst3'), [32, 1024], F32)
            kb.dma('sp', st3[:, :], net.inp['rwkv_g2'][l, 128:160, :])
            kb.copy(g2b[:, :], st3[:, :])
            if l == 1:
                st4 = p2.sb(net.n('lst4'), [32, 1024], F32)
                kb.dma('sp', st4[:, :], net.inp['rwkv_v2'][0])
                kb.copy(v2[:, :], st4[:, :])
        KR = 2
        W = TT + 1
        zin = {k_: ph.sb(net.n('z' + k_), [128, 8, W], BF16) for k_ in 'rkv'}
        zwa = ph.sb(net.n('zwa'), [128, W], BF16)
        zg1 = ph.sb(net.n('zg1'), [128, W], BF16)
        zg2 = ph.sb(net.n('zg2'), [32, W], BF16)
        uv1 = ph.sb(net.n('uv1'), [32, TT], BF16)
        ft = {k_: ph.sb(net.n('ft' + k_), [128, TT], F32) for k_ in ['d', 'wa', 'g1', 'g2']}
        bt16 = {k_: ph.sb(net.n('bt' + k_), [128, TT], BF16) for k_ in ['wa', 'g1']}
        bg2 = ph.sb(net.n('bg2'), [32, TT], BF16)
        yb = ph.sb(net.n('yb'), [128, 8, TT], BF16)
        STf = [ph.sb(net.n('STf'), [128, 128], F32) for _ in range(8)]
        STb = [ph.sb(net.n('STb'), [128, 128], BF16) for _ in range(8)]
        for j in range(8):
            kb.memset(STf[j][:, :], 0.0)
            kb.memset(STb[j][:, :], 0.0)

        def mkset():
            X = {}
            X['vf_t'] = ph.sb(net.n('vf_t'), [128, TT], BF16)
            X['f'] = {k_: ph.sb(net.n('f' + k_), [128, TT], F32) for k_ in
                      ['d', 'r', 'k', 'v', 'sg', 'a', 'g', 's', 'kk', 'kkn', 'k2', 'tmp', 'cs', 'e', 'ka', 'y', 'yc', 'bon']}
            X['b16'] = {k_: ph.sb(net.n('b' + k_), [128, TT], BF16) for k_ in ['sq', 'rk']}
            X['ARt'] = ph.sb(net.n('ARt'), [128, 4, 192], BF16)
            for k_ in ('Bt', 'Kt', 'Bb', 'Kb', 'Vb'):
                X[k_] = ph.sb(net.n(k_), [128, 4, 128], BF16)
            for k_ in ('ARt', 'Bt', 'Kt', 'Bb', 'Kb', 'Vb'):
                kb.memset(X[k_][:, :, :], 0.0)
            X['AB'] = ph.sb(net.n('AB'), [128, 4, 192], BF16)
            X['AK'] = ph.sb(net.n('AK'), [128, 4, 192], BF16)
            for k_ in ('Ui', 'Li', 'Gi'):
                X[k_] = [ph.sb(net.n(k_), [128, 4, 128], BF16) for _ in range(2)]
            for k_ in ('Vtm', 'Bbtm', 'Kbtm'):
                X[k_] = ph.sb(net.n(k_), [128, 4, 128], BF16)
            X['RHS'] = ph.sb(net.n('RHS'), [128, 128], BF16)
            X['SA'] = ph.sb(net.n('SA'), [128, 128], BF16)
            X['Wc'] = ph.sb(net.n('Wc'), [128, 4], F32)
            P = [ph.ps(net.n('P'), [128, 512], F32) for _ in range(4)]
            X['P'] = P
            X['pA'] = lambda c: View(P[c // 2], P[c // 2].t[:, (c % 2) * 256:(c % 2) * 256 + 192])
            X['pA2'] = lambda h2: View(P[h2], P[h2].t[:, :].rearrange("p (c t) -> p c t", t=256)[:, :, 0:192])
            X['pB'] = Sub(P[0], P[0].t[:, :].rearrange("p (c t) -> p c t", t=128))
            X['pC'] = Sub(P[1], P[1].t[:, :].rearrange("p (c t) -> p c t", t=128))
            X['pD'] = Sub(P[2], P[2].t[:, :].rearrange("p (c t) -> p c t", t=128))
            X['pS'] = Sub(P[3], P[3].t[:, 0:256])
            X['pTr'] = Sub(P[3], P[3].t[:, 256:512].bitcast(BF16).rearrange("p (c t) -> p c t", t=128))
            return X
        sets = [mkset() for _ in range(KR)]
        T = {'gt': ph.sb(net.n('gt'), [128, 8, TT], BF16), 'sg': ph.sb(net.n('sg'), [128, TT], F32),
             'go': ph.sb(net.n('go'), [128, 8, TT], BF16), 'pp': [sets[0]['P'][2]]}

        def rows(off, a, b):
            return View(zT, zT.t[off:off + 1024, a:b].rearrange("(m p) t -> p m t", p=128))

        def vc(col, j=0):
            return vec[:, col + j:col + j + 1]

        def shift(dst, src, mu, d_):
            P_ = src.ap.shape[0]
            dd = d_[0:P_, :]
            kb.tt(dd, View(src.buf, src.ap[:, 0:TT]), View(src.buf, src.ap[:, 1:W]), ALU.subtract, eng='pool')
            kb.stt(dst, dd, mu, View(src.buf, src.ap[:, 1:W]), ALU.mult, ALU.add)

        def bd_write(dst, c_lo, src_fn):
            for hd in range(2):
                rs_ = slice(hd * 64, hd * 64 + 64)
                src_fn(rs_, dst[rs_, :, c_lo + hd * 64:c_lo + hd * 64 + 64])

        def v3(b, rs_=slice(0, 128)):
            return b.v(b.t[rs_, :].rearrange("p (c t) -> p c t", t=64))

        def body(ti, j, X):
            t0 = ti * TT
            f = X['f']; b16 = X['b16']; vf_t = X['vf_t']
            ARt = X['ARt']; Bt = X['Bt']; Kt = X['Kt']; Bb = X['Bb']; Kb = X['Kb']; Vb = X['Vb']
            AB = X['AB']; AK = X['AK']; Ui = X['Ui']; Li = X['Li']; Gi = X['Gi']
            Vtm = X['Vtm']; Bbtm = X['Bbtm']; Kbtm = X['Kbtm']; RHS = X['RHS']; SA = X['SA']; Wc = X['Wc']
            pA = X['pA']; pA2 = X['pA2']; pB = X['pB']; pC = X['pC']; pD = X['pD']; pS = X['pS']; pTr = X['pTr']
            cs_ = slice(j * 128, (j + 1) * 128)
            shift(f['r'][:, :], zin['r'][:, j, :], vc(V_MUR, j), f['d'])
            shift(f['k'][:, :], zin['k'][:, j, :], vc(V_MUK, j), f['d'])
            shift(f['v'][:, :], zin['v'][:, j, :], vc(V_MUV, j), f['d'])
            yield
            kb.mm(pS[:, :], wa2[0:64, cs_], bt16['wa'][0:64, :])
            kb.act(f['sg'][:, :], pS[:, :], AF.Sigmoid, bias=vc(V_W0, j))
            kb.mm(pS[:, :], wa2[64:128, cs_], bt16['wa'][64:128, :])
            kb.act(f['a'][:, :], pS[:, :], AF.Sigmoid, bias=vc(V_A0, j))
            yield
            kb.mm(pS[:, :], g2a[:, cs_], bt16['g1'][:, :], start=True, stop=False)
            kb.mm(pS[:, :], g2b[:, cs_], bg2[:, :], start=False, stop=True)
            kb.copy(f['g'][:, :], pS[:, :], eng='act')
            if l == 1:
                kb.mm(pS[:, :], v2[:, cs_], uv1[:, :])
                kb.act(f['s'][:, :], pS[:, :], AF.Sigmoid, bias=vc(V_V0, j))
                kb.dma('sp', vf_t[:, :], VF[cs_, t0:t0 + TT])
                kb.tt(f['tmp'][:, :], vf_t[:, :], f['v'][:, :], ALU.subtract)
                kb.tt(f['tmp'][:, :], f['tmp'][:, :], f['s'][:, :], ALU.mult)
                kb.tt(f['v'][:, :], f['v'][:, :], f['tmp'][:, :], ALU.add)
            else:
                kb.copy(vf_t[:, :], f['v'][:, :], eng='act')
                kb.dma('pool', VF[cs_, t0:t0 + TT], vf_t[:, :])
            yield
            kb.ts(f['kk'][:, :], f['k'][:, :], vc(V_KK, j), ALU.mult)
            kb.tt(b16['sq'][:, :], f['kk'][:, :], f['kk'][:, :], ALU.mult)
            kb.mm(pS[:, :], C['bones_b'][:, :], b16['sq'][:, :])
            kb.ts(f['tmp'][:, :], pS[:, :], 1e-24, ALU.max)
            kb.act(f['tmp'][:, :], f['tmp'][:, :], AF.Sqrt)
            kb.op('dve', lambda E, o, i: E.reciprocal(out=o, in_=i), [f['tmp'][:, :]], [f['tmp'][:, :]])
            kb.tt(f['kkn'][:, :], f['kk'][:, :], f['tmp'][:, :], ALU.mult)
            yield
            kb.ts(f['tmp'][:, :], f['a'][:, :], vc(V_KA, j), ALU.mult, vc(V_OMKA, j), ALU.add)
            kb.tt(f['k2'][:, :], f['k'][:, :], f['tmp'][:, :], ALU.mult)
            kb.tt(f['ka'][:, :], f['kkn'][:, :], f['a'][:, :], ALU.mult)
            kb.scan(f['cs'][:, :], C['cmask'][:, :], f['sg'][:, :], 0.0, ALU.mult, ALU.add)
            cs3 = v3(f['cs'])
            yield
            kb.tt(f['tmp'][:, :], f['cs'][:, :], f['sg'][:, :], ALU.subtract)
            kb.act(f['e'][:, :], f['tmp'][:, :], AF.Exp, scale=-C0)
            kb.tt(f['tmp'][:, :], f['kkn'][:, :], f['e'][:, :], ALU.mult)
            bd_write(ARt, 0, lambda rs_, o: kb.ts(o, v3(f['tmp'], rs_), -1.0, ALU.mult))
            kb.act(f['e'][:, :], f['cs'][:, :], AF.Exp, scale=-C0)
            kb.tt(ARt.v(ARt.t[:, :, 128:192]), v3(f['r']), v3(f['e']), ALU.mult)
            yield
            kb.act(f['e'][:, :], f['cs'][:, :], AF.Exp, scale=C0)
            bd_write(Bt, 0, lambda rs_, o: kb.tt(o, v3(f['ka'], rs_), v3(f['e'], rs_), ALU.mult))
            bd_write(Kt, 0, lambda rs_, o: kb.tt(o, v3(f['k2'], rs_), v3(f['e'], rs_), ALU.mult, eng='pool'))
            yield
            kb.tt(v3(f['tmp']), f['cs'].v(cs3.ap[:, :, 63:64].to_broadcast([128, 4, 64])), cs3, ALU.subtract)
            kb.act(f['e'][:, :], f['tmp'][:, :], AF.Exp, scale=-C0)
            kb.act(Wc[:, :], f['cs'].v(cs3.ap[:, :, 63]), AF.Exp, scale=-C0)
            bd_write(Bb, 0, lambda rs_, o: kb.tt(o, v3(f['ka'], rs_), v3(f['e'], rs_), ALU.mult))
            bd_write(Kb, 0, lambda rs_, o: kb.tt(o, v3(f['k2'], rs_), v3(f['e'], rs_), ALU.mult, eng='pool'))
            bd_write(Vb, 0, lambda rs_, o: kb.copy(o, v3(f['v'], rs_), eng='act'))
            yield
            mab2 = C['m_ab'].v(C['m_ab'].t[:, :].unsqueeze(1).to_broadcast([128, 2, 192]))
            for c in range(4):
                kb.mm(pA(c), Bt[:, c, :], ARt[:, c, :])
            for h2 in range(2):
                kb.tt(AB[:, 2 * h2:2 * h2 + 2, :], pA2(h2), mab2, ALU.mult)
            yield
            for c in range(4):
                kb.mm(pA(c), Kt[:, c, :], ARt[:, c, :])
            for h2 in range(2):
                kb.tt(AK[:, 2 * h2:2 * h2 + 2, :], pA2(h2), mab2, ALU.mult)
            yield
            for c in range(4):
                kb.mm(pB[:, c, :], ARt[:, c, 0:128], Bt[:, c, :])
            kb.tt(Li[0][:, :, :], pB[:, :, :], C['m_l'].v(C['m_l'].t[:, :].unsqueeze(1).to_broadcast([128, 4, 128])), ALU.mult)
            kb.copy(Ui[0][:, :, :], AB[:, :, 0:128], eng='pool')
            kb.tt(Gi[0][:, :, :], AB[:, :, 0:128], C['ident_b'].v(C['ident_b'].t[:, :].unsqueeze(1).to_broadcast([128, 4, 128])), ALU.add, eng='pool')
            yield
            cu, cl_, cg = 0, 0, 0
            for lev in range(5):
                Uo, Lo, Go = Ui[cu], Li[cl_], Gi[cg]
                Un, Ln, Gn = Ui[1 - cu], Li[1 - cl_], Gi[1 - cg]
                for c in range(4):
                    kb.mm(pB[:, c, :], Uo[:, c, :], Lo[:, c, :])
                if lev < 4:
                    for c in range(4):
                        kb.mm(pC[:, c, :], Lo[:, c, :], Uo[:, c, :])
                kb.copy(Ln[:, :, :], pB[:, :, :], eng='act')
                if lev < 4:
                    kb.copy(Un[:, :, :], pC[:, :, :], eng='dve')
                yield
                for c in range(4):
                    kb.mm(pD[:, c, :], Ln[:, c, :], Go[:, c, :])
                kb.tt(Gn[:, :, :], pD[:, :, :], Go[:, :, :], ALU.add)
                cu, cl_, cg = 1 - cu, 1 - cl_, 1 - cg
                yield
            G = Gi[cg]
            for src, dst in ((Vb, Vtm), (Bb, Bbtm), (Kb, Kbtm)):
                for c in range(4):
                    kb.transpose(pTr[:, c, :], src[:, c, :], C['ident_b'][:, :])
                kb.copy(dst[:, :, :], pTr[:, :, :], eng='act')
            yield
            for c in range(4):
                kb.mm(pD[:, 0, :], ARt[:, c, 0:128], STb[j][:, :], start=True, stop=False)
                kb.mm(pD[:, 0, :], AK[:, c, 0:128], Vtm[:, c, :], start=False, stop=True)
                kb.copy(RHS[:, :], pD[:, 0, :], eng='act')
                yield
                kb.mm(pD[:, 1, :], G[:, c, :], RHS[:, :])
                kb.copy(SA[:, :], pD[:, 1, :], eng='act')
                yield
                kb.mm(pS[:, c * 64:(c + 1) * 64], STb[j][:, :], ARt[:, c, 128:192], start=True, stop=False)
                kb.mm(pS[:, c * 64:(c + 1) * 64], SA[:, :], AB[:, c, 128:192], start=False, stop=False)
                kb.mm(pS[:, c * 64:(c + 1) * 64], Vtm[:, c, :], AK[:, c, 128:192], start=False, stop=True)
                kb.mm(pD[:, 2, :], Bbtm[:, c, :], SA[:, :], start=True, stop=False)
                kb.mm(pD[:, 2, :], Kbtm[:, c, :], Vtm[:, c, :], start=False, stop=True)
                kb.stt(STf[j][:, :], STf[j][:, :], Wc[:, c:c + 1], pD[:, 2, :], ALU.mult, ALU.add)
                kb.copy(STb[j][:, :], STf[j][:, :], eng='act')
                yield
            kb.copy(f['y'][:, :], pS[:, :], eng='act')
            kb.mm(pS[:, :], C['bmean_f'][:, :], f['y'][:, :])
            kb.tt(f['yc'][:, :], f['y'][:, :], pS[:, :], ALU.subtract)
            kb.tt(f['tmp'][:, :], f['yc'][:, :], f['yc'][:, :], ALU.mult)
            yield
            kb.mm(pS[:, :], C['bmean_f'][:, :], f['tmp'][:, :])
            kb.ts(f['tmp'][:, :], pS[:, :], GN_EPS, ALU.add)
            kb.act(f['tmp'][:, :], f['tmp'][:, :], AF.Sqrt)
            kb.op('dve', lambda E, o, i: E.reciprocal(out=o, in_=i), [f['tmp'][:, :]], [f['tmp'][:, :]])
            kb.tt(f['yc'][:, :], f['yc'][:, :], f['tmp'][:, :], ALU.mult)
            kb.ts(f['yc'][:, :], f['yc'][:, :], vc(V_LG, j), ALU.mult, vc(V_LB, j), ALU.add)
            yield
            kb.tt(f['tmp'][:, :], f['r'][:, :], f['k2'][:, :], ALU.mult)
            kb.ts(b16['rk'][:, :], f['tmp'][:, :], vc(V_RK, j), ALU.mult)
            kb.mm(pS[:, :], C['bones_b'][:, :], b16['rk'][:, :])
            kb.tt(f['bon'][:, :], pS[:, :], f['v'][:, :], ALU.mult)
            kb.tt(f['yc'][:, :], f['yc'][:, :], f['bon'][:, :], ALU.add)
            kb.tt(yb[:, j, :], f['yc'][:, :], f['g'][:, :], ALU.mult)
        NST = 38

        for ti in range(S // TT):
            t0 = ti * TT
            for k_, off in (('r', O_RWKV), ('k', O_RWKV + 1024), ('v', O_RWKV + 2048)):
                if ti == 0:
                    kb.memset(zin[k_][:, :, 0:1], 0.0)
                    kb.dma('sp', zin[k_][:, :, 1:W], rows(off, 0, TT))
                else:
                    kb.dma('sp', zin[k_][:, :, :], rows(off, t0 - 1, t0 + TT))
            o2 = O_RWKV + 3072
            for tl, a_, n_ in ((zwa, o2, 128), (zg1, o2 + 128, 128), (zg2, o2 + 256, 32)):
                if ti == 0:
                    kb.memset(tl[0:n_, 0:1], 0.0)
                    kb.dma('sp', tl[0:n_, 1:W], zT[a_:a_ + n_, 0:TT])
                else:
                    kb.dma('sp', tl[0:n_, :], zT[a_:a_ + n_, t0 - 1:t0 + TT])
            shift(ft['wa'][:, :], zwa[:, :], vc(V_MUWA), ft['d'])
            shift(ft['g1'][:, :], zg1[:, :], vc(V_MUG), ft['d'])
            shift(ft['g2'][0:32, :], zg2[0:32, :], vec[0:32, V_MUG + 1:V_MUG + 2], ft['d'])
            kb.act(bt16['wa'][0:64, :], ft['wa'][0:64, :], AF.Tanh)
            kb.copy(bt16['wa'][64:128, :], ft['wa'][64:128, :])
            kb.act(bt16['g1'][:, :], ft['g1'][:, :], AF.Sigmoid)
            kb.act(bg2[:, :], ft['g2'][0:32, :], AF.Sigmoid)
            if l == 1:
                kb.dma('sp', uv1[:, :], zT[O_UV1:O_UV1 + 32, t0:t0 + TT])
            interleave(range(8), lambda j, slot, ti=ti: body(ti, j, sets[slot]), KR, NST)
            branch_out(net, T, pbw, [yb[:, kc, :] for kc in range(8)], O_GATE + 1024, net.dr['GB'], t0, TT)
def host_consts():
    cf = {}
    cf['ident_f'] = np.eye(128, dtype=np.float32)
    p = np.arange(128)
    inv_freq = 500000.0 ** (-np.arange(0, 16, 2, dtype=np.float32) / 16)
    invf = np.where((p % 64) < 16, inv_freq[(p % 64) % 8] / (2 * np.pi), 0.0).astype(np.float32)
    cf['invf'] = invf[:, None]
    qi = np.arange(128)[:, None]; kj = np.arange(256)[None, :]
    cf['amask'] = np.where((kj >= qi) & (kj <= qi + 128), 0.0, -1e30).astype(np.float32)
    blk = (p[:, None] // 64) == (p[None, :] // 64)
    cf['bmean_f'] = (blk / 64.0).astype(np.float32)
    cm = np.ones((128, 256), np.float32); cm[:, ::64] = 0.0
    cf['cmask'] = cm
    s_ = (p % 64)[:, None]
    mab = np.zeros((128, 192), np.float32)
    mab[:, :128] = blk & (s_ < (p % 64)[None, :])
    mab[:, 128:] = (s_ <= np.arange(64)[None, :])
    cf['m_ab'] = mab
    cf['m_l'] = (blk & (s_ > (p % 64)[None, :])).astype(np.float32)
    cb = {}
    cb['ident_b'] = np.eye(128, dtype=np.float32)
    PT = np.zeros((128, 128), np.float32)
    for h in range(2):
        for i in range(8):
            PT[h * 64 + i + 8, h * 64 + i] = -1.0
            PT[h * 64 + i, h * 64 + i + 8] = 1.0
    cb['PT'] = PT
    cb['ones_b'] = np.ones((128, 128), np.float32)
    cb['bones_b'] = blk.astype(np.float32)
    return cf, cb


CF_KEYS = ['ident_f', 'invf', 'amask', 'bmean_f', 'cmask', 'm_ab', 'm_l']
CB_KEYS = ['ident_b', 'PT', 'ones_b', 'bones_b']


def host_vecs(I):
    out = np.zeros((L, 128, NV), np.float32)

    def pc(v):
        return np.ascontiguousarray(v.reshape(-1, 128).T)
    for l in range(L):
        o = out[l]
        for j in range(3):
            o[:, V_CONV + j * 8:V_CONV + j * 8 + 8] = pc(I['conv_w'][l, j])
        mu = I['rwkv_mu'][l]
        o[:, V_MUR:V_MUR + 8] = pc(mu[0:1024]); o[:, V_MUK:V_MUK + 8] = pc(mu[1024:2048]); o[:, V_MUV:V_MUV + 8] = pc(mu[2048:3072])
        o[:, V_MUWA] = mu[3072:3200]
        o[:, V_MUG] = mu[3200:3328]
        o[0:32, V_MUG + 1] = mu[3328:3360]
        o[:, V_W0:V_W0 + 8] = pc(I['rwkv_w0'][l]); o[:, V_A0:V_A0 + 8] = pc(I['rwkv_a0'][l])
        if l >= 1:
            o[:, V_V0:V_V0 + 8] = pc(I['rwkv_v0'][l - 1])
        o[:, V_KK:V_KK + 8] = pc(I['rwkv_k_k'][l]); o[:, V_KA:V_KA + 8] = pc(I['rwkv_k_a'][l])
        o[:, V_RK:V_RK + 8] = pc(I['rwkv_r_k'][l].reshape(-1))
        o[:, V_LG:V_LG + 8] = pc(I['rwkv_lnx_g'][l]); o[:, V_LB:V_LB + 8] = pc(I['rwkv_lnx_b'][l])
    return out


IN_SHAPES = {'x': [S, D], 'positions': [S], 'ln_in_g': [D], 'ln_in_b': [D], 'w_in': [L, D, NIN], 'rwkv_w2': [L, 64, D], 'rwkv_a2': [L, 64, D],
             'rwkv_g2': [L, 160, D], 'rwkv_v1': [1, D, 32], 'rwkv_v2': [1, 32, D], 'p_a': [L, D, D], 'p_b': [L, D, D], 'p_c': [L, 512, D],
             'w_o': [L, D, D], 'ln1_g': [L, D], 'ln1_b': [L, D], 'w_router': [128, 8, 128], 'router_bias': [128, 16], 'w_gate': [L, 16, D, 512],
             'w_up': [L, 16, D, 512], 'w_down': [L, 16, 512, D], 'ln2_g': [L, D], 'ln2_b': [L, D], 'vecs': [L, 128, NV]}


def build(debug=(), stop_after=None, skip=()):
    net = Net(debug=debug)
    kb = net.kb
    for k_, sh in IN_SHAPES.items():
        net.din(k_, sh, I32 if k_ == 'positions' else F32)
    cfh, cbh = host_consts()
    for k_ in CF_KEYS:
        net.din('c_' + k_, list(cfh[k_].shape), F32)
    for k_ in CB_KEYS:
        net.din('c_' + k_, list(cbh[k_].shape), BF16)
    out = net.nc.dram_tensor('out', [S, D], F32, kind="ExternalOutput")
    out = Buf(kb, out.ap(), dram=True)
    net.dscr('hA', [S, D], F32); net.dscr('hB', [S, D], F32); net.dscr('zT', [NZ, S], BF16)
    for k_ in ('GA', 'GB', 'GC', 'VF'):
        net.dscr(k_, [D, S], BF16)
    net.dscr('YC', [512, S], BF16)
    with Phase(kb) as g:
        C = {}
        for k_ in CF_KEYS:
            C[k_] = g.sb(net.n('k' + k_), list(cfh[k_].shape), F32)
            kb.dma('sp', C[k_][:, :], net.inp['c_' + k_][:, :])
        for k_ in CB_KEYS:
            C[k_] = g.sb(net.n('k' + k_), list(cbh[k_].shape), BF16)
            kb.dma('sp', C[k_][:, :], net.inp['c_' + k_][:, :])
        vec = []
        for l in range(L):
            v_ = g.sb(net.n('vec'), [128, NV], F32)
            kb.dma('sp', v_[:, :], net.inp['vecs'][l])
            kb.ts(v_[:, V_OMKA:V_OMKA + 8], v_[:, V_KA:V_KA + 8], -1.0, ALU.mult, 1.0, ALU.add)
            vec.append(v_)
        CW = g.sb(net.n('CW'), [128, 32, 16], F32)

        def mk_hT(ph):
            return [ph.sb(net.n('hT'), [128, 8, 1024], BF16) for _ in range(4)]
        with Phase(kb) as A:
            hT = mk_hT(A)
            phase0(net, hT, C)
            phase1(net, 0, hT)
        for l in range(L):
            if stop_after == ('p1', l):
                return net
            phase_conv(net, l, vec[l])
            if stop_after == ('conv', l):
                return net
            if 'rwkv' not in skip:
                phase_rwkv(net, l, vec[l], C)
            if stop_after == ('rwkv', l):
                return net
            if 'attn' not in skip:
                phase_attn(net, l, C)
            if stop_after == ('attn', l):
                return net
            with Phase(kb) as B:
                hT = mk_hT(B)
                phase_mix(net, l, vec[l], hT, C, net.dr['hA'], net.dr['hB'], CW)
                if stop_after == ('mix', l):
                    return net
                phase_moe(net, l, hT, C, CW, net.dr['hB'], net.dr['hA'], l == L - 1, out)
                if stop_after == ('moe', l):
                    return net
                if l < L - 1:
                    phase1(net, l + 1, hT)
    return net


def make_inputs(I, b):
    cfh, cbh = host_consts()
    m = {}
    for k_ in IN_SHAPES:
        if k_ == 'vecs':
            continue
        a = I[k_]
        if k_ in ('x', 'positions'):
            a = a[b]
        if k_ == 'w_router':
            a = np.concatenate([a.reshape(8, 128, 16).transpose(1, 0, 2), np.zeros((128, 8, 112), np.float32)], axis=2)
        if k_ == 'router_bias':
            a = np.broadcast_to(a[None, :], (128, 16))
        m[k_] = np.ascontiguousarray(a)
    m['vecs'] = host_vecs(I)
    for k_ in CF_KEYS:
        m['c_' + k_] = cfh[k_]
    for k_ in CB_KEYS:
        m['c_' + k_] = cbh[k_].astype(ml_dtypes.bfloat16)
    return m


def kernel(**inputs):
    I = {k_: np.asarray(v_) for k_, v_ in inputs.items()}
    net = build()
    in_maps = [make_inputs(I, b) for b in range(8)]
    res = run_bass_kernel_spmd(net.nc, in_maps, core_ids=list(range(8)))
    return np.stack([np.asarray(r["out"], dtype=np.float32) for r in res.results], axis=0)
```

```python
import ml_dtypes
import numpy as np
from contextlib import ExitStack
import concourse.bass as bass
import concourse.mybir as mybir
from concourse.bass_utils import run_bass_kernel_spmd

F32 = mybir.dt.float32
BF16 = mybir.dt.bfloat16
I32 = mybir.dt.int32
AF = mybir.ActivationFunctionType
ALU = mybir.AluOpType
AX = mybir.AxisListType


class Sem:
    def __init__(self, h, is_dma):
        self.h = h
        self.is_dma = is_dma
        self.total = 0


class View:
    def __init__(self, buf, ap):
        self.buf = buf
        self.ap = ap


class Buf:
    def __init__(self, kb, t, dram=False):
        self.kb = kb
        self.t = t
        self.dram = dram
        self.w = {}
        self.r = {}
        self.ds = {}

    def __getitem__(self, idx):
        return View(self, self.t[idx])

    def v(self, ap):
        return View(self, ap)

    def dsem(self, q):
        if q not in self.ds:
            self.ds[q] = self.kb.new_dma_sem(q)
        return self.ds[q]


class KB:
    def __init__(self, nc):
        self.nc = nc
        self.engs = {'pe': nc.tensor, 'act': nc.scalar, 'dve': nc.vector, 'pool': nc.gpsimd, 'sp': nc.sync}
        self.esem = {}
        for k in ['pe', 'act', 'dve', 'pool']:
            self.esem[k] = Sem(nc.alloc_semaphore('es_' + k), False)
        self.seen = {k: {} for k in self.engs}
        self.allsems = list(self.esem.values())
        self.nds = 0
        self.ninst = 0
        self.free_ds = {'sp': [], 'pool': [], 'act': []}

    def new_dma_sem(self, q):
        if self.free_ds[q]:
            return self.free_ds[q].pop()
        s = Sem(self.nc.alloc_semaphore('ds%d' % self.nds), True)
        self.nds += 1
        self.allsems.append(s)
        return s

    def _wait(self, eng, need):
        E = self.engs[eng]
        seen = self.seen[eng]
        for s, c in need.items():
            if s.is_dma:
                c = s.total
            elif eng == 'pe' and s is self.esem['pe']:
                continue
            if seen.get(s, 0) < c:
                E.wait_ge(s.h, c)
                seen[s] = c
                self.ninst += 1

    def _deps(self, reads, writes):
        need = {}

        def add(d):
            for s, c in d.items():
                if need.get(s, 0) < c:
                    need[s] = c
        for b in reads:
            add(b.w)
        for b in writes:
            add(b.w)
            add(b.r)
        return need

    def _stamp(self, reads, writes, s, c):
        for b in reads:
            if b.r.get(s, 0) < c:
                b.r[s] = c
        for b in writes:
            if b.dram:
                if b.w.get(s, 0) < c:
                    b.w[s] = c
            else:
                b.w = {s: c}
                b.r = {}

    def op(self, eng, fn, outs, ins):
        reads = [v.buf for v in ins]
        writes = [v.buf for v in outs]
        self._wait(eng, self._deps(reads, writes))
        ins_ = fn(self.engs[eng], *[v.ap for v in outs], *[v.ap for v in ins])
        s = self.esem[eng]
        s.total += 1
        ins_.then_inc(s.h, 1)
        self.ninst += 1
        self._stamp(reads, writes, s, s.total)
        return ins_

    def dma(self, q, out, in_, **kw):
        reads = [in_.buf]
        writes = [out.buf]
        self._wait(q, self._deps(reads, writes))
        sb = out.buf if not out.buf.dram else in_.buf
        s = sb.dsem(q)
        ins_ = self.engs[q].dma_start(out=out.ap, in_=in_.ap, **kw)
        s.total += 16
        ins_.then_inc(s.h, 16)
        self.ninst += 1
        self._stamp(reads, writes, s, s.total)
        return ins_

    def barrier(self):
        need = {s: s.total for s in self.allsems if s.total > 0}
        for eng in self.engs:
            self._wait(eng, need)

    def mm(self, out, lhsT, rhs, start=True, stop=True):
        return self.op('pe', lambda E, o, a, b: E.matmul(o, lhsT=a, rhs=b, start=start, stop=stop), [out], [lhsT, rhs])

    def transpose(self, out, in_, ident):
        return self.op('pe', lambda E, o, a, b: E.transpose(o, a, b), [out], [in_, ident])

    def act(self, out, in_, func, bias=None, scale=None, accum=None, eng='act'):
        outs = [out] + ([accum] if accum is not None else [])
        ins = [in_]
        kw = {}
        if isinstance(bias, View):
            ins.append(bias)
        if isinstance(scale, View):
            ins.append(scale)

        def fn(E, *aps):
            aps = list(aps)
            o = aps.pop(0)
            if accum is not None:
                kw['accum_out'] = aps.pop(0)
            i = aps.pop(0)
            if isinstance(bias, View):
                kw['bias'] = aps.pop(0)
            elif bias is not None:
                kw['bias'] = bias
            if isinstance(scale, View):
                kw['scale'] = aps.pop(0)
            elif scale is not None:
                kw['scale'] = scale
            return E.activation(out=o, in_=i, func=func, **kw)
        return self.op(eng, fn, outs, ins)

    def tt(self, out, a, b, op, eng='dve'):
        return self.op(eng, lambda E, o, x, y: E.tensor_tensor(out=o, in0=x, in1=y, op=op), [out], [a, b])

    def ts(self, out, a, s1, op0, s2=None, op1=None, eng='dve', accum=None):
        ins = [a]
        outs = [out] + ([accum] if accum is not None else [])
        if isinstance(s1, View):
            ins.append(s1)
        if isinstance(s2, View):
            ins.append(s2)

        def fn(E, *aps):
            aps = list(aps)
            o = aps.pop(0)
            kw = {}
            if accum is not None:
                kw['accum_out'] = aps.pop(0)
            x = aps.pop(0)
            a1 = aps.pop(0) if isinstance(s1, View) else s1
            a2 = aps.pop(0) if isinstance(s2, View) else s2
            if op1 is None:
                return E.tensor_scalar(out=o, in0=x, scalar1=a1, scalar2=None, op0=op0, **kw)
            return E.tensor_scalar(out=o, in0=x, scalar1=a1, scalar2=a2, op0=op0, op1=op1, **kw)
        return self.op(eng, fn, outs, ins)

    def stt(self, out, a, s, b, op0, op1, eng='dve'):
        ins = [a, b]
        if isinstance(s, View):
            ins.append(s)

        def fn(E, o, x, y, *rest):
            sc = rest[0] if rest else s
            return E.scalar_tensor_tensor(out=o, in0=x, scalar=sc, in1=y, op0=op0, op1=op1)
        return self.op(eng, fn, [out], ins)

    def copy(self, out, in_, eng='dve'):
        if eng == 'act':
            return self.op('act', lambda E, o, i: E.copy(out=o, in_=i), [out], [in_])
        return self.op(eng, lambda E, o, i: E.tensor_copy(out=o, in_=i), [out], [in_])

    def memset(self, out, val, eng='pool'):
        return self.op(eng, lambda E, o: E.memset(o, val), [out], [])

    def reduce(self, out, in_, op, axis=AX.X, eng='dve'):
        return self.op(eng, lambda E, o, i: E.tensor_reduce(out=o, in_=i, axis=axis, op=op), [out], [in_])

    def scan(self, out, d0, d1, initial, op0, op1):
        return self.op('dve', lambda E, o, x, y: E.tensor_tensor_scan(out=o, data0=x, data1=y, initial=initial, op0=op0, op1=op1), [out], [d0, d1])


class Phase:
    def __init__(self, kb):
        self.kb = kb
        self.st = ExitStack()
        self.bufs = []

    def __enter__(self):
        self.st.__enter__()
        return self

    def __exit__(self, *a):
        self.kb.barrier()
        for b in self.bufs:
            for q_, s_ in b.ds.items():
                self.kb.free_ds[q_].append(s_)
            b.ds = {}
        return self.st.__exit__(*a)

    def sb(self, name, shape, dt):
        t = self.st.enter_context(self.kb.nc.sbuf_tensor(name, list(shape), dt))
        b = Buf(self.kb, t)
        self.bufs.append(b)
        return b

    def ps(self, name, shape, dt=F32):
        t = self.st.enter_context(self.kb.nc.psum_tensor(name, list(shape), dt))
        return Buf(self.kb, t)

    def bank(self, name, dt=F32):
        n = 512 if dt == F32 else 1024
        return self.st.enter_context(self.kb.nc.psum_tensor(name, [128, n], dt))

    def sub(self, ap):
        return Buf(self.kb, ap)


class Sub:
    def __init__(self, buf, ap):
        self.buf = buf
        self.ap = ap

    def __getitem__(self, idx):
        return View(self.buf, self.ap[idx])


def interleave(items, make_gen, K, nstage):
    it = iter(items)
    slots = [None] * K
    start_at = [(k * nstage) // K for k in range(K)]
    rnd = 0
    exhausted = False
    while True:
        alive = False
        for k in range(K):
            if slots[k] is None and not exhausted and rnd >= start_at[k]:
                try:
                    slots[k] = make_gen(next(it), k)
                except StopIteration:
                    exhausted = True
            if slots[k] is not None:
                alive = True
                try:
                    next(slots[k])
                except StopIteration:
                    slots[k] = None
        if not alive and exhausted:
            break
        rnd += 1

S = 4096; D = 1024; NIN = 14112; L = 2
ALPHA = (2 * L) ** 0.25
LN_EPS = 1e-5
O_GATE = 0; O_CONV = 3072; O_RWKV = 6144; O_ATTN = 9504
O_UV1 = NIN
NZ = NIN + 32


def cdiv(a, b):
    return (a + b - 1) // b


class Net:
    def __init__(self, debug=()):
        nc = self.nc = bass.Bass("TRN2", target_bir_lowering=False)
        self.kb = KB(nc)
        self.debug = debug
        self.inp = {}
        self.dr = {}
        self.uid = 0

    def din(self, name, shape, dt=F32):
        t = self.nc.dram_tensor(name, list(shape), dt, kind="ExternalInput")
        b = Buf(self.kb, t.ap(), dram=True)
        self.inp[name] = b
        return b

    def dscr(self, name, shape, dt):
        kind = "ExternalOutput" if name in self.debug else "Internal"
        t = self.nc.dram_tensor(name, list(shape), dt, kind=kind)
        b = Buf(self.kb, t.ap(), dram=True)
        self.dr[name] = b
        return b

    def n(self, s):
        self.uid += 1
        return "%s_%d" % (s, self.uid)


def layernorm_tile(net, ph, T, xin, gB, bB, out):
    kb = net.kb
    st = T['st']; mv = T['mv']; sd = T['sd']
    for c in range(2):
        kb.op('dve', lambda E, o, i: E.bn_stats(out=o, in_=i), [st[:, c, :]], [View(xin.buf, xin.ap[:, c * 512:(c + 1) * 512])])
    kb.op('dve', lambda E, o, i: E.bn_aggr(out=o, in_=i), [mv[:, :]], [st[:, :, :]])
    kb.ts(sd[:, 0:1], mv[:, 1:2], LN_EPS, ALU.add)
    kb.act(sd[:, 1:2], sd[:, 0:1], AF.Sqrt)
    kb.op('dve', lambda E, o, i: E.reciprocal(out=o, in_=i), [sd[:, 2:3]], [sd[:, 1:2]])
    kb.ts(out, xin, mv[:, 0:1], ALU.subtract, sd[:, 2:3], ALU.mult)
    kb.tt(out, out, gB, ALU.mult, eng='pool')
    kb.tt(out, out, bB, ALU.add, eng='pool')


def ln_scratch(net, ph):
    return {'st': ph.sb(net.n('lnst'), [128, 2, 6], F32), 'mv': ph.sb(net.n('lnmv'), [128, 2], F32),
            'sd': ph.sb(net.n('lnsd'), [128, 4], F32)}


def bcast_load(net, ph, name, src_ap):
    b = ph.sb(net.n(name), [128, 1024], F32)
    net.kb.dma('sp', b[:, :], View(net.cur_in, src_ap.partition_broadcast(128)))
    return b
def emit_h_tile(net, ph, T, hv, ti, hdst, hT, ident_f, router=None):
    kb = net.kb
    if hdst is not None:
        kb.dma('pool', hdst[ti * 128:(ti + 1) * 128, :], hv)
    if hT is None:
        return
    pT = T['pT']
    q, r = divmod(ti, 8)
    for hf in range(2):
        for kc in range(4):
            kb.transpose(pT[hf][:, kc, :], View(hv.buf, hv.ap[:, (hf * 4 + kc) * 128:(hf * 4 + kc + 1) * 128]), ident_f[:, :])
        kb.copy(hT[q][:, hf * 4:hf * 4 + 4, r * 128:(r + 1) * 128], pT[hf][:, :, :], eng='act')
    if router is not None:
        router(pT, ti)


def phase0(net, hT, C):
    kb = net.kb
    with Phase(kb) as ph:
        net.cur_in = net.inp['ln_in_g']
        gB = bcast_load(net, ph, 'gB', net.inp['ln_in_g'].t[:])
        net.cur_in = net.inp['ln_in_b']
        bB = bcast_load(net, ph, 'bB', net.inp['ln_in_b'].t[:])
        T = ln_scratch(net, ph)
        T['pT'] = [ph.ps(net.n('pT'), [128, 4, 128], F32) for _ in range(2)]
        xs = [ph.sb(net.n('x'), [128, 1024], F32) for _ in range(2)]
        hs = [ph.sb(net.n('h'), [128, 1024], F32) for _ in range(2)]
        x = net.inp['x']
        for ti in range(32):
            xt = xs[ti % 2]; ht = hs[ti % 2]
            kb.dma('sp', xt[:, :], x[ti * 128:(ti + 1) * 128, :])
            layernorm_tile(net, ph, T, xt[:, :], gB[:, :], bB[:, :], ht[:, :])
            emit_h_tile(net, ph, T, ht[:, :], ti, net.dr['hA'], hT, C['ident_f'])


def phase1(net, l, hT):
    kb = net.kb
    w_in = net.inp['w_in']
    zT = net.dr['zT']
    with Phase(kb) as ph:
        wst = [ph.sb(net.n('wst'), [128, 8, 128], F32) for _ in range(2)]
        wb = [ph.sb(net.n('wb'), [128, 8, 128], BF16) for _ in range(2)]
        zs = [ph.sb(net.n('zs'), [128, S], BF16) for _ in range(2)]
        pz = [ph.ps(net.n('pz'), [128, 512], F32) for _ in range(4)]
        blocks = [(c0, min(128, NIN - c0), None) for c0 in range(0, NIN, 128)]
        if l == 1:
            blocks.append((O_UV1, 32, 'v1'))
        wv = w_in.t[l].rearrange("(kc p) n -> p kc n", p=128)
        k = 0

        def prefetch(bi):
            c0, ncol, kind = blocks[bi]
            st = wst[bi % 2]; w = wb[bi % 2]
            if kind is None:
                kb.dma('sp', st[:, :, 0:ncol], View(w_in, wv[:, :, c0:c0 + ncol]))
            else:
                v1 = net.inp['rwkv_v1']
                kb.dma('sp', st[:, :, 0:ncol], View(v1, v1.t[0].rearrange("(kc p) n -> p kc n", p=128)))
            kb.copy(w[:, :, 0:ncol], st[:, :, 0:ncol], eng='pool')
        prefetch(0)
        for bi, (c0, ncol, kind) in enumerate(blocks):
            w = wb[bi % 2]; z = zs[bi % 2]
            if bi + 1 < len(blocks):
                prefetch(bi + 1)
            for tt in range(8):
                p = pz[k % 4]
                for kc in range(8):
                    kb.mm(p[0:ncol, :], w[:, kc, 0:ncol], hT[tt // 2][:, kc, (tt % 2) * 512:(tt % 2 + 1) * 512], start=(kc == 0), stop=(kc == 7))
                kb.copy(z[0:ncol, tt * 512:(tt + 1) * 512], p[0:ncol, :], eng=('act' if k % 2 == 0 else 'dve'))
                k += 1
            kb.dma('pool' if bi % 2 else 'sp', zT[c0:c0 + ncol, :], z[0:ncol, :])
NV = 123
V_CONV = 0; V_MUR = 24; V_MUK = 32; V_MUV = 40; V_MUWA = 48; V_MUG = 49; V_W0 = 51; V_A0 = 59; V_V0 = 67
V_KK = 75; V_KA = 83; V_RK = 91; V_LG = 99; V_LB = 107; V_OMKA = 115


def load_w_bf16(net, ph, src, ap, kc, ncol, name):
    kb = net.kb
    w = ph.sb(net.n(name), [128, kc, ncol], BF16)
    v = ap.rearrange("(kc p) n -> p kc n", p=128)
    with Phase(kb) as p2:
        st = [p2.sb(net.n('wstg'), [128, kc, 256], F32) for _ in range(2)]
        for i, c0 in enumerate(range(0, ncol, 256)):
            n = min(256, ncol - c0)
            kb.dma('sp', st[i % 2][:, :, 0:n], View(src, v[:, :, c0:c0 + n]))
            kb.copy(w[:, :, c0:c0 + n], st[i % 2][:, :, 0:n], eng='pool')
    return w


def branch_out(net, T, pw, rhs_list, goff, Gdst, t0, N, kp=128):
    kb = net.kb
    zT = net.dr['zT']
    gt = T['gt']; sg = T['sg']; go = T['go']; pp = T['pp']
    kb.dma('sp', gt[:, :, 0:N], View(zT, zT.t[goff:goff + 1024, t0:t0 + N].rearrange("(m p) t -> p m t", p=128)))
    for m in range(8):
        p = pp[m % len(pp)]
        for kc, r in enumerate(rhs_list):
            kb.mm(p[:, 0:N], pw[0:kp, kc, m * 128:(m + 1) * 128], r, start=(kc == 0), stop=(kc == len(rhs_list) - 1))
        kb.act(sg[:, 0:N], gt[:, m, 0:N], AF.Sigmoid)
        kb.tt(go[:, m, 0:N], p[:, 0:N], sg[:, 0:N], ALU.mult)
    kb.dma('pool', View(Gdst, Gdst.t[:, t0:t0 + N].rearrange("(m p) t -> p m t", p=128)), go[:, :, 0:N])


def bo_scratch(net, ph, N, npp=2):
    return {'gt': ph.sb(net.n('gt'), [128, 8, N], BF16), 'sg': ph.sb(net.n('sg'), [128, N], F32),
            'go': ph.sb(net.n('go'), [128, 8, N], BF16), 'pp': [ph.ps(net.n('pp'), [128, 512], F32) for _ in range(npp)]}


def phase_conv(net, l, vec):
    kb = net.kb
    zT = net.dr['zT']
    with Phase(kb) as ph:
        pa = load_w_bf16(net, ph, net.inp['p_a'], net.inp['p_a'].t[l], 8, 1024, 'pa')
        T = bo_scratch(net, ph, 512)
        cc = ph.sb(net.n('cc'), [128, 8, 514], BF16)
        chh = ph.sb(net.n('chh'), [128, 8, 514], BF16)
        cb = ph.sb(net.n('cb'), [128, 8, 512], BF16)
        yf = [ph.sb(net.n('yf'), [128, 514], F32) for _ in range(2)]
        of = [ph.sb(net.n('of'), [128, 512], F32) for _ in range(2)]
        u = ph.sb(net.n('u'), [128, 8, 512], BF16)

        def rows(off, a, b):
            return View(zT, zT.t[off:off + 1024, a:b].rearrange("(m p) t -> p m t", p=128))
        for tt in range(8):
            t0 = tt * 512
            if tt == 0:
                kb.memset(cc[:, :, 0:2], 0.0)
                kb.memset(chh[:, :, 0:2], 0.0)
                kb.dma('sp', cc[:, :, 2:514], rows(O_CONV + 1024, 0, 512))
                kb.dma('sp', chh[:, :, 2:514], rows(O_CONV + 2048, 0, 512))
            else:
                kb.dma('sp', cc[:, :, :], rows(O_CONV + 1024, t0 - 2, t0 + 512))
                kb.dma('sp', chh[:, :, :], rows(O_CONV + 2048, t0 - 2, t0 + 512))
            kb.dma('sp', cb[:, :, :], rows(O_CONV, t0, t0 + 512))
            for c in range(8):
                y = yf[c % 2]; o = of[c % 2]
                kb.tt(y[:, :], cc[:, c, :], chh[:, c, :], ALU.mult, eng='pool')
                kb.ts(o[:, :], y[:, 2:514], vec[:, V_CONV + 16 + c:V_CONV + 17 + c], ALU.mult)
                kb.stt(o[:, :], y[:, 1:513], vec[:, V_CONV + 8 + c:V_CONV + 9 + c], o[:, :], ALU.mult, ALU.add)
                kb.stt(o[:, :], y[:, 0:512], vec[:, V_CONV + c:V_CONV + 1 + c], o[:, :], ALU.mult, ALU.add)
                kb.tt(u[:, c, :], o[:, :], cb[:, c, :], ALU.mult, eng='pool')
            branch_out(net, T, pa, [u[:, c, :] for c in range(8)], O_GATE, net.dr['GA'], t0, 512)


def phase_mix(net, l, vec, hT, C, hsrc, hdst, CW):
    kb = net.kb
    with Phase(kb) as ph:
        wo = load_w_bf16(net, ph, net.inp['w_o'], net.inp['w_o'].t[l], 8, 1024, 'wo')
        net.cur_in = net.inp['ln1_g']; gB = bcast_load(net, ph, 'g1B', net.inp['ln1_g'].t[l])
        net.cur_in = net.inp['ln1_b']; bB = bcast_load(net, ph, 'b1B', net.inp['ln1_b'].t[l])
        T = ln_scratch(net, ph)
        T['pT'] = [ph.ps(net.n('pT'), [128, 4, 128], F32) for _ in range(2)]
        pm = [ph.ps(net.n('pm'), [128, 512], F32) for _ in range(2)]
        pr = ph.ps(net.n('pr'), [128, 128], F32)
        G = [ph.sb(net.n('G'), [128, 8, 512], BF16) for _ in range(3)]
        hres = [ph.sb(net.n('hres'), [128, 1024], F32) for _ in range(2)]
        pre = [ph.sb(net.n('pre'), [128, 1024], F32) for _ in range(2)]
        h1 = [ph.sb(net.n('h1'), [128, 1024], F32) for _ in range(2)]
        hTf = ph.sb(net.n('hTf'), [128, 8, 128], F32)
        wr = ph.sb(net.n('wr'), [128, 8, 128], F32)
        if True:
          kb.dma('sp', wr[:, :, :], net.inp['w_router'][:, :, :])
        whi = ph.sb(net.n('whi'), [128, 8, 128], BF16)
        wlo = ph.sb(net.n('wlo'), [128, 8, 128], BF16)
        hlo = ph.sb(net.n('hlo'), [128, 8, 128], BF16)
        if True:
            kb.copy(whi[:, :, :], wr[:, :, :])
            kb.tt(wr[:, :, :], wr[:, :, :], whi[:, :, :], ALU.subtract)
            kb.copy(wlo[:, :, :], wr[:, :, :])
        rb = ph.sb(net.n('rb'), [128, 16], F32)
        if True:
          kb.dma('sp', rb[:, :], net.inp['router_bias'][:, :])
        R = {k: ph.sb(net.n('r' + k), [128, 16], F32) for k in ['lg', 'e', 'pr', 'sel', 'selm', 'oh1', 'oh2', 'gw']}
        r1 = ph.sb(net.n('r1'), [128, 16], F32)
        ps6 = ph.sb(net.n('ps6'), [128, 4, 6], F32)
        gs = ph.sb(net.n('gs'), [128, 4], F32)
        eq = ph.sb(net.n('eq'), [128, 4], F32)
        pen = ph.sb(net.n('pen'), [128, 4], F32)
        Gsrc = [net.dr['GA'], net.dr['GB'], net.dr['GC']]

        def router(pT, ti):
            for hf in range(2):
                kb.copy(hTf[:, hf * 4:hf * 4 + 4, :], pT[hf][:, :, :], eng='act')
            q_, r_ = divmod(ti, 8)
            hi_v = hT[q_][:, :, r_ * 128:(r_ + 1) * 128]
            kb.tt(hlo[:, :, :], hTf[:, :, :], hi_v, ALU.subtract)
            n_ = 0
            for kc in range(8):
                for a_, b_ in ((hT[q_][:, kc, r_ * 128:(r_ + 1) * 128], whi[:, kc, :]), (hT[q_][:, kc, r_ * 128:(r_ + 1) * 128], wlo[:, kc, :]), (hlo[:, kc, :], whi[:, kc, :])):
                    kb.mm(pr[:, :], a_, b_, start=(n_ == 0), stop=(n_ == 23))
                    n_ += 1
            lg = R['lg']
            kb.copy(lg[:, :], pr[:, 0:16])
            kb.reduce(r1[:, 0:1], lg[:, :], ALU.max)
            kb.ts(r1[:, 1:2], r1[:, 0:1], -1.0, ALU.mult)
            kb.act(R['e'][:, :], lg[:, :], AF.Exp, bias=r1[:, 1:2], accum=r1[:, 2:3])
            kb.op('dve', lambda E, o, i: E.reciprocal(out=o, in_=i), [r1[:, 3:4]], [r1[:, 2:3]])
            kb.ts(R['pr'][:, :], R['e'][:, :], r1[:, 3:4], ALU.mult)
            kb.tt(R['sel'][:, :], R['pr'][:, :], rb[:, :], ALU.add)
            s3 = R['sel'].t[:, :].rearrange("p (g e) -> p g e", e=4)
            k = 0
            for i in range(4):
                for j in range(i + 1, 4):
                    kb.tt(ps6[:, :, k], R['sel'].v(s3[:, :, i]), R['sel'].v(s3[:, :, j]), ALU.add)
                    k += 1
            kb.reduce(gs[:, :], ps6[:, :, :], ALU.max)
            kb.reduce(r1[:, 4:5], gs[:, :], ALU.max)
            kb.ts(eq[:, :], gs[:, :], r1[:, 4:5], ALU.is_ge)
            kb.ts(pen[:, :], eq[:, :], -1.0, ALU.add, 1e30, ALU.mult)
            m3 = R['selm'].t[:, :].rearrange("p (g e) -> p g e", e=4)
            kb.tt(R['selm'].v(m3), R['sel'].v(s3), eq.v(eq.t[:, :].unsqueeze(2).to_broadcast([128, 4, 4])), ALU.mult)
            kb.tt(R['selm'].v(m3), R['selm'].v(m3), pen.v(pen.t[:, :].unsqueeze(2).to_broadcast([128, 4, 4])), ALU.add)
            kb.reduce(r1[:, 5:6], R['selm'][:, :], ALU.max)
            kb.ts(R['oh1'][:, :], R['selm'][:, :], r1[:, 5:6], ALU.is_ge)
            kb.stt(R['selm'][:, :], R['oh1'][:, :], -1e30, R['selm'][:, :], ALU.mult, ALU.add)
            kb.reduce(r1[:, 6:7], R['selm'][:, :], ALU.max)
            kb.ts(R['oh2'][:, :], R['selm'][:, :], r1[:, 6:7], ALU.is_ge)
            kb.tt(R['oh1'][:, :], R['oh1'][:, :], R['oh2'][:, :], ALU.add)
            kb.tt(R['gw'][:, :], R['pr'][:, :], R['oh1'][:, :], ALU.mult)
            kb.reduce(r1[:, 7:8], R['gw'][:, :], ALU.add)
            kb.op('dve', lambda E, o, i: E.reciprocal(out=o, in_=i), [r1[:, 8:9]], [r1[:, 7:8]])
            kb.ts(CW[:, ti, :], R['gw'][:, :], r1[:, 8:9], ALU.mult)

        for tt in range(8):
            for b in range(3):
                kb.dma('sp', G[b][:, :, :], View(Gsrc[b], Gsrc[b].t[:, tt * 512:(tt + 1) * 512].rearrange("(m p) t -> p m t", p=128)))
            for s in range(4):
                ti = tt * 4 + s
                hr = hres[ti % 2]; pv = pre[ti % 2]; ho = h1[ti % 2]
                kb.dma('sp', hr[:, :], hsrc[ti * 128:(ti + 1) * 128, :])
                for dh in range(2):
                    p = pm[dh]
                    n = 0
                    for b in range(3):
                        for kc in range(8):
                            kb.mm(p[:, :], G[b][:, kc, s * 128:(s + 1) * 128], wo[:, kc, dh * 512:(dh + 1) * 512], start=(n == 0), stop=(n == 23))
                            n += 1
                    kb.stt(pv[:, dh * 512:(dh + 1) * 512], hr[:, dh * 512:(dh + 1) * 512], ALPHA, p[:, :], ALU.mult, ALU.add)
                layernorm_tile(net, ph, T, pv[:, :], gB[:, :], bB[:, :], ho[:, :])
                emit_h_tile(net, ph, T, ho[:, :], ti, hdst, hT, C['ident_f'], router=router)


def phase_moe(net, l, hT, C, CW, hsrc, hdst, last, out_dst):
    kb = net.kb
    wg_d = net.inp['w_gate']; wu_d = net.inp['w_up']; wd_d = net.inp['w_down']
    with Phase(kb) as ph:
        net.cur_in = net.inp['ln2_g']; gB = bcast_load(net, ph, 'g2B', net.inp['ln2_g'].t[l])
        net.cur_in = net.inp['ln2_b']; bB = bcast_load(net, ph, 'b2B', net.inp['ln2_b'].t[l])
        T = ln_scratch(net, ph)
        T['pT'] = [ph.ps(net.n('pT'), [128, 4, 128], F32) for _ in range(2)]
        pg = [ph.ps(net.n('pg'), [128, 512], F32) for _ in range(2)]
        pu = [ph.ps(net.n('pu'), [128, 512], F32) for _ in range(2)]
        pd = [ph.ps(net.n('pd'), [128, 512], F32) for _ in range(2)]
        pd = pd + [Sub(b_, b_.t[:, :, :].rearrange("p c t -> p (c t)")) for b_ in T['pT']]
        npd = 0
        deferred = []
        acc = ph.sb(net.n('acc'), [128, 8, 1024], F32)
        wgb = [ph.sb(net.n('wgb'), [128, 8, 512], BF16) for _ in range(2)]
        wub = [ph.sb(net.n('wub'), [128, 8, 512], BF16) for _ in range(2)]
        wdb = [ph.sb(net.n('wdb'), [128, 4, 1024], BF16) for _ in range(2)]
        stg = [ph.sb(net.n('stg'), [128, 4, 512], F32) for _ in range(3)]
        hid = [ph.sb(net.n('hid'), [128, 4, 512], BF16) for _ in range(2)]
        sl = [ph.sb(net.n('sl'), [128, 512], F32) for _ in range(2)]
        hres = [ph.sb(net.n('hres'), [128, 1024], F32) for _ in range(1)]
        h2 = [ph.sb(net.n('h2'), [128, 1024], F32) for _ in range(1)]
        ns = 0
        hT_new = hT
        def load_expert(e):
            nonlocal ns
            wgt = wgb[e % 2]; wut = wub[e % 2]; wdt = wdb[e % 2]
            gv = wg_d.t[l, e].rearrange("(kc p) n -> p kc n", p=128)
            uv = wu_d.t[l, e].rearrange("(kc p) n -> p kc n", p=128)
            dv = wd_d.t[l, e].rearrange("(kc p) n -> p kc n", p=128)
            for hh in range(2):
                s_ = stg[ns % 3]; ns += 1
                kb.dma('sp', s_[:, :, :], View(wg_d, gv[:, hh * 4:(hh + 1) * 4, :]))
                kb.copy(wgt[:, hh * 4:(hh + 1) * 4, :], s_[:, :, :], eng='pool')
            for hh in range(2):
                s_ = stg[ns % 3]; ns += 1
                kb.dma('sp', s_[:, :, :], View(wu_d, uv[:, hh * 4:(hh + 1) * 4, :]))
                kb.copy(wut[:, hh * 4:(hh + 1) * 4, :], s_[:, :, :], eng='pool')
            pend = []
            for hh in range(2):
                s_ = stg[ns % 3]; ns += 1
                kb.dma('sp', s_[:, :, :], View(wd_d, dv[:, :, hh * 512:(hh + 1) * 512]))
                pend.append((wdt, hh, s_))
            return pend

        def cast_d(pend):
            for wdt, hh, s_ in pend:
                kb.copy(wdt[:, :, hh * 512:(hh + 1) * 512], s_[:, :, :], eng='act')
        seq = [(q, e) for q in range(4) for e in range(16)]
        cast_d(load_expert(0))
        for si, (q, e) in enumerate(seq):
            if True:
                wgt = wgb[e % 2]; wut = wub[e % 2]; wdt = wdb[e % 2]
                pend = load_expert(seq[si + 1][1]) if si + 1 < len(seq) else []
                for tt in range(2):
                    hd = hid[tt % 2]
                    rhs = lambda kc: hT[q][:, kc, tt * 512:(tt + 1) * 512]
                    for f in range(4):
                        g_ = pg[f % 2]; u_ = pu[f % 2]; s2 = sl[f % 2]
                        for kc in range(8):
                            kb.mm(g_[:, :], wgt[:, kc, f * 128:(f + 1) * 128], rhs(kc), start=(kc == 0), stop=(kc == 7))
                        for kc in range(8):
                            kb.mm(u_[:, :], wut[:, kc, f * 128:(f + 1) * 128], rhs(kc), start=(kc == 0), stop=(kc == 7))
                        kb.act(s2[:, :], g_[:, :], AF.Silu)
                        kb.tt(hd[:, f, :], u_[:, :], s2[:, :], ALU.mult)
                    if tt == 1:
                        cast_d(pend)
                    if deferred:
                        deferred.pop()()

                    def down(q=q, e=e, tt=tt, hd=hd, wdt=wdt):
                        nonlocal npd
                        for s in range(4):
                            tl = tt * 4 + s
                            ti = q * 8 + tl
                            for dh in range(2):
                                p = pd[npd % 4]; npd += 1
                                for f in range(4):
                                    kb.mm(p[:, :], hd[:, f, s * 128:(s + 1) * 128], wdt[:, f, dh * 512:(dh + 1) * 512], start=(f == 0), stop=(f == 3))
                                a = acc[:, tl, dh * 512:(dh + 1) * 512]
                                if e == 0:
                                    kb.ts(a, p[:, :], CW[:, ti, e:e + 1], ALU.mult)
                                else:
                                    kb.stt(a, p[:, :], CW[:, ti, e:e + 1], a, ALU.mult, ALU.add)
                    deferred.append(down)
                if e == 15:
                    deferred.pop()()
            for tl in (range(8) if e == 15 else ()):
                ti = q * 8 + tl
                hr = hres[0]; pv = hres[0]; ho = h2[0]
                kb.dma('sp', hr[:, :], hsrc[ti * 128:(ti + 1) * 128, :])
                kb.stt(pv[:, :], hr[:, :], ALPHA, acc[:, tl, :], ALU.mult, ALU.add)
                layernorm_tile(net, ph, T, pv[:, :], gB[:, :], bB[:, :], ho[:, :])
                if last:
                    kb.dma('pool', out_dst[ti * 128:(ti + 1) * 128, :], ho[:, :])
                else:
                    emit_h_tile(net, ph, T, ho[:, :], ti, hdst, hT, C['ident_f'])
GROUPS = ((128, 1), (512, 4), (2048, 16))


def phase_attn(net, l, C):
    kb = net.kb
    zT = net.dr['zT']; YC = net.dr['YC']
    with Phase(kb) as ph:
        C2 = ph.sb(net.n('C2'), [128, S], BF16)
        S2 = ph.sb(net.n('S2'), [128, S], BF16)
        with Phase(kb) as p2:
            pi_ = p2.sb(net.n('posi'), [128, 512], I32)
            pf = p2.sb(net.n('posf'), [128, 512], F32)
            uf = p2.sb(net.n('uf'), [128, 512], F32)
            ui = p2.sb(net.n('ui'), [128, 512], I32)
            fr = p2.sb(net.n('fr'), [128, 512], F32)
            pos = net.inp['positions']
            for c in range(8):
                kb.dma('sp', pi_[:, :], View(pos, pos.t[c * 512:(c + 1) * 512].partition_broadcast(128)))
                kb.copy(pf[:, :], pi_[:, :])
                for tab, sh in ((S2, 0.0), (C2, 0.25)):
                    kb.ts(uf[:, :], pf[:, :], C['invf'][:, 0:1], ALU.mult, sh, ALU.add)
                    kb.copy(ui[:, :], uf[:, :])
                    kb.copy(fr[:, :], ui[:, :])
                    kb.tt(fr[:, :], uf[:, :], fr[:, :], ALU.subtract)
                    kb.act(tab[:, c * 512:(c + 1) * 512], fr[:, :], AF.Sin, scale=6.28318)
        q = ph.sb(net.n('q'), [128, S], BF16)
        k = ph.sb(net.n('k'), [128, S], BF16)
        v = ph.sb(net.n('v'), [128, S], BF16)
        qr = q; kr = k
        VT = ph.sb(net.n('VT'), [128, 32, 128], BF16)
        OG = [[ph.sb(net.n('OG'), [65, S], F32) for _ in range(2)] for _ in range(3)]
        t1 = ph.sb(net.n('t1'), [128, 512], F32)
        t2 = ph.sb(net.n('t2'), [128, 512], F32)
        KA = 4
        lrow = ph.sb(net.n('lrow'), [65, 6, 512], F32)
        wrow = ph.sb(net.n('wrow'), [65, 3, 512], BF16)
        ycs = ph.sb(net.n('ycs'), [64, 512], F32)
        ycb = ph.sb(net.n('ycb'), [64, 512], BF16)
        pf_ = [ph.ps(net.n('pf'), [128, 512], F32) for _ in range(KA)]
        ps_pt_ = [ph.ps(net.n('ps_pt'), [128, 2, 128], BF16) for _ in range(KA)]
        ps_s_ = [Sub(b, b.t[:, 0:256]) for b in pf_]
        ps_o_ = [Sub(b, b.t[:, 256:320]) for b in pf_]
        ps_t_ = [Sub(b, b.t[0:65, 320:448]) for b in pf_]
        ps_rot = pf_[0]
        ps_bc = Sub(pf_[1], pf_[1].t[0:64, :])
        ps_vt = Sub(ps_pt_[0], ps_pt_[0].t[:, 0, :])
        sm_ = [ph.sb(net.n('sm'), [128, 256], F32) for _ in range(KA)]
        pb_ = [ph.sb(net.n('pb'), [128, 256], BF16) for _ in range(KA)]
        PT_ = [ph.sb(net.n('PT'), [128, 2, 128], BF16) for _ in range(KA)]
        aug_ = [ph.sb(net.n('aug'), [128, 65], F32) for _ in range(KA)]
        r1_ = [ph.sb(net.n('ar1'), [128, 8], F32) for _ in range(KA)]
        for hp in range(4):
            for g, (window, d) in enumerate(GROUPS):
                base = O_ATTN + g * 512 + hp * 128
                kb.dma('sp', q[:, :], zT[base:base + 128, :])
                kb.dma('sp', k[:, :], zT[base + 1536:base + 1536 + 128, :])
                kb.dma('sp', v[:, :], zT[base + 3072:base + 3072 + 128, :])
                for src, dst in ((q, qr), (k, kr)):
                    for c in range(8):
                        sl_ = slice(c * 512, (c + 1) * 512)
                        kb.mm(ps_rot[:, :], C['PT'][:, :], src[:, sl_])
                        kb.tt(t1[:, :], ps_rot[:, :], S2[:, sl_], ALU.mult)
                        kb.tt(t2[:, :], src[:, sl_], C2[:, sl_], ALU.mult, eng='pool')
                        kb.tt(dst[:, sl_], t1[:, :], t2[:, :], ALU.add)
                Lr = S // d
                nb = Lr // 128

                def toks(r, n0, cnt):
                    a = r + d * n0 * 128
                    return slice(a, a + d * 128 * cnt - (d - 1), d)
                for r in range(d):
                    for n in range(nb):
                        kb.transpose(ps_vt[:, :], v[:, toks(r, n, 1)], C['ident_b'][:, :])
                        kb.copy(VT[:, r * nb + n, :], ps_vt[:, :], eng='act')
                def block_gen(item, slot, g=g, d=d, nb=nb, toks=toks):
                    hd, r, n = item
                    rows = slice(hd * 64, hd * 64 + 64)
                    og = OG[g][hd]
                    sm = sm_[slot]; pb = pb_[slot]; PT = PT_[slot]; aug = aug_[slot]; r1 = r1_[slot]
                    ps_s = ps_s_[slot]; ps_pt = ps_pt_[slot]; ps_o = ps_o_[slot]; ps_t = ps_t_[slot]
                    bi = r * nb + n
                    if n == 0:
                        kb.mm(ps_s[:, 128:256], qr[rows, toks(r, n, 1)], kr[rows, toks(r, n, 1)])
                        kb.stt(sm[:, 128:256], ps_s[:, 128:256], 0.125, C['amask'][:, 128:256], ALU.mult, ALU.add)
                        kb.memset(sm[:, 0:128], -1e30)
                    else:
                        kb.mm(ps_s[:, :], qr[rows, toks(r, n, 1)], kr[rows, toks(r, n - 1, 2)])
                        kb.stt(sm[:, :], ps_s[:, :], 0.125, C['amask'][:, :], ALU.mult, ALU.add)
                    yield
                    kb.reduce(r1[:, 0:1], sm[:, :], ALU.max)
                    kb.ts(r1[:, 1:2], r1[:, 0:1], -1.0, ALU.mult)
                    kb.act(pb[:, :], sm[:, :], AF.Exp, bias=r1[:, 1:2], accum=r1[:, 2:3])
                    yield
                    for j in range(2):
                        kb.transpose(ps_pt[:, j, :], pb[:, j * 128:(j + 1) * 128], C['ident_b'][:, :])
                    kb.copy(PT[:, :, :], ps_pt[:, :, :])
                    yield
                    if n == 0:
                        kb.mm(ps_o[:, :], PT[:, 1, :], VT[:, bi, rows])
                    else:
                        kb.mm(ps_o[:, :], PT[:, 0, :], VT[:, bi - 1, rows], start=True, stop=False)
                        kb.mm(ps_o[:, :], PT[:, 1, :], VT[:, bi, rows], start=False, stop=True)
                    kb.op('dve', lambda E, o, i: E.reciprocal(out=o, in_=i), [r1[:, 3:4]], [r1[:, 2:3]])
                    kb.ts(aug[:, 0:64], ps_o[:, :], r1[:, 3:4], ALU.mult)
                    kb.act(r1[:, 4:5], r1[:, 2:3], AF.Ln)
                    kb.tt(aug[:, 64:65], r1[:, 4:5], r1[:, 0:1], ALU.add)
                    yield
                    kb.transpose(ps_t[:, :], aug[:, :], C['ident_f'][:, :])
                    kb.copy(og[:, toks(r, n, 1)], ps_t[:, :], eng='act')
                interleave([(hd, r, n) for hd in range(2) for r in range(d) for n in range(nb)], block_gen, KA, 5)
            for hd in range(2):
                for c in range(8):
                    sl_ = slice(c * 512, (c + 1) * 512)
                    L0 = OG[0][hd][64:65, sl_]; L1 = OG[1][hd][64:65, sl_]; L2 = OG[2][hd][64:65, sl_]
                    m = lrow[64:65, 0, :]
                    kb.tt(m, L0, L1, ALU.max)
                    kb.tt(m, m, L2, ALU.max)
                    for g, Lg in enumerate((L0, L1, L2)):
                        kb.tt(lrow[64:65, 1 + g, :], Lg, m, ALU.subtract)
                        kb.act(lrow[64:65, 1 + g, :], lrow[64:65, 1 + g, :], AF.Exp)
                    den = lrow[64:65, 4, :]
                    kb.tt(den, lrow[64:65, 1, :], lrow[64:65, 2, :], ALU.add)
                    kb.tt(den, den, lrow[64:65, 3, :], ALU.add)
                    kb.op('dve', lambda E, o, i: E.reciprocal(out=o, in_=i), [lrow[64:65, 5, :]], [den])
                    for g in range(3):
                        kb.tt(wrow[64:65, g, :], lrow[64:65, 1 + g, :], lrow[64:65, 5, :], ALU.mult)
                    for g in range(3):
                        kb.mm(ps_bc[:, :], C['ones_b'][64:65, 0:64], wrow[64:65, g, :])
                        if g == 0:
                            kb.tt(ycs[:, :], ps_bc[:, :], OG[g][hd][0:64, sl_], ALU.mult)
                        else:
                            kb.tt(t1[0:64, :], ps_bc[:, :], OG[g][hd][0:64, sl_], ALU.mult)
                            kb.tt(ycs[:, :], ycs[:, :], t1[0:64, :], ALU.add)
                    kb.copy(ycb[:, :], ycs[:, :], eng='act')
                    kb.dma('pool', YC[hp * 128 + hd * 64:hp * 128 + hd * 64 + 64, sl_], ycb[:, :])
    with Phase(kb) as ph:
        pc = load_w_bf16(net, ph, net.inp['p_c'], net.inp['p_c'].t[l], 4, 1024, 'pc')
        T = bo_scratch(net, ph, 512, npp=1)
        yc = ph.sb(net.n('yc'), [128, 4, 512], BF16)
        for tt in range(8):
            kb.dma('sp', yc[:, :, :], View(YC, YC.t[:, tt * 512:(tt + 1) * 512].rearrange("(m p) t -> p m t", p=128)))
            branch_out(net, T, pc, [yc[:, kc, :] for kc in range(4)], O_GATE + 2048, net.dr['GC'], tt * 512, 512)
C0 = float(np.exp(-0.5))
GN_EPS = 1e-5 * 64
TT = 256


def phase_rwkv(net, l, vec, C):
    kb = net.kb
    zT = net.dr['zT']; VF = net.dr['VF']
    with Phase(kb) as ph:
        pbw = load_w_bf16(net, ph, net.inp['p_b'], net.inp['p_b'].t[l], 8, 1024, 'pbw')
        wa2 = ph.sb(net.n('wa2'), [128, 1024], BF16)
        g2a = ph.sb(net.n('g2a'), [128, 1024], BF16)
        g2b = ph.sb(net.n('g2b'), [32, 1024], BF16)
        v2 = ph.sb(net.n('v2'), [32, 1024], BF16)
        with Phase(kb) as p2:
            st = p2.sb(net.n('lst'), [128, 1024], F32)
            kb.dma('sp', st[0:64, :], net.inp['rwkv_w2'][l])
            kb.dma('sp', st[64:128, :], net.inp['rwkv_a2'][l])
            kb.copy(wa2[:, :], st[:, :])
            st2 = p2.sb(net.n('lst2'), [128, 1024], F32)
            kb.dma('sp', st2[:, :], net.inp['rwkv_g2'][l, 0:128, :])
            kb.copy(g2a[:, :], st2[:, :])
            st3 = p2.sb(net.n('lst3'), [32, 1024], F32)
            kb.dma('sp', st3[:, :], net.inp['rwkv_g2'][l, 128:160, :])
            kb.copy(g2b[:, :], st3[:, :])
            if l == 1:
                st4 = p2.sb(net.n('lst4'), [32, 1024], F32)
                kb.dma('sp', st4[:, :], net.inp['rwkv_v2'][0])
                kb.copy(v2[:, :], st4[:, :])
        KR = 2
        W = TT + 1
        zin = {k_: ph.sb(net.n('z' + k_), [128, 8, W], BF16) for k_ in 'rkv'}
        zwa = ph.sb(net.n('zwa'), [128, W], BF16)
        zg1 = ph.sb(net.n('zg1'), [128, W], BF16)
        zg2 = ph.sb(net.n('zg2'), [32, W], BF16)
        uv1 = ph.sb(net.n('uv1'), [32, TT], BF16)
        ft = {k_: ph.sb(net.n('ft' + k_), [128, TT], F32) for k_ in ['d', 'wa', 'g1', 'g2']}
        bt16 = {k_: ph.sb(net.n('bt' + k_), [128, TT], BF16) for k_ in ['wa', 'g1']}
        bg2 = ph.sb(net.n('bg2'), [32, TT], BF16)
        yb = ph.sb(net.n('yb'), [128, 8, TT], BF16)
        STf = [ph.sb(net.n('STf'), [128, 128], F32) for _ in range(8)]
        STb = [ph.sb(net.n('STb'), [128, 128], BF16) for _ in range(8)]
        for j in range(8):
            kb.memset(STf[j][:, :], 0.0)
            kb.memset(STb[j][:, :], 0.0)

        def mkset():
            X = {}
            X['vf_t'] = ph.sb(net.n('vf_t'), [128, TT], BF16)
            X['f'] = {k_: ph.sb(net.n('f' + k_), [128, TT], F32) for k_ in
                      ['d', 'r', 'k', 'v', 'sg', 'a', 'g', 's', 'kk', 'kkn', 'k2', 'tmp', 'cs', 'e', 'ka', 'y', 'yc', 'bon']}
            X['b16'] = {k_: ph.sb(net.n('b' + k_), [128, TT], BF16) for k_ in ['sq', 'rk']}
            X['ARt'] = ph.sb(net.n('ARt'), [128, 4, 192], BF16)
            for k_ in ('Bt', 'Kt', 'Bb', 'Kb', 'Vb'):
                X[k_] = ph.sb(net.n(k_), [128, 4, 128], BF16)
            for k_ in ('ARt', 'Bt', 'Kt', 'Bb', 'Kb', 'Vb'):
                kb.memset(X[k_][:, :, :], 0.0)
            X['AB'] = ph.sb(net.n('AB'), [128, 4, 192], BF16)
            X['AK'] = ph.sb(net.n('AK'), [128, 4, 192], BF16)
            for k_ in ('Ui', 'Li', 'Gi'):
                X[k_] = [ph.sb(net.n(k_), [128, 4, 128], BF16) for _ in range(2)]
            for k_ in ('Vtm', 'Bbtm', 'Kbtm'):
                X[k_] = ph.sb(net.n(k_), [128, 4, 128], BF16)
            X['RHS'] = ph.sb(net.n('RHS'), [128, 128], BF16)
            X['SA'] = ph.sb(net.n('SA'), [128, 128], BF16)
            X['Wc'] = ph.sb(net.n('Wc'), [128, 4], F32)
            P = [ph.ps(net.n('P'), [128, 512], F32) for _ in range(4)]
            X['P'] = P
            X['pA'] = lambda c: View(P[c // 2], P[c // 2].t[:, (c % 2) * 256:(c % 2) * 256 + 192])
            X['pA2'] = lambda h2: View(P[h2], P[h2].t[:, :].rearrange("p (c t) -> p c t", t=256)[:, :, 0:192])
            X['pB'] = Sub(P[0], P[0].t[:, :].rearrange("p (c t) -> p c t", t=128))
            X['pC'] = Sub(P[1], P[1].t[:, :].rearrange("p (c t) -> p c t", t=128))
            X['pD'] = Sub(P[2], P[2].t[:, :].rearrange("p (c t) -> p c t", t=128))
            X['pS'] = Sub(P[3], P[3].t[:, 0:256])
            X['pTr'] = Sub(P[3], P[3].t[:, 256:512].bitcast(BF16).rearrange("p (c t) -> p c t", t=128))
            return X
        sets = [mkset() for _ in range(KR)]
        T = {'gt': ph.sb(net.n('gt'), [128, 8, TT], BF16), 'sg': ph.sb(net.n('sg'), [128, TT], F32),
             'go': ph.sb(net.n('go'), [128, 8, TT], BF16), 'pp': [sets[0]['P'][2]]}

        def rows(off, a, b):
            return View(zT, zT.t[off:off + 1024, a:b].rearrange("(m p) t -> p m t", p=128))

        def vc(col, j=0):
            return vec[:, col + j:col + j + 1]

        def shift(dst, src, mu, d_):
            P_ = src.ap.shape[0]
            dd = d_[0:P_, :]
            kb.tt(dd, View(src.buf, src.ap[:, 0:TT]), View(src.buf, src.ap[:, 1:W]), ALU.subtract, eng='pool')
            kb.stt(dst, dd, mu, View(src.buf, src.ap[:, 1:W]), ALU.mult, ALU.add)

        def bd_write(dst, c_lo, src_fn):
            for hd in range(2):
                rs_ = slice(hd * 64, hd * 64 + 64)
                src_fn(rs_, dst[rs_, :, c_lo + hd * 64:c_lo + hd * 64 + 64])

        def v3(b, rs_=slice(0, 128)):
            return b.v(b.t[rs_, :].rearrange("p (c t) -> p c t", t=64))

        def body(ti, j, X):
            t0 = ti * TT
            f = X['f']; b16 = X['b16']; vf_t = X['vf_t']
            ARt = X['ARt']; Bt = X['Bt']; Kt = X['Kt']; Bb = X['Bb']; Kb = X['Kb']; Vb = X['Vb']
            AB = X['AB']; AK = X['AK']; Ui = X['Ui']; Li = X['Li']; Gi = X['Gi']
            Vtm = X['Vtm']; Bbtm = X['Bbtm']; Kbtm = X['Kbtm']; RHS = X['RHS']; SA = X['SA']; Wc = X['Wc']
            pA = X['pA']; pA2 = X['pA2']; pB = X['pB']; pC = X['pC']; pD = X['pD']; pS = X['pS']; pTr = X['pTr']
            cs_ = slice(j * 128, (j + 1) * 128)
            shift(f['r'][:, :], zin['r'][:, j, :], vc(V_MUR, j), f['d'])
            shift(f['k'][:, :], zin['k'][:, j, :], vc(V_MUK, j), f['d'])
            shift(f['v'][:, :], zin['v'][:, j, :], vc(V_MUV, j), f['d'])
            yield
            kb.mm(pS[:, :], wa2[0:64, cs_], bt16['wa'][0:64, :])
            kb.act(f['sg'][:, :], pS[:, :], AF.Sigmoid, bias=vc(V_W0, j))
            kb.mm(pS[:, :], wa2[64:128, cs_], bt16['wa'][64:128, :])
            kb.act(f['a'][:, :], pS[:, :], AF.Sigmoid, bias=vc(V_A0, j))
            yield
            kb.mm(pS[:, :], g2a[:, cs_], bt16['g1'][:, :], start=True, stop=False)
            kb.mm(pS[:, :], g2b[:, cs_], bg2[:, :], start=False, stop=True)
            kb.copy(f['g'][:, :], pS[:, :], eng='act')
            if l == 1:
                kb.mm(pS[:, :], v2[:, cs_], uv1[:, :])
                kb.act(f['s'][:, :], pS[:, :], AF.Sigmoid, bias=vc(V_V0, j))
                kb.dma('sp', vf_t[:, :], VF[cs_, t0:t0 + TT])
                kb.tt(f['tmp'][:, :], vf_t[:, :], f['v'][:, :], ALU.subtract)
                kb.tt(f['tmp'][:, :], f['tmp'][:, :], f['s'][:, :], ALU.mult)
                kb.tt(f['v'][:, :], f['v'][:, :], f['tmp'][:, :], ALU.add)
            else:
                kb.copy(vf_t[:, :], f['v'][:, :], eng='act')
                kb.dma('pool', VF[cs_, t0:t0 + TT], vf_t[:, :])
            yield
            kb.ts(f['kk'][:, :], f['k'][:, :], vc(V_KK, j), ALU.mult)
            kb.tt(b16['sq'][:, :], f['kk'][:, :], f['kk'][:, :], ALU.mult)
            kb.mm(pS[:, :], C['bones_b'][:, :], b16['sq'][:, :])
            kb.ts(f['tmp'][:, :], pS[:, :], 1e-24, ALU.max)
            kb.act(f['tmp'][:, :], f['tmp'][:, :], AF.Sqrt)
            kb.op('dve', lambda E, o, i: E.reciprocal(out=o, in_=i), [f['tmp'][:, :]], [f['tmp'][:, :]])
            kb.tt(f['kkn'][:, :], f['kk'][:, :], f['tmp'][:, :], ALU.mult)
            yield
            kb.ts(f['tmp'][:, :], f['a'][:, :], vc(V_KA, j), ALU.mult, vc(V_OMKA, j), ALU.add)
            kb.tt(f['k2'][:, :], f['k'][:, :], f['tmp'][:, :], ALU.mult)
            kb.tt(f['ka'][:, :], f['kkn'][:, :], f['a'][:, :], ALU.mult)
            kb.scan(f['cs'][:, :], C['cmask'][:, :], f['sg'][:, :], 0.0, ALU.mult, ALU.add)
            cs3 = v3(f['cs'])
            yield
            kb.tt(f['tmp'][:, :], f['cs'][:, :], f['sg'][:, :], ALU.subtract)
            kb.act(f['e'][:, :], f['tmp'][:, :], AF.Exp, scale=-C0)
            kb.tt(f['tmp'][:, :], f['kkn'][:, :], f['e'][:, :], ALU.mult)
            bd_write(ARt, 0, lambda rs_, o: kb.ts(o, v3(f['tmp'], rs_), -1.0, ALU.mult))
            kb.act(f['e'][:, :], f['cs'][:, :], AF.Exp, scale=-C0)
            kb.tt(ARt.v(ARt.t[:, :, 128:192]), v3(f['r']), v3(f['e']), ALU.mult)
            yield
            kb.act(f['e'][:, :], f['cs'][:, :], AF.Exp, scale=C0)
            bd_write(Bt, 0, lambda rs_, o: kb.tt(o, v3(f['ka'], rs_), v3(f['e'], rs_), ALU.mult))
            bd_write(Kt, 0, lambda rs_, o: kb.tt(o, v3(f['k2'], rs_), v3(f['e'], rs_), ALU.mult, eng='pool'))
            yield
            kb.tt(v3(f['tmp']), f['cs'].v(cs3.ap[:, :, 63:64].to_broadcast([128, 4, 64])), cs3, ALU.subtract)
            kb.act(f['e'][:, :], f['tmp'][:, :], AF.Exp, scale=-C0)
            kb.act(Wc[:, :], f['cs'].v(cs3.ap[:, :, 63]), AF.Exp, scale=-C0)
            bd_write(Bb, 0, lambda rs_, o: kb.tt(o, v3(f['ka'], rs_), v3(f['e'], rs_), ALU.mult))
            bd_write(Kb, 0, lambda rs_, o: kb.tt(o, v3(f['k2'], rs_), v3(f['e'], rs_), ALU.mult, eng='pool'))
            bd_write(Vb, 0, lambda rs_, o: kb.copy(o, v3(f['v'], rs_), eng='act'))
            yield
            mab2 = C['m_ab'].v(C['m_ab'].t[:, :].unsqueeze(1).to_broadcast([128, 2, 192]))
            for c in range(4):
                kb.mm(pA(c), Bt[:, c, :], ARt[:, c, :])
            for h2 in range(2):
                kb.tt(AB[:, 2 * h2:2 * h2 + 2, :], pA2(h2), mab2, ALU.mult)
            yield
            for c in range(4):
                kb.mm(pA(c), Kt[:, c, :], ARt[:, c, :])
            for h2 in range(2):
                kb.tt(AK[:, 2 * h2:2 * h2 + 2, :], pA2(h2), mab2, ALU.mult)
            yield
            for c in range(4):
                kb.mm(pB[:, c, :], ARt[:, c, 0:128], Bt[:, c, :])
            kb.tt(Li[0][:, :, :], pB[:, :, :], C['m_l'].v(C['m_l'].t[:, :].unsqueeze(1).to_broadcast([128, 4, 128])), ALU.mult)
            kb.copy(Ui[0][:, :, :], AB[:, :, 0:128], eng='pool')
            kb.tt(Gi[0][:, :, :], AB[:, :, 0:128], C['ident_b'].v(C['ident_b'].t[:, :].unsqueeze(1).to_broadcast([128, 4, 128])), ALU.add, eng='pool')
            yield
            cu, cl_, cg = 0, 0, 0
            for lev in range(5):
                Uo, Lo, Go = Ui[cu], Li[cl_], Gi[cg]
                Un, Ln, Gn = Ui[1 - cu], Li[1 - cl_], Gi[1 - cg]
                for c in range(4):
                    kb.mm(pB[:, c, :], Uo[:, c, :], Lo[:, c, :])
                if lev < 4:
                    for c in range(4):
                        kb.mm(pC[:, c, :], Lo[:, c, :], Uo[:, c, :])
                kb.copy(Ln[:, :, :], pB[:, :, :], eng='act')
                if lev < 4:
                    kb.copy(Un[:, :, :], pC[:, :, :], eng='dve')
                yield
                for c in range(4):
                    kb.mm(pD[:, c, :], Ln[:, c, :], Go[:, c, :])
                kb.tt(Gn[:, :, :], pD[:, :, :], Go[:, :, :], ALU.add)
                cu, cl_, cg = 1 - cu, 1 - cl_, 1 - cg
                yield
            G = Gi[cg]
            for src, dst in ((Vb, Vtm), (Bb, Bbtm), (Kb, Kbtm)):
                for c in range(4):
                    kb.transpose(pTr[:, c, :], src[:, c, :], C['ident_b'][:, :])
                kb.copy(dst[:, :, :], pTr[:, :, :], eng='act')
            yield
            for c in range(4):
                kb.mm(pD[:, 0, :], ARt[:, c, 0:128], STb[j][:, :], start=True, stop=False)
                kb.mm(pD[:, 0, :], AK[:, c, 0:128], Vtm[:, c, :], start=False, stop=True)
                kb.copy(RHS[:, :], pD[:, 0, :], eng='act')
                yield
                kb.mm(pD[:, 1, :], G[:, c, :], RHS[:, :])
                kb.copy(SA[:, :], pD[:, 1, :], eng='act')
                yield
                kb.mm(pS[:, c * 64:(c + 1) * 64], STb[j][:, :], ARt[:, c, 128:192], start=True, stop=False)
                kb.mm(pS[:, c * 64:(c + 1) * 64], SA[:, :], AB[:, c, 128:192], start=False, stop=False)
                kb.mm(pS[:, c * 64:(c + 1) * 64], Vtm[:, c, :], AK[:, c, 128:192], start=False, stop=True)
                kb.mm(pD[:, 2, :], Bbtm[:, c, :], SA[:, :], start=True, stop=False)
                kb.mm(pD[:, 2, :], Kbtm[:, c, :], Vtm[:, c, :], start=False, stop=True)
                kb.stt(STf[j][:, :], STf[j][:, :], Wc[:, c:c + 1], pD[:, 2, :], ALU.mult, ALU.add)
                kb.copy(STb[j][:, :], STf[j][:, :], eng='act')
                yield
            kb.copy(f['y'][:, :], pS[:, :], eng='act')
            kb.mm(pS[:, :], C['bmean_f'][:, :], f['y'][:, :])
            kb.tt(f['yc'][:, :], f['y'][:, :], pS[:, :], ALU.subtract)
            kb.tt(f['tmp'][:, :], f['yc'][:, :], f['yc'][:, :], ALU.mult)
            yield
            kb.mm(pS[:, :], C['bmean_f'][:, :], f['tmp'][:, :])
            kb.ts(f['tmp'][:, :], pS[:, :], GN_EPS, ALU.add)
            kb.act(f['tmp'][:, :], f['tmp'][:, :], AF.Sqrt)
            kb.op('dve', lambda E, o, i: E.reciprocal(out=o, in_=i), [f['tmp'][:, :]], [f['tmp'][:, :]])
            kb.tt(f['yc'][:, :], f['yc'][:, :], f['tmp'][:, :], ALU.mult)
            kb.ts(f['yc'][:, :], f['yc'][:, :], vc(V_LG, j), ALU.mult, vc(V_LB, j), ALU.add)
            yield
            kb.tt(f['tmp'][:, :], f['r'][:, :], f['k2'][:, :], ALU.mult)
            kb.ts(b16['rk'][:, :], f['tmp'][:, :], vc(V_RK, j), ALU.mult)
            kb.mm(pS[:, :], C['bones_b'][:, :], b16['rk'][:, :])
            kb.tt(f['bon'][:, :], pS[:, :], f['v'][:, :], ALU.mult)
            kb.tt(f['yc'][:, :], f['yc'][:, :], f['bon'][:, :], ALU.add)
            kb.tt(yb[:, j, :], f['yc'][:, :], f['g'][:, :], ALU.mult)
        NST = 38

        for ti in range(S // TT):
            t0 = ti * TT
            for k_, off in (('r', O_RWKV), ('k', O_RWKV + 1024), ('v', O_RWKV + 2048)):
                if ti == 0:
                    kb.memset(zin[k_][:, :, 0:1], 0.0)
                    kb.dma('sp', zin[k_][:, :, 1:W], rows(off, 0, TT))
                else:
                    kb.dma('sp', zin[k_][:, :, :], rows(off, t0 - 1, t0 + TT))
            o2 = O_RWKV + 3072
            for tl, a_, n_ in ((zwa, o2, 128), (zg1, o2 + 128, 128), (zg2, o2 + 256, 32)):
                if ti == 0:
                    kb.memset(tl[0:n_, 0:1], 0.0)
                    kb.dma('sp', tl[0:n_, 1:W], zT[a_:a_ + n_, 0:TT])
                else:
                    kb.dma('sp', tl[0:n_, :], zT[a_:a_ + n_, t0 - 1:t0 + TT])
            shift(ft['wa'][:, :], zwa[:, :], vc(V_MUWA), ft['d'])
            shift(ft['g1'][:, :], zg1[:, :], vc(V_MUG), ft['d'])
            shift(ft['g2'][0:32, :], zg2[0:32, :], vec[0:32, V_MUG + 1:V_MUG + 2], ft['d'])
            kb.act(bt16['wa'][0:64, :], ft['wa'][0:64, :], AF.Tanh)
            kb.copy(bt16['wa'][64:128, :], ft['wa'][64:128, :])
            kb.act(bt16['g1'][:, :], ft['g1'][:, :], AF.Sigmoid)
            kb.act(bg2[:, :], ft['g2'][0:32, :], AF.Sigmoid)
            if l == 1:
                kb.dma('sp', uv1[:, :], zT[O_UV1:O_UV1 + 32, t0:t0 + TT])
            interleave(range(8), lambda j, slot, ti=ti: body(ti, j, sets[slot]), KR, NST)
            branch_out(net, T, pbw, [yb[:, kc, :] for kc in range(8)], O_GATE + 1024, net.dr['GB'], t0, TT)
def host_consts():
    cf = {}
    cf['ident_f'] = np.eye(128, dtype=np.float32)
    p = np.arange(128)
    inv_freq = 500000.0 ** (-np.arange(0, 16, 2, dtype=np.float32) / 16)
    invf = np.where((p % 64) < 16, inv_freq[(p % 64) % 8] / (2 * np.pi), 0.0).astype(np.float32)
    cf['invf'] = invf[:, None]
    qi = np.arange(128)[:, None]; kj = np.arange(256)[None, :]
    cf['amask'] = np.where((kj >= qi) & (kj <= qi + 128), 0.0, -1e30).astype(np.float32)
    blk = (p[:, None] // 64) == (p[None, :] // 64)
    cf['bmean_f'] = (blk / 64.0).astype(np.float32)
    cm = np.ones((128, 256), np.float32); cm[:, ::64] = 0.0
    cf['cmask'] = cm
    s_ = (p % 64)[:, None]
    mab = np.zeros((128, 192), np.float32)
    mab[:, :128] = blk & (s_ < (p % 64)[None, :])
    mab[:, 128:] = (s_ <= np.arange(64)[None, :])
    cf['m_ab'] = mab
    cf['m_l'] = (blk & (s_ > (p % 64)[None, :])).astype(np.float32)
    cb = {}
    cb['ident_b'] = np.eye(128, dtype=np.float32)
    PT = np.zeros((128, 128), np.float32)
    for h in range(2):
        for i in range(8):
            PT[h * 64 + i + 8, h * 64 + i] = -1.0
            PT[h * 64 + i, h * 64 + i + 8] = 1.0
    cb['PT'] = PT
    cb['ones_b'] = np.ones((128, 128), np.float32)
    cb['bones_b'] = blk.astype(np.float32)
    return cf, cb


CF_KEYS = ['ident_f', 'invf', 'amask', 'bmean_f', 'cmask', 'm_ab', 'm_l']
CB_KEYS = ['ident_b', 'PT', 'ones_b', 'bones_b']


def host_vecs(I):
    out = np.zeros((L, 128, NV), np.float32)

    def pc(v):
        return np.ascontiguousarray(v.reshape(-1, 128).T)
    for l in range(L):
        o = out[l]
        for j in range(3):
            o[:, V_CONV + j * 8:V_CONV + j * 8 + 8] = pc(I['conv_w'][l, j])
        mu = I['rwkv_mu'][l]
        o[:, V_MUR:V_MUR + 8] = pc(mu[0:1024]); o[:, V_MUK:V_MUK + 8] = pc(mu[1024:2048]); o[:, V_MUV:V_MUV + 8] = pc(mu[2048:3072])
        o[:, V_MUWA] = mu[3072:3200]
        o[:, V_MUG] = mu[3200:3328]
        o[0:32, V_MUG + 1] = mu[3328:3360]
        o[:, V_W0:V_W0 + 8] = pc(I['rwkv_w0'][l]); o[:, V_A0:V_A0 + 8] = pc(I['rwkv_a0'][l])
        if l >= 1:
            o[:, V_V0:V_V0 + 8] = pc(I['rwkv_v0'][l - 1])
        o[:, V_KK:V_KK + 8] = pc(I['rwkv_k_k'][l]); o[:, V_KA:V_KA + 8] = pc(I['rwkv_k_a'][l])
        o[:, V_RK:V_RK + 8] = pc(I['rwkv_r_k'][l].reshape(-1))
        o[:, V_LG:V_LG + 8] = pc(I['rwkv_lnx_g'][l]); o[:, V_LB:V_LB + 8] = pc(I['rwkv_lnx_b'][l])
    return out


IN_SHAPES = {'x': [S, D], 'positions': [S], 'ln_in_g': [D], 'ln_in_b': [D], 'w_in': [L, D, NIN], 'rwkv_w2': [L, 64, D], 'rwkv_a2': [L, 64, D],
             'rwkv_g2': [L, 160, D], 'rwkv_v1': [1, D, 32], 'rwkv_v2': [1, 32, D], 'p_a': [L, D, D], 'p_b': [L, D, D], 'p_c': [L, 512, D],
             'w_o': [L, D, D], 'ln1_g': [L, D], 'ln1_b': [L, D], 'w_router': [128, 8, 128], 'router_bias': [128, 16], 'w_gate': [L, 16, D, 512],
             'w_up': [L, 16, D, 512], 'w_down': [L, 16, 512, D], 'ln2_g': [L, D], 'ln2_b': [L, D], 'vecs': [L, 128, NV]}


def build(debug=(), stop_after=None, skip=()):
    net = Net(debug=debug)
    kb = net.kb
    for k_, sh in IN_SHAPES.items():
        net.din(k_, sh, I32 if k_ == 'positions' else F32)
    cfh, cbh = host_consts()
    for k_ in CF_KEYS:
        net.din('c_' + k_, list(cfh[k_].shape), F32)
    for k_ in CB_KEYS:
        net.din('c_' + k_, list(cbh[k_].shape), BF16)
    out = net.nc.dram_tensor('out', [S, D], F32, kind="ExternalOutput")
    out = Buf(kb, out.ap(), dram=True)
    net.dscr('hA', [S, D], F32); net.dscr('hB', [S, D], F32); net.dscr('zT', [NZ, S], BF16)
    for k_ in ('GA', 'GB', 'GC', 'VF'):
        net.dscr(k_, [D, S], BF16)
    net.dscr('YC', [512, S], BF16)
    with Phase(kb) as g:
        C = {}
        for k_ in CF_KEYS:
            C[k_] = g.sb(net.n('k' + k_), list(cfh[k_].shape), F32)
            kb.dma('sp', C[k_][:, :], net.inp['c_' + k_][:, :])
        for k_ in CB_KEYS:
            C[k_] = g.sb(net.n('k' + k_), list(cbh[k_].shape), BF16)
            kb.dma('sp', C[k_][:, :], net.inp['c_' + k_][:, :])
        vec = []
        for l in range(L):
            v_ = g.sb(net.n('vec'), [128, NV], F32)
            kb.dma('sp', v_[:, :], net.inp['vecs'][l])
            kb.ts(v_[:, V_OMKA:V_OMKA + 8], v_[:, V_KA:V_KA + 8], -1.0, ALU.mult, 1.0, ALU.add)
            vec.append(v_)
        CW = g.sb(net.n('CW'), [128, 32, 16], F32)

        def mk_hT(ph):
            return [ph.sb(net.n('hT'), [128, 8, 1024], BF16) for _ in range(4)]
        with Phase(kb) as A:
            hT = mk_hT(A)
            phase0(net, hT, C)
            phase1(net, 0, hT)
        for l in range(L):
            if stop_after == ('p1', l):
                return net
            phase_conv(net, l, vec[l])
            if stop_after == ('conv', l):
                return net
            if 'rwkv' not in skip:
                phase_rwkv(net, l, vec[l], C)
            if stop_after == ('rwkv', l):
                return net
            if 'attn' not in skip:
                phase_attn(net, l, C)
            if stop_after == ('attn', l):
                return net
            with Phase(kb) as B:
                hT = mk_hT(B)
                phase_mix(net, l, vec[l], hT, C, net.dr['hA'], net.dr['hB'], CW)
                if stop_after == ('mix', l):
                    return net
                phase_moe(net, l, hT, C, CW, net.dr['hB'], net.dr['hA'], l == L - 1, out)
                if stop_after == ('moe', l):
                    return net
                if l < L - 1:
                    phase1(net, l + 1, hT)
    return net


def make_inputs(I, b):
    cfh, cbh = host_consts()
    m = {}
    for k_ in IN_SHAPES:
        if k_ == 'vecs':
            continue
        a = I[k_]
        if k_ in ('x', 'positions'):
            a = a[b]
        if k_ == 'w_router':
            a = np.concatenate([a.reshape(8, 128, 16).transpose(1, 0, 2), np.zeros((128, 8, 112), np.float32)], axis=2)
        if k_ == 'router_bias':
            a = np.broadcast_to(a[None, :], (128, 16))
        m[k_] = np.ascontiguousarray(a)
    m['vecs'] = host_vecs(I)
    for k_ in CF_KEYS:
        m['c_' + k_] = cfh[k_]
    for k_ in CB_KEYS:
        m['c_' + k_] = cbh[k_].astype(ml_dtypes.bfloat16)
    return m


def kernel(**inputs):
    I = {k_: np.asarray(v_) for k_, v_ in inputs.items()}
    net = build()
    in_maps = [make_inputs(I, b) for b in range(8)]
    res = run_bass_kernel_spmd(net.nc, in_maps, core_ids=list(range(8)))
    return np.stack([np.asarray(r["out"], dtype=np.float32) for r in res.results], axis=0)
```

```python
import ml_dtypes
import numpy as np
from contextlib import ExitStack
import concourse.bass as bass
import concourse.mybir as mybir
from concourse.bass_utils import run_bass_kernel_spmd

F32 = mybir.dt.float32
BF16 = mybir.dt.bfloat16
I32 = mybir.dt.int32
AF = mybir.ActivationFunctionType
ALU = mybir.AluOpType
AX = mybir.AxisListType


class Sem:
    def __init__(self, h, is_dma):
        self.h = h
        self.is_dma = is_dma
        self.total = 0


class View:
    def __init__(self, buf, ap):
        self.buf = buf
        self.ap = ap


class Buf:
    def __init__(self, kb, t, dram=False):
        self.kb = kb
        self.t = t
        self.dram = dram
        self.w = {}
        self.r = {}
        self.ds = {}

    def __getitem__(self, idx):
        return View(self, self.t[idx])

    def v(self, ap):
        return View(self, ap)

    def dsem(self, q):
        if q not in self.ds:
            self.ds[q] = self.kb.new_dma_sem(q)
        return self.ds[q]


class KB:
    def __init__(self, nc):
        self.nc = nc
        self.engs = {'pe': nc.tensor, 'act': nc.scalar, 'dve': nc.vector, 'pool': nc.gpsimd, 'sp': nc.sync}
        self.esem = {}
        for k in ['pe', 'act', 'dve', 'pool']:
            self.esem[k] = Sem(nc.alloc_semaphore('es_' + k), False)
        self.seen = {k: {} for k in self.engs}
        self.allsems = list(self.esem.values())
        self.nds = 0
        self.ninst = 0
        self.free_ds = {'sp': [], 'pool': [], 'act': []}

    def new_dma_sem(self, q):
        if self.free_ds[q]:
            return self.free_ds[q].pop()
        s = Sem(self.nc.alloc_semaphore('ds%d' % self.nds), True)
        self.nds += 1
        self.allsems.append(s)
        return s

    def _wait(self, eng, need):
        E = self.engs[eng]
        seen = self.seen[eng]
        for s, c in need.items():
            if s.is_dma:
                c = s.total
            elif eng == 'pe' and s is self.esem['pe']:
                continue
            if seen.get(s, 0) < c:
                E.wait_ge(s.h, c)
                seen[s] = c
                self.ninst += 1

    def _deps(self, reads, writes):
        need = {}

        def add(d):
            for s, c in d.items():
                if need.get(s, 0) < c:
                    need[s] = c
        for b in reads:
            add(b.w)
        for b in writes:
            add(b.w)
            add(b.r)
        return need

    def _stamp(self, reads, writes, s, c):
        for b in reads:
            if b.r.get(s, 0) < c:
                b.r[s] = c
        for b in writes:
            if b.dram:
                if b.w.get(s, 0) < c:
                    b.w[s] = c
            else:
                b.w = {s: c}
                b.r = {}

    def op(self, eng, fn, outs, ins):
        reads = [v.buf for v in ins]
        writes = [v.buf for v in outs]
        self._wait(eng, self._deps(reads, writes))
        ins_ = fn(self.engs[eng], *[v.ap for v in outs], *[v.ap for v in ins])
        s = self.esem[eng]
        s.total += 1
        ins_.then_inc(s.h, 1)
        self.ninst += 1
        self._stamp(reads, writes, s, s.total)
        return ins_

    def dma(self, q, out, in_, **kw):
        reads = [in_.buf]
        writes = [out.buf]
        self._wait(q, self._deps(reads, writes))
        sb = out.buf if not out.buf.dram else in_.buf
        s = sb.dsem(q)
        ins_ = self.engs[q].dma_start(out=out.ap, in_=in_.ap, **kw)
        s.total += 16
        ins_.then_inc(s.h, 16)
        self.ninst += 1
        self._stamp(reads, writes, s, s.total)
        return ins_

    def barrier(self):
        need = {s: s.total for s in self.allsems if s.total > 0}
        for eng in self.engs:
            self._wait(eng, need)

    def mm(self, out, lhsT, rhs, start=True, stop=True):
        return self.op('pe', lambda E, o, a, b: E.matmul(o, lhsT=a, rhs=b, start=start, stop=stop), [out], [lhsT, rhs])

    def transpose(self, out, in_, ident):
        return self.op('pe', lambda E, o, a, b: E.transpose(o, a, b), [out], [in_, ident])

    def act(self, out, in_, func, bias=None, scale=None, accum=None, eng='act'):
        outs = [out] + ([accum] if accum is not None else [])
        ins = [in_]
        kw = {}
        if isinstance(bias, View):
            ins.append(bias)
        if isinstance(scale, View):
            ins.append(scale)

        def fn(E, *aps):
            aps = list(aps)
            o = aps.pop(0)
            if accum is not None:
                kw['accum_out'] = aps.pop(0)
            i = aps.pop(0)
            if isinstance(bias, View):
                kw['bias'] = aps.pop(0)
            elif bias is not None:
                kw['bias'] = bias
            if isinstance(scale, View):
                kw['scale'] = aps.pop(0)
            elif scale is not None:
                kw['scale'] = scale
            return E.activation(out=o, in_=i, func=func, **kw)
        return self.op(eng, fn, outs, ins)

    def tt(self, out, a, b, op, eng='dve'):
        return self.op(eng, lambda E, o, x, y: E.tensor_tensor(out=o, in0=x, in1=y, op=op), [out], [a, b])

    def ts(self, out, a, s1, op0, s2=None, op1=None, eng='dve', accum=None):
        ins = [a]
        outs = [out] + ([accum] if accum is not None else [])
        if isinstance(s1, View):
            ins.append(s1)
        if isinstance(s2, View):
            ins.append(s2)

        def fn(E, *aps):
            aps = list(aps)
            o = aps.pop(0)
            kw = {}
            if accum is not None:
                kw['accum_out'] = aps.pop(0)
            x = aps.pop(0)
            a1 = aps.pop(0) if isinstance(s1, View) else s1
            a2 = aps.pop(0) if isinstance(s2, View) else s2
            if op1 is None:
                return E.tensor_scalar(out=o, in0=x, scalar1=a1, scalar2=None, op0=op0, **kw)
            return E.tensor_scalar(out=o, in0=x, scalar1=a1, scalar2=a2, op0=op0, op1=op1, **kw)
        return self.op(eng, fn, outs, ins)

    def stt(self, out, a, s, b, op0, op1, eng='dve'):
        ins = [a, b]
        if isinstance(s, View):
            ins.append(s)

        def fn(E, o, x, y, *rest):
            sc = rest[0] if rest else s
            return E.scalar_tensor_tensor(out=o, in0=x, scalar=sc, in1=y, op0=op0, op1=op1)
        return self.op(eng, fn, [out], ins)

    def copy(self, out, in_, eng='dve'):
        if eng == 'act':
            return self.op('act', lambda E, o, i: E.copy(out=o, in_=i), [out], [in_])
        return self.op(eng, lambda E, o, i: E.tensor_copy(out=o, in_=i), [out], [in_])

    def memset(self, out, val, eng='pool'):
        return self.op(eng, lambda E, o: E.memset(o, val), [out], [])

    def reduce(self, out, in_, op, axis=AX.X, eng='dve'):
        return self.op(eng, lambda E, o, i: E.tensor_reduce(out=o, in_=i, axis=axis, op=op), [out], [in_])

    def scan(self, out, d0, d1, initial, op0, op1):
        return self.op('dve', lambda E, o, x, y: E.tensor_tensor_scan(out=o, data0=x, data1=y, initial=initial, op0=op0, op1=op1), [out], [d0, d1])


class Phase:
    def __init__(self, kb):
        self.kb = kb
        self.st = ExitStack()
        self.bufs = []

    def __enter__(self):
        self.st.__enter__()
        return self

    def __exit__(self, *a):
        self.kb.barrier()
        for b in self.bufs:
            for q_, s_ in b.ds.items():
                self.kb.free_ds[q_].append(s_)
            b.ds = {}
        return self.st.__exit__(*a)

    def sb(self, name, shape, dt):
        t = self.st.enter_context(self.kb.nc.sbuf_tensor(name, list(shape), dt))
        b = Buf(self.kb, t)
        self.bufs.append(b)
        return b

    def ps(self, name, shape, dt=F32):
        t = self.st.enter_context(self.kb.nc.psum_tensor(name, list(shape), dt))
        return Buf(self.kb, t)

    def bank(self, name, dt=F32):
        n = 512 if dt == F32 else 1024
        return self.st.enter_context(self.kb.nc.psum_tensor(name, [128, n], dt))

    def sub(self, ap):
        return Buf(self.kb, ap)


class Sub:
    def __init__(self, buf, ap):
        self.buf = buf
        self.ap = ap

    def __getitem__(self, idx):
        return View(self.buf, self.ap[idx])


def interleave(items, make_gen, K, nstage):
    it = iter(items)
    slots = [None] * K
    start_at = [(k * nstage) // K for k in range(K)]
    rnd = 0
    exhausted = False
    while True:
        alive = False
        for k in range(K):
            if slots[k] is None and not exhausted and rnd >= start_at[k]:
                try:
                    slots[k] = make_gen(next(it), k)
                except StopIteration:
                    exhausted = True
            if slots[k] is not None:
                alive = True
                try:
                    next(slots[k])
                except StopIteration:
                    slots[k] = None
        if not alive and exhausted:
            break
        rnd += 1

S = 4096; D = 1024; NIN = 14112; L = 2
ALPHA = (2 * L) ** 0.25
LN_EPS = 1e-5
O_GATE = 0; O_CONV = 3072; O_RWKV = 6144; O_ATTN = 9504
O_UV1 = NIN
NZ = NIN + 32


def cdiv(a, b):
    return (a + b - 1) // b


class Net:
    def __init__(self, debug=()):
        nc = self.nc = bass.Bass("TRN2", target_bir_lowering=False)
        self.kb = KB(nc)
        self.debug = debug
        self.inp = {}
        self.dr = {}
        self.uid = 0

    def din(self, name, shape, dt=F32):
        t = self.nc.dram_tensor(name, list(shape), dt, kind="ExternalInput")
        b = Buf(self.kb, t.ap(), dram=True)
        self.inp[name] = b
        return b

    def dscr(self, name, shape, dt):
        kind = "ExternalOutput" if name in self.debug else "Internal"
        t = self.nc.dram_tensor(name, list(shape), dt, kind=kind)
        b = Buf(self.kb, t.ap(), dram=True)
        self.dr[name] = b
        return b

    def n(self, s):
        self.uid += 1
        return "%s_%d" % (s, self.uid)


def layernorm_tile(net, ph, T, xin, gB, bB, out):
    kb = net.kb
    st = T['st']; mv = T['mv']; sd = T['sd']
    for c in range(2):
        kb.op('dve', lambda E, o, i: E.bn_stats(out=o, in_=i), [st[:, c, :]], [View(xin.buf, xin.ap[:, c * 512:(c + 1) * 512])])
    kb.op('dve', lambda E, o, i: E.bn_aggr(out=o, in_=i), [mv[:, :]], [st[:, :, :]])
    kb.ts(sd[:, 0:1], mv[:, 1:2], LN_EPS, ALU.add)
    kb.act(sd[:, 1:2], sd[:, 0:1], AF.Sqrt)
    kb.op('dve', lambda E, o, i: E.reciprocal(out=o, in_=i), [sd[:, 2:3]], [sd[:, 1:2]])
    kb.ts(out, xin, mv[:, 0:1], ALU.subtract, sd[:, 2:3], ALU.mult)
    kb.tt(out, out, gB, ALU.mult, eng='pool')
    kb.tt(out, out, bB, ALU.add, eng='pool')


def ln_scratch(net, ph):
    return {'st': ph.sb(net.n('lnst'), [128, 2, 6], F32), 'mv': ph.sb(net.n('lnmv'), [128, 2], F32),
            'sd': ph.sb(net.n('lnsd'), [128, 4], F32)}


def bcast_load(net, ph, name, src_ap):
    b = ph.sb(net.n(name), [128, 1024], F32)
    net.kb.dma('sp', b[:, :], View(net.cur_in, src_ap.partition_broadcast(128)))
    return b
def emit_h_tile(net, ph, T, hv, ti, hdst, hT, ident_f, router=None):
    kb = net.kb
    if hdst is not None:
        kb.dma('pool', hdst[ti * 128:(ti + 1) * 128, :], hv)
    if hT is None:
        return
    pT = T['pT']
    q, r = divmod(ti, 8)
    for hf in range(2):
        for kc in range(4):
            kb.transpose(pT[hf][:, kc, :], View(hv.buf, hv.ap[:, (hf * 4 + kc) * 128:(hf * 4 + kc + 1) * 128]), ident_f[:, :])
        kb.copy(hT[q][:, hf * 4:hf * 4 + 4, r * 128:(r + 1) * 128], pT[hf][:, :, :], eng='act')
    if router is not None:
        router(pT, ti)


def phase0(net, hT, C):
    kb = net.kb
    with Phase(kb) as ph:
        net.cur_in = net.inp['ln_in_g']
        gB = bcast_load(net, ph, 'gB', net.inp['ln_in_g'].t[:])
        net.cur_in = net.inp['ln_in_b']
        bB = bcast_load(net, ph, 'bB', net.inp['ln_in_b'].t[:])
        T = ln_scratch(net, ph)
        T['pT'] = [ph.ps(net.n('pT'), [128, 4, 128], F32) for _ in range(2)]
        xs = [ph.sb(net.n('x'), [128, 1024], F32) for _ in range(2)]
        hs = [ph.sb(net.n('h'), [128, 1024], F32) for _ in range(2)]
        x = net.inp['x']
        for ti in range(32):
            xt = xs[ti % 2]; ht = hs[ti % 2]
            kb.dma('sp', xt[:, :], x[ti * 128:(ti + 1) * 128, :])
            layernorm_tile(net, ph, T, xt[:, :], gB[:, :], bB[:, :], ht[:, :])
            emit_h_tile(net, ph, T, ht[:, :], ti, net.dr['hA'], hT, C['ident_f'])


def phase1(net, l, hT):
    kb = net.kb
    w_in = net.inp['w_in']
    zT = net.dr['zT']
    with Phase(kb) as ph:
        wst = [ph.sb(net.n('wst'), [128, 8, 128], F32) for _ in range(2)]
        wb = [ph.sb(net.n('wb'), [128, 8, 128], BF16) for _ in range(2)]
        zs = [ph.sb(net.n('zs'), [128, S], BF16) for _ in range(2)]
        pz = [ph.ps(net.n('pz'), [128, 512], F32) for _ in range(4)]
        blocks = [(c0, min(128, NIN - c0), None) for c0 in range(0, NIN, 128)]
        if l == 1:
            blocks.append((O_UV1, 32, 'v1'))
        wv = w_in.t[l].rearrange("(kc p) n -> p kc n", p=128)
        k = 0

        def prefetch(bi):
            c0, ncol, kind = blocks[bi]
            st = wst[bi % 2]; w = wb[bi % 2]
            if kind is None:
                kb.dma('sp', st[:, :, 0:ncol], View(w_in, wv[:, :, c0:c0 + ncol]))
            else:
                v1 = net.inp['rwkv_v1']
                kb.dma('sp', st[:, :, 0:ncol], View(v1, v1.t[0].rearrange("(kc p) n -> p kc n", p=128)))
            kb.copy(w[:, :, 0:ncol], st[:, :, 0:ncol], eng='pool')
        prefetch(0)
        for bi, (c0, ncol, kind) in enumerate(blocks):
            w = wb[bi % 2]; z = zs[bi % 2]
            if bi + 1 < len(blocks):
                prefetch(bi + 1)
            for tt in range(8):
                p = pz[k % 4]
                for kc in range(8):
                    kb.mm(p[0:ncol, :], w[:, kc, 0:ncol], hT[tt // 2][:, kc, (tt % 2) * 512:(tt % 2 + 1) * 512], start=(kc == 0), stop=(kc == 7))
                kb.copy(z[0:ncol, tt * 512:(tt + 1) * 512], p[0:ncol, :], eng=('act' if k % 2 == 0 else 'dve'))
                k += 1
            kb.dma('pool' if bi % 2 else 'sp', zT[c0:c0 + ncol, :], z[0:ncol, :])
NV = 123
V_CONV = 0; V_MUR = 24; V_MUK = 32; V_MUV = 40; V_MUWA = 48; V_MUG = 49; V_W0 = 51; V_A0 = 59; V_V0 = 67
V_KK = 75; V_KA = 83; V_RK = 91; V_LG = 99; V_LB = 107; V_OMKA = 115


def load_w_bf16(net, ph, src, ap, kc, ncol, name):
    kb = net.kb
    w = ph.sb(net.n(name), [128, kc, ncol], BF16)
    v = ap.rearrange("(kc p) n -> p kc n", p=128)
    with Phase(kb) as p2:
        st = [p2.sb(net.n('wstg'), [128, kc, 256], F32) for _ in range(2)]
        for i, c0 in enumerate(range(0, ncol, 256)):
            n = min(256, ncol - c0)
            kb.dma('sp', st[i % 2][:, :, 0:n], View(src, v[:, :, c0:c0 + n]))
            kb.copy(w[:, :, c0:c0 + n], st[i % 2][:, :, 0:n], eng='pool')
    return w


def branch_out(net, T, pw, rhs_list, goff, Gdst, t0, N, kp=128):
    kb = net.kb
    zT = net.dr['zT']
    gt = T['gt']; sg = T['sg']; go = T['go']; pp = T['pp']
    kb.dma('sp', gt[:, :, 0:N], View(zT, zT.t[goff:goff + 1024, t0:t0 + N].rearrange("(m p) t -> p m t", p=128)))
    for m in range(8):
        p = pp[m % len(pp)]
        for kc, r in enumerate(rhs_list):
            kb.mm(p[:, 0:N], pw[0:kp, kc, m * 128:(m + 1) * 128], r, start=(kc == 0), stop=(kc == len(rhs_list) - 1))
        kb.act(sg[:, 0:N], gt[:, m, 0:N], AF.Sigmoid)
        kb.tt(go[:, m, 0:N], p[:, 0:N], sg[:, 0:N], ALU.mult)
    kb.dma('pool', View(Gdst, Gdst.t[:, t0:t0 + N].rearrange("(m p) t -> p m t", p=128)), go[:, :, 0:N])


def bo_scratch(net, ph, N, npp=2):
    return {'gt': ph.sb(net.n('gt'), [128, 8, N], BF16), 'sg': ph.sb(net.n('sg'), [128, N], F32),
            'go': ph.sb(net.n('go'), [128, 8, N], BF16), 'pp': [ph.ps(net.n('pp'), [128, 512], F32) for _ in range(npp)]}


def phase_conv(net, l, vec):
    kb = net.kb
    zT = net.dr['zT']
    with Phase(kb) as ph:
        pa = load_w_bf16(net, ph, net.inp['p_a'], net.inp['p_a'].t[l], 8, 1024, 'pa')
        T = bo_scratch(net, ph, 512)
        cc = ph.sb(net.n('cc'), [128, 8, 514], BF16)
        chh = ph.sb(net.n('chh'), [128, 8, 514], BF16)
        cb = ph.sb(net.n('cb'), [128, 8, 512], BF16)
        yf = [ph.sb(net.n('yf'), [128, 514], F32) for _ in range(2)]
        of = [ph.sb(net.n('of'), [128, 512], F32) for _ in range(2)]
        u = ph.sb(net.n('u'), [128, 8, 512], BF16)

        def rows(off, a, b):
            return View(zT, zT.t[off:off + 1024, a:b].rearrange("(m p) t -> p m t", p=128))
        for tt in range(8):
            t0 = tt * 512
            if tt == 0:
                kb.memset(cc[:, :, 0:2], 0.0)
                kb.memset(chh[:, :, 0:2], 0.0)
                kb.dma('sp', cc[:, :, 2:514], rows(O_CONV + 1024, 0, 512))
                kb.dma('sp', chh[:, :, 2:514], rows(O_CONV + 2048, 0, 512))
            else:
                kb.dma('sp', cc[:, :, :], rows(O_CONV + 1024, t0 - 2, t0 + 512))
                kb.dma('sp', chh[:, :, :], rows(O_CONV + 2048, t0 - 2, t0 + 512))
            kb.dma('sp', cb[:, :, :], rows(O_CONV, t0, t0 + 512))
            for c in range(8):
                y = yf[c % 2]; o = of[c % 2]
                kb.tt(y[:, :], cc[:, c, :], chh[:, c, :], ALU.mult, eng='pool')
                kb.ts(o[:, :], y[:, 2:514], vec[:, V_CONV + 16 + c:V_CONV + 17 + c], ALU.mult)
                kb.stt(o[:, :], y[:, 1:513], vec[:, V_CONV + 8 + c:V_CONV + 9 + c], o[:, :], ALU.mult, ALU.add)
                kb.stt(o[:, :], y[:, 0:512], vec[:, V_CONV + c:V_CONV + 1 + c], o[:, :], ALU.mult, ALU.add)
                kb.tt(u[:, c, :], o[:, :], cb[:, c, :], ALU.mult, eng='pool')
            branch_out(net, T, pa, [u[:, c, :] for c in range(8)], O_GATE, net.dr['GA'], t0, 512)


def phase_mix(net, l, vec, hT, C, hsrc, hdst, CW):
    kb = net.kb
    with Phase(kb) as ph:
        wo = load_w_bf16(net, ph, net.inp['w_o'], net.inp['w_o'].t[l], 8, 1024, 'wo')
        net.cur_in = net.inp['ln1_g']; gB = bcast_load(net, ph, 'g1B', net.inp['ln1_g'].t[l])
        net.cur_in = net.inp['ln1_b']; bB = bcast_load(net, ph, 'b1B', net.inp['ln1_b'].t[l])
        T = ln_scratch(net, ph)
        T['pT'] = [ph.ps(net.n('pT'), [128, 4, 128], F32) for _ in range(2)]
        pm = [ph.ps(net.n('pm'), [128, 512], F32) for _ in range(2)]
        pr = ph.ps(net.n('pr'), [128, 128], F32)
        G = [ph.sb(net.n('G'), [128, 8, 512], BF16) for _ in range(3)]
        hres = [ph.sb(net.n('hres'), [128, 1024], F32) for _ in range(2)]
        pre = [ph.sb(net.n('pre'), [128, 1024], F32) for _ in range(2)]
        h1 = [ph.sb(net.n('h1'), [128, 1024], F32) for _ in range(2)]
        hTf = ph.sb(net.n('hTf'), [128, 8, 128], F32)
        wr = ph.sb(net.n('wr'), [128, 8, 128], F32)
        if True:
          kb.dma('sp', wr[:, :, :], net.inp['w_router'][:, :, :])
        whi = ph.sb(net.n('whi'), [128, 8, 128], BF16)
        wlo = ph.sb(net.n('wlo'), [128, 8, 128], BF16)
        hlo = ph.sb(net.n('hlo'), [128, 8, 128], BF16)
        if True:
            kb.copy(whi[:, :, :], wr[:, :, :])
            kb.tt(wr[:, :, :], wr[:, :, :], whi[:, :, :], ALU.subtract)
            kb.copy(wlo[:, :, :], wr[:, :, :])
        rb = ph.sb(net.n('rb'), [128, 16], F32)
        if True:
          kb.dma('sp', rb[:, :], net.inp['router_bias'][:, :])
        R = {k: ph.sb(net.n('r' + k), [128, 16], F32) for k in ['lg', 'e', 'pr', 'sel', 'selm', 'oh1', 'oh2', 'gw']}
        r1 = ph.sb(net.n('r1'), [128, 16], F32)
        ps6 = ph.sb(net.n('ps6'), [128, 4, 6], F32)
        gs = ph.sb(net.n('gs'), [128, 4], F32)
        eq = ph.sb(net.n('eq'), [128, 4], F32)
        pen = ph.sb(net.n('pen'), [128, 4], F32)
        Gsrc = [net.dr['GA'], net.dr['GB'], net.dr['GC']]

        def router(pT, ti):
            for hf in range(2):
                kb.copy(hTf[:, hf * 4:hf * 4 + 4, :], pT[hf][:, :, :], eng='act')
            q_, r_ = divmod(ti, 8)
            hi_v = hT[q_][:, :, r_ * 128:(r_ + 1) * 128]
            kb.tt(hlo[:, :, :], hTf[:, :, :], hi_v, ALU.subtract)
            n_ = 0
            for kc in range(8):
                for a_, b_ in ((hT[q_][:, kc, r_ * 128:(r_ + 1) * 128], whi[:, kc, :]), (hT[q_][:, kc, r_ * 128:(r_ + 1) * 128], wlo[:, kc, :]), (hlo[:, kc, :], whi[:, kc, :])):
                    kb.mm(pr[:, :], a_, b_, start=(n_ == 0), stop=(n_ == 23))
                    n_ += 1
            lg = R['lg']
            kb.copy(lg[:, :], pr[:, 0:16])
            kb.reduce(r1[:, 0:1], lg[:, :], ALU.max)
            kb.ts(r1[:, 1:2], r1[:, 0:1], -1.0, ALU.mult)
            kb.act(R['e'][:, :], lg[:, :], AF.Exp, bias=r1[:, 1:2], accum=r1[:, 2:3])
            kb.op('dve', lambda E, o, i: E.reciprocal(out=o, in_=i), [r1[:, 3:4]], [r1[:, 2:3]])
            kb.ts(R['pr'][:, :], R['e'][:, :], r1[:, 3:4], ALU.mult)
            kb.tt(R['sel'][:, :], R['pr'][:, :], rb[:, :], ALU.add)
            s3 = R['sel'].t[:, :].rearrange("p (g e) -> p g e", e=4)
            k = 0
            for i in range(4):
                for j in range(i + 1, 4):
                    kb.tt(ps6[:, :, k], R['sel'].v(s3[:, :, i]), R['sel'].v(s3[:, :, j]), ALU.add)
                    k += 1
            kb.reduce(gs[:, :], ps6[:, :, :], ALU.max)
            kb.reduce(r1[:, 4:5], gs[:, :], ALU.max)
            kb.ts(eq[:, :], gs[:, :], r1[:, 4:5], ALU.is_ge)
            kb.ts(pen[:, :], eq[:, :], -1.0, ALU.add, 1e30, ALU.mult)
            m3 = R['selm'].t[:, :].rearrange("p (g e) -> p g e", e=4)
            kb.tt(R['selm'].v(m3), R['sel'].v(s3), eq.v(eq.t[:, :].unsqueeze(2).to_broadcast([128, 4, 4])), ALU.mult)
            kb.tt(R['selm'].v(m3), R['selm'].v(m3), pen.v(pen.t[:, :].unsqueeze(2).to_broadcast([128, 4, 4])), ALU.add)
            kb.reduce(r1[:, 5:6], R['selm'][:, :], ALU.max)
            kb.ts(R['oh1'][:, :], R['selm'][:, :], r1[:, 5:6], ALU.is_ge)
            kb.stt(R['selm'][:, :], R['oh1'][:, :], -1e30, R['selm'][:, :], ALU.mult, ALU.add)
            kb.reduce(r1[:, 6:7], R['selm'][:, :], ALU.max)
            kb.ts(R['oh2'][:, :], R['selm'][:, :], r1[:, 6:7], ALU.is_ge)
            kb.tt(R['oh1'][:, :], R['oh1'][:, :], R['oh2'][:, :], ALU.add)
            kb.tt(R['gw'][:, :], R['pr'][:, :], R['oh1'][:, :], ALU.mult)
            kb.reduce(r1[:, 7:8], R['gw'][:, :], ALU.add)
            kb.op('dve', lambda E, o, i: E.reciprocal(out=o, in_=i), [r1[:, 8:9]], [r1[:, 7:8]])
            kb.ts(CW[:, ti, :], R['gw'][:, :], r1[:, 8:9], ALU.mult)

        for tt in range(8):
            for b in range(3):
                kb.dma('sp', G[b][:, :, :], View(Gsrc[b], Gsrc[b].t[:, tt * 512:(tt + 1) * 512].rearrange("(m p) t -> p m t", p=128)))
            for s in range(4):
                ti = tt * 4 + s
                hr = hres[ti % 2]; pv = pre[ti % 2]; ho = h1[ti % 2]
                kb.dma('sp', hr[:, :], hsrc[ti * 128:(ti + 1) * 128, :])
                for dh in range(2):
                    p = pm[dh]
                    n = 0
                    for b in range(3):
                        for kc in range(8):
                            kb.mm(p[:, :], G[b][:, kc, s * 128:(s + 1) * 128], wo[:, kc, dh * 512:(dh + 1) * 512], start=(n == 0), stop=(n == 23))
                            n += 1
                    kb.stt(pv[:, dh * 512:(dh + 1) * 512], hr[:, dh * 512:(dh + 1) * 512], ALPHA, p[:, :], ALU.mult, ALU.add)
                layernorm_tile(net, ph, T, pv[:, :], gB[:, :], bB[:, :], ho[:, :])
                emit_h_tile(net, ph, T, ho[:, :], ti, hdst, hT, C['ident_f'], router=router)


def phase_moe(net, l, hT, C, CW, hsrc, hdst, last, out_dst):
    kb = net.kb
    wg_d = net.inp['w_gate']; wu_d = net.inp['w_up']; wd_d = net.inp['w_down']
    with Phase(kb) as ph:
        net.cur_in = net.inp['ln2_g']; gB = bcast_load(net, ph, 'g2B', net.inp['ln2_g'].t[l])
        net.cur_in = net.inp['ln2_b']; bB = bcast_load(net, ph, 'b2B', net.inp['ln2_b'].t[l])
        T = ln_scratch(net, ph)
        T['pT'] = [ph.ps(net.n('pT'), [128, 4, 128], F32) for _ in range(2)]
        pg = [ph.ps(net.n('pg'), [128, 512], F32) for _ in range(2)]
        pu = [ph.ps(net.n('pu'), [128, 512], F32) for _ in range(2)]
        pd = [ph.ps(net.n('pd'), [128, 512], F32) for _ in range(2)]
        pd = pd + [Sub(b_, b_.t[:, :, :].rearrange("p c t -> p (c t)")) for b_ in T['pT']]
        npd = 0
        deferred = []
        acc = ph.sb(net.n('acc'), [128, 8, 1024], F32)
        wgb = [ph.sb(net.n('wgb'), [128, 8, 512], BF16) for _ in range(2)]
        wub = [ph.sb(net.n('wub'), [128, 8, 512], BF16) for _ in range(2)]
        wdb = [ph.sb(net.n('wdb'), [128, 4, 1024], BF16) for _ in range(2)]
        stg = [ph.sb(net.n('stg'), [128, 4, 512], F32) for _ in range(3)]
        hid = [ph.sb(net.n('hid'), [128, 4, 512], BF16) for _ in range(2)]
        sl = [ph.sb(net.n('sl'), [128, 512], F32) for _ in range(2)]
        hres = [ph.sb(net.n('hres'), [128, 1024], F32) for _ in range(1)]
        h2 = [ph.sb(net.n('h2'), [128, 1024], F32) for _ in range(1)]
        ns = 0
        hT_new = hT
        def load_expert(e):
            nonlocal ns
            wgt = wgb[e % 2]; wut = wub[e % 2]; wdt = wdb[e % 2]
            gv = wg_d.t[l, e].rearrange("(kc p) n -> p kc n", p=128)
            uv = wu_d.t[l, e].rearrange("(kc p) n -> p kc n", p=128)
            dv = wd_d.t[l, e].rearrange("(kc p) n -> p kc n", p=128)
            for hh in range(2):
                s_ = stg[ns % 3]; ns += 1
                kb.dma('sp', s_[:, :, :], View(wg_d, gv[:, hh * 4:(hh + 1) * 4, :]))
                kb.copy(wgt[:, hh * 4:(hh + 1) * 4, :], s_[:, :, :], eng='pool')
            for hh in range(2):
                s_ = stg[ns % 3]; ns += 1
                kb.dma('sp', s_[:, :, :], View(wu_d, uv[:, hh * 4:(hh + 1) * 4, :]))
                kb.copy(wut[:, hh * 4:(hh + 1) * 4, :], s_[:, :, :], eng='pool')
            pend = []
            for hh in range(2):
                s_ = stg[ns % 3]; ns += 1
                kb.dma('sp', s_[:, :, :], View(wd_d, dv[:, :, hh * 512:(hh + 1) * 512]))
                pend.append((wdt, hh, s_))
            return pend

        def cast_d(pend):
            for wdt, hh, s_ in pend:
                kb.copy(wdt[:, :, hh * 512:(hh + 1) * 512], s_[:, :, :], eng='act')
        seq = [(q, e) for q in range(4) for e in range(16)]
        cast_d(load_expert(0))
        for si, (q, e) in enumerate(seq):
            if True:
                wgt = wgb[e % 2]; wut = wub[e % 2]; wdt = wdb[e % 2]
                pend = load_expert(seq[si + 1][1]) if si + 1 < len(seq) else []
                for tt in range(2):
                    hd = hid[tt % 2]
                    rhs = lambda kc: hT[q][:, kc, tt * 512:(tt + 1) * 512]
                    for f in range(4):
                        g_ = pg[f % 2]; u_ = pu[f % 2]; s2 = sl[f % 2]
                        for kc in range(8):
                            kb.mm(g_[:, :], wgt[:, kc, f * 128:(f + 1) * 128], rhs(kc), start=(kc == 0), stop=(kc == 7))
                        for kc in range(8):
                            kb.mm(u_[:, :], wut[:, kc, f * 128:(f + 1) * 128], rhs(kc), start=(kc == 0), stop=(kc == 7))
                        kb.act(s2[:, :], g_[:, :], AF.Silu)
                        kb.tt(hd[:, f, :], u_[:, :], s2[:, :], ALU.mult)
                    if tt == 1:
                        cast_d(pend)
                    if deferred:
                        deferred.pop()()

                    def down(q=q, e=e, tt=tt, hd=hd, wdt=wdt):
                        nonlocal npd
                        for s in range(4):
                            tl = tt * 4 + s
                            ti = q * 8 + tl
                            for dh in range(2):
                                p = pd[npd % 4]; npd += 1
                                for f in range(4):
                                    kb.mm(p[:, :], hd[:, f, s * 128:(s + 1) * 128], wdt[:, f, dh * 512:(dh + 1) * 512], start=(f == 0), stop=(f == 3))
                                a = acc[:, tl, dh * 512:(dh + 1) * 512]
                                if e == 0:
                                    kb.ts(a, p[:, :], CW[:, ti, e:e + 1], ALU.mult)
                                else:
                                    kb.stt(a, p[:, :], CW[:, ti, e:e + 1], a, ALU.mult, ALU.add)
                    deferred.append(down)
                if e == 15:
                    deferred.pop()()
            for tl in (range(8) if e == 15 else ()):
                ti = q * 8 + tl
                hr = hres[0]; pv = hres[0]; ho = h2[0]
                kb.dma('sp', hr[:, :], hsrc[ti * 128:(ti + 1) * 128, :])
                kb.stt(pv[:, :], hr[:, :], ALPHA, acc[:, tl, :], ALU.mult, ALU.add)
                layernorm_tile(net, ph, T, pv[:, :], gB[:, :], bB[:, :], ho[:, :])
                if last:
                    kb.dma('pool', out_dst[ti * 128:(ti + 1) * 128, :], ho[:, :])
                else:
                    emit_h_tile(net, ph, T, ho[:, :], ti, hdst, hT, C['ident_f'])
GROUPS = ((128, 1), (512, 4), (2048, 16))


def phase_attn(net, l, C):
    kb = net.kb
    zT = net.dr['zT']; YC = net.dr['YC']
    with Phase(kb) as ph:
        C2 = ph.sb(net.n('C2'), [128, S], BF16)
        S2 = ph.sb(net.n('S2'), [128, S], BF16)
        with Phase(kb) as p2:
            pi_ = p2.sb(net.n('posi'), [128, 512], I32)
            pf = p2.sb(net.n('posf'), [128, 512], F32)
            uf = p2.sb(net.n('uf'), [128, 512], F32)
            ui = p2.sb(net.n('ui'), [128, 512], I32)
            fr = p2.sb(net.n('fr'), [128, 512], F32)
            pos = net.inp['positions']
            for c in range(8):
                kb.dma('sp', pi_[:, :], View(pos, pos.t[c * 512:(c + 1) * 512].partition_broadcast(128)))
                kb.copy(pf[:, :], pi_[:, :])
                for tab, sh in ((S2, 0.0), (C2, 0.25)):
                    kb.ts(uf[:, :], pf[:, :], C['invf'][:, 0:1], ALU.mult, sh, ALU.add)
                    kb.copy(ui[:, :], uf[:, :])
                    kb.copy(fr[:, :], ui[:, :])
                    kb.tt(fr[:, :], uf[:, :], fr[:, :], ALU.subtract)
                    kb.act(tab[:, c * 512:(c + 1) * 512], fr[:, :], AF.Sin, scale=6.28318)
        q = ph.sb(net.n('q'), [128, S], BF16)
        k = ph.sb(net.n('k'), [128, S], BF16)
        v = ph.sb(net.n('v'), [128, S], BF16)
        qr = q
        kr = ph.sb(net.n('kr'), [128, S], BF16)
        VT = ph.sb(net.n('VT'), [128, 32, 128], BF16)
        OG = [[ph.sb(net.n('OG'), [65, S], F32) for _ in range(2)] for _ in range(3)]
        t1_ = [ph.sb(net.n('t1'), [128, 512], F32) for _ in range(3)]
        t2_ = [ph.sb(net.n('t2'), [128, 512], F32) for _ in range(3)]
        t1 = t1_[0]
        KA = 4
        lrow_ = [ph.sb(net.n('lrow'), [65, 4, 512], F32) for _ in range(2)]
        wrow_ = [ph.sb(net.n('wrow'), [65, 3, 512], BF16) for _ in range(2)]
        ycs_ = [Sub(b_, b_.t[0:64, 1, :]) for b_ in lrow_]
        ycb_ = [Sub(b_, b_.t[0:64, 0, :]) for b_ in wrow_]
        pf_ = [ph.ps(net.n('pf'), [128, 512], F32) for _ in range(KA)]
        ps_pt_ = [ph.ps(net.n('ps_pt'), [128, 2, 128], BF16) for _ in range(KA)]
        ps_s_ = [Sub(b, b.t[:, 0:256]) for b in pf_]
        ps_o_ = [Sub(b, b.t[:, 256:320]) for b in pf_]
        ps_t_ = [Sub(b, b.t[0:65, 320:448]) for b in pf_]
        ps_rot = pf_[0]
        ps_bc = Sub(pf_[1], pf_[1].t[0:64, :])
        ps_vt = Sub(ps_pt_[0], ps_pt_[0].t[:, 0, :])
        sm_ = [ph.sb(net.n('sm'), [128, 256], F32) for _ in range(KA)]
        pb_ = [ph.sb(net.n('pb'), [128, 256], BF16) for _ in range(KA)]
        PT_ = [ph.sb(net.n('PT'), [128, 2, 128], BF16) for _ in range(KA)]
        aug_ = [ph.sb(net.n('aug'), [128, 65], F32) for _ in range(KA)]
        r1_ = [ph.sb(net.n('ar1'), [128, 8], F32) for _ in range(KA)]
        for hp in range(4):
            for g, (window, d) in enumerate(GROUPS):
                base = O_ATTN + g * 512 + hp * 128
                kb.dma('sp', q[:, :], zT[base:base + 128, :])
                kb.dma('sp', k[:, :], zT[base + 1536:base + 1536 + 128, :])
                kb.dma('sp', v[:, :], zT[base + 3072:base + 3072 + 128, :])
                Lr = S // d
                nb = Lr // 128

                def toks(r, n0, cnt, d=d):
                    a = r + d * n0 * 128
                    return slice(a, a + d * 128 * cnt - (d - 1), d)

                def pro_gen(item, kslot, nb=nb, toks=toks):
                    if item[0] == 'rope':
                        _, src, dst, c = item
                        sl_ = slice(c * 512, (c + 1) * 512)
                        pr_ = pf_[kslot]
                        kb.mm(pr_[:, :], C['PT'][:, :], src[:, sl_])
                        kb.tt(t2_[kslot][:, :], src[:, sl_], C2[:, sl_], ALU.mult, eng='pool')
                        yield
                        kb.tt(t1_[kslot][:, :], pr_[:, :], S2[:, sl_], ALU.mult)
                        yield
                        kb.tt(dst[:, sl_], t1_[kslot][:, :], t2_[kslot][:, :], ALU.add)
                    else:
                        _, r, n = item
                        pv_ = Sub(ps_pt_[kslot], ps_pt_[kslot].t[:, 0, :])
                        kb.transpose(pv_[:, :], v[:, toks(r, n, 1)], C['ident_b'][:, :])
                        yield
                        kb.copy(VT[:, r * nb + n, :], pv_[:, :], eng='act')
                        yield
                ropes = [('rope', src, dst, c) for src, dst in ((q, qr), (k, kr)) for c in range(8)]
                vts = [('vt', r, n) for r in range(d) for n in range(nb)]
                items = []
                for ii in range(16):
                    items += [ropes[ii], vts[2 * ii], vts[2 * ii + 1]]
                interleave(items, pro_gen, 3, 3)
                def block_gen(item, slot, g=g, d=d, nb=nb, toks=toks):
                    hd, r, n = item
                    rows = slice(hd * 64, hd * 64 + 64)
                    og = OG[g][hd]
                    sm = sm_[slot]; pb = pb_[slot]; PT = PT_[slot]; aug = aug_[slot]; r1 = r1_[slot]
                    ps_s = ps_s_[slot]; ps_pt = ps_pt_[slot]; ps_o = ps_o_[slot]; ps_t = ps_t_[slot]
                    bi = r * nb + n
                    if n == 0:
                        kb.mm(ps_s[:, 128:256], qr[rows, toks(r, n, 1)], kr[rows, toks(r, n, 1)])
                        kb.stt(sm[:, 128:256], ps_s[:, 128:256], 0.125, C['amask'][:, 128:256], ALU.mult, ALU.add)
                        kb.memset(sm[:, 0:128], -1e30)
                    else:
                        kb.mm(ps_s[:, :], qr[rows, toks(r, n, 1)], kr[rows, toks(r, n - 1, 2)])
                        kb.stt(sm[:, :], ps_s[:, :], 0.125, C['amask'][:, :], ALU.mult, ALU.add)
                    yield
                    kb.reduce(r1[:, 0:1], sm[:, :], ALU.max)
                    kb.ts(r1[:, 1:2], r1[:, 0:1], -1.0, ALU.mult)
                    kb.act(pb[:, :], sm[:, :], AF.Exp, bias=r1[:, 1:2], accum=r1[:, 2:3])
                    yield
                    for j in range(2):
                        kb.transpose(ps_pt[:, j, :], pb[:, j * 128:(j + 1) * 128], C['ident_b'][:, :])
                    kb.copy(PT[:, :, :], ps_pt[:, :, :])
                    yield
                    if n == 0:
                        kb.mm(ps_o[:, :], PT[:, 1, :], VT[:, bi, rows])
                    else:
                        kb.mm(ps_o[:, :], PT[:, 0, :], VT[:, bi - 1, rows], start=True, stop=False)
                        kb.mm(ps_o[:, :], PT[:, 1, :], VT[:, bi, rows], start=False, stop=True)
                    kb.op('dve', lambda E, o, i: E.reciprocal(out=o, in_=i), [r1[:, 3:4]], [r1[:, 2:3]])
                    kb.ts(aug[:, 0:64], ps_o[:, :], r1[:, 3:4], ALU.mult)
                    kb.act(r1[:, 4:5], r1[:, 2:3], AF.Ln)
                    kb.tt(aug[:, 64:65], r1[:, 4:5], r1[:, 0:1], ALU.add)
                    yield
                    kb.transpose(ps_t[:, :], aug[:, :], C['ident_f'][:, :])
                    kb.copy(og[:, toks(r, n, 1)], ps_t[:, :], eng='act')
                interleave([(hd, r, n) for hd in range(2) for r in range(d) for n in range(nb)], block_gen, KA, 5)
            def merge_gen(item, ks, hp=hp):
                hd, c = item
                lrow = lrow_[ks]; wrow = wrow_[ks]; ycs = ycs_[ks]; ycb = ycb_[ks]; tq = t1_[1 + ks]
                psb = Sub(pf_[1 + ks], pf_[1 + ks].t[0:64, :])
                sl_ = slice(c * 512, (c + 1) * 512)
                L0 = OG[0][hd][64:65, sl_]; L1 = OG[1][hd][64:65, sl_]; L2 = OG[2][hd][64:65, sl_]
                m = lrow[64:65, 0, :]
                kb.tt(m, L0, L1, ALU.max)
                kb.tt(m, m, L2, ALU.max)
                yield
                for g, Lg in enumerate((L0, L1, L2)):
                    kb.tt(lrow[64:65, 1 + g, :], Lg, m, ALU.subtract)
                    kb.act(lrow[64:65, 1 + g, :], lrow[64:65, 1 + g, :], AF.Exp)
                yield
                den = lrow[64:65, 0, :]
                kb.tt(den, lrow[64:65, 1, :], lrow[64:65, 2, :], ALU.add)
                kb.tt(den, den, lrow[64:65, 3, :], ALU.add)
                kb.op('dve', lambda E, o, i: E.reciprocal(out=o, in_=i), [den], [den])
                yield
                for g in range(3):
                    kb.tt(wrow[64:65, g, :], lrow[64:65, 1 + g, :], den, ALU.mult)
                yield
                for g in range(3):
                    kb.mm(psb[:, :], C['ones_b'][64:65, 0:64], wrow[64:65, g, :])
                    if g == 0:
                        kb.tt(ycs[:, :], psb[:, :], OG[g][hd][0:64, sl_], ALU.mult)
                    else:
                        kb.tt(tq[0:64, :], psb[:, :], OG[g][hd][0:64, sl_], ALU.mult)
                        kb.tt(ycs[:, :], ycs[:, :], tq[0:64, :], ALU.add)
                    yield
                kb.copy(ycb[:, :], ycs[:, :], eng='act')
                kb.dma('pool', YC[hp * 128 + hd * 64:hp * 128 + hd * 64 + 64, sl_], ycb[:, :])
            interleave([(hd, c) for hd in range(2) for c in range(8)], merge_gen, 2, 8)
    with Phase(kb) as ph:
        pc = load_w_bf16(net, ph, net.inp['p_c'], net.inp['p_c'].t[l], 4, 1024, 'pc')
        T = bo_scratch(net, ph, 512, npp=1)
        yc = ph.sb(net.n('yc'), [128, 4, 512], BF16)
        for tt in range(8):
            kb.dma('sp', yc[:, :, :], View(YC, YC.t[:, tt * 512:(tt + 1) * 512].rearrange("(m p) t -> p m t", p=128)))
            branch_out(net, T, pc, [yc[:, kc, :] for kc in range(4)], O_GATE + 2048, net.dr['GC'], tt * 512, 512)
C0 = float(np.exp(-0.5))
GN_EPS = 1e-5 * 64
TT = 256


def phase_rwkv(net, l, vec, C):
    kb = net.kb
    zT = net.dr['zT']; VF = net.dr['VF']
    with Phase(kb) as ph:
        pbw = load_w_bf16(net, ph, net.inp['p_b'], net.inp['p_b'].t[l], 8, 1024, 'pbw')
        wa2 = ph.sb(net.n('wa2'), [128, 1024], BF16)
        g2a = ph.sb(net.n('g2a'), [128, 1024], BF16)
        g2b = ph.sb(net.n('g2b'), [32, 1024], BF16)
        v2 = ph.sb(net.n('v2'), [32, 1024], BF16)
        with Phase(kb) as p2:
            st = p2.sb(net.n('lst'), [128, 1024], F32)
            kb.dma('sp', st[0:64, :], net.inp['rwkv_w2'][l])
            kb.dma('sp', st[64:128, :], net.inp['rwkv_a2'][l])
            kb.copy(wa2[:, :], st[:, :])
            st2 = p2.sb(net.n('lst2'), [128, 1024], F32)
            kb.dma('sp', st2[:, :], net.inp['rwkv_g2'][l, 0:128, :])
            kb.copy(g2a[:, :], st2[:, :])
            st3 = p2.sb(net.n('lst3'), [32, 1024], F32)
            kb.dma('sp', st3[:, :], net.inp['rwkv_g2'][l, 128:160, :])
            kb.copy(g2b[:, :], st3[:, :])
            if l == 1:
                st4 = p2.sb(net.n('lst4'), [32, 1024], F32)
                kb.dma('sp', st4[:, :], net.inp['rwkv_v2'][0])
                kb.copy(v2[:, :], st4[:, :])
        KR = 2
        W = TT + 1
        zin = {k_: ph.sb(net.n('z' + k_), [128, 8, W], BF16) for k_ in 'rkv'}
        zwa = ph.sb(net.n('zwa'), [128, W], BF16)
        zg1 = ph.sb(net.n('zg1'), [128, W], BF16)
        zg2 = ph.sb(net.n('zg2'), [32, W], BF16)
        uv1 = ph.sb(net.n('uv1'), [32, TT], BF16)
        ft = {k_: ph.sb(net.n('ft' + k_), [128, TT], F32) for k_ in ['d', 'wa', 'g1', 'g2']}
        bt16 = {k_: ph.sb(net.n('bt' + k_), [128, TT], BF16) for k_ in ['wa', 'g1']}
        bg2 = ph.sb(net.n('bg2'), [32, TT], BF16)
        yb = ph.sb(net.n('yb'), [128, 8, TT], BF16)
        STf = [ph.sb(net.n('STf'), [128, 128], F32) for _ in range(8)]
        STb = [ph.sb(net.n('STb'), [128, 128], BF16) for _ in range(8)]
        for j in range(8):
            kb.memset(STf[j][:, :], 0.0)
            kb.memset(STb[j][:, :], 0.0)

        def mkset():
            X = {}
            X['vf_t'] = ph.sb(net.n('vf_t'), [128, TT], BF16)
            X['f'] = {k_: ph.sb(net.n('f' + k_), [128, TT], F32) for k_ in
                      ['d', 'r', 'k', 'v', 'sg', 'a', 'g', 's', 'kk', 'kkn', 'k2', 'tmp', 'cs', 'e', 'ka', 'y', 'yc', 'bon']}
            X['b16'] = {k_: ph.sb(net.n('b' + k_), [128, TT], BF16) for k_ in ['sq', 'rk']}
            X['ARt'] = ph.sb(net.n('ARt'), [128, 4, 192], BF16)
            for k_ in ('Bt', 'Kt', 'Bb', 'Kb', 'Vb'):
                X[k_] = ph.sb(net.n(k_), [128, 4, 128], BF16)
            for k_ in ('ARt', 'Bt', 'Kt', 'Bb', 'Kb', 'Vb'):
                kb.memset(X[k_][:, :, :], 0.0)
            X['AB'] = ph.sb(net.n('AB'), [128, 4, 192], BF16)
            X['AK'] = ph.sb(net.n('AK'), [128, 4, 192], BF16)
            for k_ in ('Ui', 'Li', 'Gi'):
                X[k_] = [ph.sb(net.n(k_), [128, 4, 128], BF16) for _ in range(2)]
            for k_ in ('Vtm', 'Bbtm', 'Kbtm'):
                X[k_] = ph.sb(net.n(k_), [128, 4, 128], BF16)
            X['RHS'] = ph.sb(net.n('RHS'), [128, 128], BF16)
            X['SA'] = ph.sb(net.n('SA'), [128, 128], BF16)
            X['Wc'] = ph.sb(net.n('Wc'), [128, 4], F32)
            P = [ph.ps(net.n('P'), [128, 512], F32) for _ in range(4)]
            X['P'] = P
            X['pA'] = lambda c: View(P[c // 2], P[c // 2].t[:, (c % 2) * 256:(c % 2) * 256 + 192])
            X['pA2'] = lambda h2: View(P[h2], P[h2].t[:, :].rearrange("p (c t) -> p c t", t=256)[:, :, 0:192])
            X['pB'] = Sub(P[0], P[0].t[:, :].rearrange("p (c t) -> p c t", t=128))
            X['pC'] = Sub(P[1], P[1].t[:, :].rearrange("p (c t) -> p c t", t=128))
            X['pD'] = Sub(P[2], P[2].t[:, :].rearrange("p (c t) -> p c t", t=128))
            X['pS'] = Sub(P[3], P[3].t[:, 0:256])
            X['pTr'] = Sub(P[3], P[3].t[:, 256:512].bitcast(BF16).rearrange("p (c t) -> p c t", t=128))
            return X
        sets = [mkset() for _ in range(KR)]
        T = {'gt': ph.sb(net.n('gt'), [128, 8, TT], BF16), 'sg': ph.sb(net.n('sg'), [128, TT], F32),
             'go': ph.sb(net.n('go'), [128, 8, TT], BF16), 'pp': [sets[0]['P'][2]]}

        def rows(off, a, b):
            return View(zT, zT.t[off:off + 1024, a:b].rearrange("(m p) t -> p m t", p=128))

        def vc(col, j=0):
            return vec[:, col + j:col + j + 1]

        def shift(dst, src, mu, d_):
            P_ = src.ap.shape[0]
            dd = d_[0:P_, :]
            kb.tt(dd, View(src.buf, src.ap[:, 0:TT]), View(src.buf, src.ap[:, 1:W]), ALU.subtract, eng='pool')
            kb.stt(dst, dd, mu, View(src.buf, src.ap[:, 1:W]), ALU.mult, ALU.add)

        def bd_write(dst, c_lo, src_fn):
            for hd in range(2):
                rs_ = slice(hd * 64, hd * 64 + 64)
                src_fn(rs_, dst[rs_, :, c_lo + hd * 64:c_lo + hd * 64 + 64])

        def v3(b, rs_=slice(0, 128)):
            return b.v(b.t[rs_, :].rearrange("p (c t) -> p c t", t=64))

        def body(ti, j, X):
            t0 = ti * TT
            f = X['f']; b16 = X['b16']; vf_t = X['vf_t']
            ARt = X['ARt']; Bt = X['Bt']; Kt = X['Kt']; Bb = X['Bb']; Kb = X['Kb']; Vb = X['Vb']
            AB = X['AB']; AK = X['AK']; Ui = X['Ui']; Li = X['Li']; Gi = X['Gi']
            Vtm = X['Vtm']; Bbtm = X['Bbtm']; Kbtm = X['Kbtm']; RHS = X['RHS']; SA = X['SA']; Wc = X['Wc']
            pA = X['pA']; pA2 = X['pA2']; pB = X['pB']; pC = X['pC']; pD = X['pD']; pS = X['pS']; pTr = X['pTr']
            cs_ = slice(j * 128, (j + 1) * 128)
            shift(f['r'][:, :], zin['r'][:, j, :], vc(V_MUR, j), f['d'])
            shift(f['k'][:, :], zin['k'][:, j, :], vc(V_MUK, j), f['d'])
            shift(f['v'][:, :], zin['v'][:, j, :], vc(V_MUV, j), f['d'])
            yield
            kb.mm(pS[:, :], wa2[0:64, cs_], bt16['wa'][0:64, :])
            kb.act(f['sg'][:, :], pS[:, :], AF.Sigmoid, bias=vc(V_W0, j))
            kb.mm(pS[:, :], wa2[64:128, cs_], bt16['wa'][64:128, :])
            kb.act(f['a'][:, :], pS[:, :], AF.Sigmoid, bias=vc(V_A0, j))
            yield
            kb.mm(pS[:, :], g2a[:, cs_], bt16['g1'][:, :], start=True, stop=False)
            kb.mm(pS[:, :], g2b[:, cs_], bg2[:, :], start=False, stop=True)
            kb.copy(f['g'][:, :], pS[:, :], eng='act')
            if l == 1:
                kb.mm(pS[:, :], v2[:, cs_], uv1[:, :])
                kb.act(f['s'][:, :], pS[:, :], AF.Sigmoid, bias=vc(V_V0, j))
                kb.dma('sp', vf_t[:, :], VF[cs_, t0:t0 + TT])
                kb.tt(f['tmp'][:, :], vf_t[:, :], f['v'][:, :], ALU.subtract)
                kb.tt(f['tmp'][:, :], f['tmp'][:, :], f['s'][:, :], ALU.mult)
                kb.tt(f['v'][:, :], f['v'][:, :], f['tmp'][:, :], ALU.add)
            else:
                kb.copy(vf_t[:, :], f['v'][:, :], eng='act')
                kb.dma('pool', VF[cs_, t0:t0 + TT], vf_t[:, :])
            yield
            kb.ts(f['kk'][:, :], f['k'][:, :], vc(V_KK, j), ALU.mult)
            kb.tt(b16['sq'][:, :], f['kk'][:, :], f['kk'][:, :], ALU.mult)
            kb.mm(pS[:, :], C['bones_b'][:, :], b16['sq'][:, :])
            kb.ts(f['tmp'][:, :], pS[:, :], 1e-24, ALU.max)
            kb.act(f['tmp'][:, :], f['tmp'][:, :], AF.Sqrt)
            kb.op('dve', lambda E, o, i: E.reciprocal(out=o, in_=i), [f['tmp'][:, :]], [f['tmp'][:, :]])
            kb.tt(f['kkn'][:, :], f['kk'][:, :], f['tmp'][:, :], ALU.mult)
            yield
            kb.ts(f['tmp'][:, :], f['a'][:, :], vc(V_KA, j), ALU.mult, vc(V_OMKA, j), ALU.add)
            kb.tt(f['k2'][:, :], f['k'][:, :], f['tmp'][:, :], ALU.mult)
            kb.tt(f['ka'][:, :], f['kkn'][:, :], f['a'][:, :], ALU.mult)
            kb.scan(f['cs'][:, :], C['cmask'][:, :], f['sg'][:, :], 0.0, ALU.mult, ALU.add)
            cs3 = v3(f['cs'])
            yield
            kb.tt(f['tmp'][:, :], f['cs'][:, :], f['sg'][:, :], ALU.subtract)
            kb.act(f['e'][:, :], f['tmp'][:, :], AF.Exp, scale=-C0)
            kb.tt(f['tmp'][:, :], f['kkn'][:, :], f['e'][:, :], ALU.mult)
            bd_write(ARt, 0, lambda rs_, o: kb.ts(o, v3(f['tmp'], rs_), -1.0, ALU.mult))
            kb.act(f['e'][:, :], f['cs'][:, :], AF.Exp, scale=-C0)
            kb.tt(ARt.v(ARt.t[:, :, 128:192]), v3(f['r']), v3(f['e']), ALU.mult)
            yield
            kb.act(f['e'][:, :], f['cs'][:, :], AF.Exp, scale=C0)
            bd_write(Bt, 0, lambda rs_, o: kb.tt(o, v3(f['ka'], rs_), v3(f['e'], rs_), ALU.mult))
            bd_write(Kt, 0, lambda rs_, o: kb.tt(o, v3(f['k2'], rs_), v3(f['e'], rs_), ALU.mult, eng='pool'))
            yield
            kb.tt(v3(f['tmp']), f['cs'].v(cs3.ap[:, :, 63:64].to_broadcast([128, 4, 64])), cs3, ALU.subtract)
            kb.act(f['e'][:, :], f['tmp'][:, :], AF.Exp, scale=-C0)
            kb.act(Wc[:, :], f['cs'].v(cs3.ap[:, :, 63]), AF.Exp, scale=-C0)
            bd_write(Bb, 0, lambda rs_, o: kb.tt(o, v3(f['ka'], rs_), v3(f['e'], rs_), ALU.mult))
            bd_write(Kb, 0, lambda rs_, o: kb.tt(o, v3(f['k2'], rs_), v3(f['e'], rs_), ALU.mult, eng='pool'))
            bd_write(Vb, 0, lambda rs_, o: kb.copy(o, v3(f['v'], rs_), eng='act'))
            yield
            mab2 = C['m_ab'].v(C['m_ab'].t[:, :].unsqueeze(1).to_broadcast([128, 2, 192]))
            for c in range(4):
                kb.mm(pA(c), Bt[:, c, :], ARt[:, c, :])
            for h2 in range(2):
                kb.tt(AB[:, 2 * h2:2 * h2 + 2, :], pA2(h2), mab2, ALU.mult)
            yield
            for c in range(4):
                kb.mm(pA(c), Kt[:, c, :], ARt[:, c, :])
            for h2 in range(2):
                kb.tt(AK[:, 2 * h2:2 * h2 + 2, :], pA2(h2), mab2, ALU.mult)
            yield
            for c in range(4):
                kb.mm(pB[:, c, :], ARt[:, c, 0:128], Bt[:, c, :])
            kb.tt(Li[0][:, :, :], pB[:, :, :], C['m_l'].v(C['m_l'].t[:, :].unsqueeze(1).to_broadcast([128, 4, 128])), ALU.mult)
            kb.copy(Ui[0][:, :, :], AB[:, :, 0:128], eng='pool')
            kb.tt(Gi[0][:, :, :], AB[:, :, 0:128], C['ident_b'].v(C['ident_b'].t[:, :].unsqueeze(1).to_broadcast([128, 4, 128])), ALU.add, eng='pool')
            yield
            cu, cl_, cg = 0, 0, 0
            for lev in range(5):
                Uo, Lo, Go = Ui[cu], Li[cl_], Gi[cg]
                Un, Ln, Gn = Ui[1 - cu], Li[1 - cl_], Gi[1 - cg]
                for c in range(4):
                    kb.mm(pB[:, c, :], Uo[:, c, :], Lo[:, c, :])
                if lev < 4:
                    for c in range(4):
                        kb.mm(pC[:, c, :], Lo[:, c, :], Uo[:, c, :])
                kb.copy(Ln[:, :, :], pB[:, :, :], eng='act')
                if lev < 4:
                    kb.copy(Un[:, :, :], pC[:, :, :], eng='dve')
                yield
                for c in range(4):
                    kb.mm(pD[:, c, :], Ln[:, c, :], Go[:, c, :])
                kb.tt(Gn[:, :, :], pD[:, :, :], Go[:, :, :], ALU.add)
                cu, cl_, cg = 1 - cu, 1 - cl_, 1 - cg
                yield
            G = Gi[cg]
            for src, dst in ((Vb, Vtm), (Bb, Bbtm), (Kb, Kbtm)):
                for c in range(4):
                    kb.transpose(pTr[:, c, :], src[:, c, :], C['ident_b'][:, :])
                kb.copy(dst[:, :, :], pTr[:, :, :], eng='act')
            yield
            for c in range(4):
                kb.mm(pD[:, 0, :], ARt[:, c, 0:128], STb[j][:, :], start=True, stop=False)
                kb.mm(pD[:, 0, :], AK[:, c, 0:128], Vtm[:, c, :], start=False, stop=True)
                kb.copy(RHS[:, :], pD[:, 0, :], eng='act')
                yield
                kb.mm(pD[:, 1, :], G[:, c, :], RHS[:, :])
                kb.copy(SA[:, :], pD[:, 1, :], eng='act')
                yield
                kb.mm(pS[:, c * 64:(c + 1) * 64], STb[j][:, :], ARt[:, c, 128:192], start=True, stop=False)
                kb.mm(pS[:, c * 64:(c + 1) * 64], SA[:, :], AB[:, c, 128:192], start=False, stop=False)
                kb.mm(pS[:, c * 64:(c + 1) * 64], Vtm[:, c, :], AK[:, c, 128:192], start=False, stop=True)
                kb.mm(pD[:, 2, :], Bbtm[:, c, :], SA[:, :], start=True, stop=False)
                kb.mm(pD[:, 2, :], Kbtm[:, c, :], Vtm[:, c, :], start=False, stop=True)
                kb.stt(STf[j][:, :], STf[j][:, :], Wc[:, c:c + 1], pD[:, 2, :], ALU.mult, ALU.add)
                kb.copy(STb[j][:, :], STf[j][:, :], eng='act')
                yield
            kb.copy(f['y'][:, :], pS[:, :], eng='act')
            kb.mm(pS[:, :], C['bmean_f'][:, :], f['y'][:, :])
            kb.tt(f['yc'][:, :], f['y'][:, :], pS[:, :], ALU.subtract)
            kb.tt(f['tmp'][:, :], f['yc'][:, :], f['yc'][:, :], ALU.mult)
            yield
            kb.mm(pS[:, :], C['bmean_f'][:, :], f['tmp'][:, :])
            kb.ts(f['tmp'][:, :], pS[:, :], GN_EPS, ALU.add)
            kb.act(f['tmp'][:, :], f['tmp'][:, :], AF.Sqrt)
            kb.op('dve', lambda E, o, i: E.reciprocal(out=o, in_=i), [f['tmp'][:, :]], [f['tmp'][:, :]])
            kb.tt(f['yc'][:, :], f['yc'][:, :], f['tmp'][:, :], ALU.mult)
            kb.ts(f['yc'][:, :], f['yc'][:, :], vc(V_LG, j), ALU.mult, vc(V_LB, j), ALU.add)
            yield
            kb.tt(f['tmp'][:, :], f['r'][:, :], f['k2'][:, :], ALU.mult)
            kb.ts(b16['rk'][:, :], f['tmp'][:, :], vc(V_RK, j), ALU.mult)
            kb.mm(pS[:, :], C['bones_b'][:, :], b16['rk'][:, :])
            kb.tt(f['bon'][:, :], pS[:, :], f['v'][:, :], ALU.mult)
            kb.tt(f['yc'][:, :], f['yc'][:, :], f['bon'][:, :], ALU.add)
            kb.tt(yb[:, j, :], f['yc'][:, :], f['g'][:, :], ALU.mult)
        NST = 38

        for ti in range(S // TT):
            t0 = ti * TT
            for k_, off in (('r', O_RWKV), ('k', O_RWKV + 1024), ('v', O_RWKV + 2048)):
                if ti == 0:
                    kb.memset(zin[k_][:, :, 0:1], 0.0)
                    kb.dma('sp', zin[k_][:, :, 1:W], rows(off, 0, TT))
                else:
                    kb.dma('sp', zin[k_][:, :, :], rows(off, t0 - 1, t0 + TT))
            o2 = O_RWKV + 3072
            for tl, a_, n_ in ((zwa, o2, 128), (zg1, o2 + 128, 128), (zg2, o2 + 256, 32)):
                if ti == 0:
                    kb.memset(tl[0:n_, 0:1], 0.0)
                    kb.dma('sp', tl[0:n_, 1:W], zT[a_:a_ + n_, 0:TT])
                else:
                    kb.dma('sp', tl[0:n_, :], zT[a_:a_ + n_, t0 - 1:t0 + TT])
            shift(ft['wa'][:, :], zwa[:, :], vc(V_MUWA), ft['d'])
            shift(ft['g1'][:, :], zg1[:, :], vc(V_MUG), ft['d'])
            shift(ft['g2'][0:32, :], zg2[0:32, :], vec[0:32, V_MUG + 1:V_MUG + 2], ft['d'])
            kb.act(bt16['wa'][0:64, :], ft['wa'][0:64, :], AF.Tanh)
            kb.copy(bt16['wa'][64:128, :], ft['wa'][64:128, :])
            kb.act(bt16['g1'][:, :], ft['g1'][:, :], AF.Sigmoid)
            kb.act(bg2[:, :], ft['g2'][0:32, :], AF.Sigmoid)
            if l == 1:
                kb.dma('sp', uv1[:, :], zT[O_UV1:O_UV1 + 32, t0:t0 + TT])
            interleave(range(8), lambda j, slot, ti=ti: body(ti, j, sets[slot]), KR, NST)
            branch_out(net, T, pbw, [yb[:, kc, :] for kc in range(8)], O_GATE + 1024, net.dr['GB'], t0, TT)
def host_consts():
    cf = {}
    cf['ident_f'] = np.eye(128, dtype=np.float32)
    p = np.arange(128)
    inv_freq = 500000.0 ** (-np.arange(0, 16, 2, dtype=np.float32) / 16)
    invf = np.where((p % 64) < 16, inv_freq[(p % 64) % 8] / (2 * np.pi), 0.0).astype(np.float32)
    cf['invf'] = invf[:, None]
    qi = np.arange(128)[:, None]; kj = np.arange(256)[None, :]
    cf['amask'] = np.where((kj >= qi) & (kj <= qi + 128), 0.0, -1e30).astype(np.float32)
    blk = (p[:, None] // 64) == (p[None, :] // 64)
    cf['bmean_f'] = (blk / 64.0).astype(np.float32)
    cm = np.ones((128, 256), np.float32); cm[:, ::64] = 0.0
    cf['cmask'] = cm
    s_ = (p % 64)[:, None]
    mab = np.zeros((128, 192), np.float32)
    mab[:, :128] = blk & (s_ < (p % 64)[None, :])
    mab[:, 128:] = (s_ <= np.arange(64)[None, :])
    cf['m_ab'] = mab
    cf['m_l'] = (blk & (s_ > (p % 64)[None, :])).astype(np.float32)
    cb = {}
    cb['ident_b'] = np.eye(128, dtype=np.float32)
    PT = np.zeros((128, 128), np.float32)
    for h in range(2):
        for i in range(8):
            PT[h * 64 + i + 8, h * 64 + i] = -1.0
            PT[h * 64 + i, h * 64 + i + 8] = 1.0
    cb['PT'] = PT
    cb['ones_b'] = np.ones((128, 128), np.float32)
    cb['bones_b'] = blk.astype(np.float32)
    return cf, cb


CF_KEYS = ['ident_f', 'invf', 'amask', 'bmean_f', 'cmask', 'm_ab', 'm_l']
CB_KEYS = ['ident_b', 'PT', 'ones_b', 'bones_b']


def host_vecs(I):
    out = np.zeros((L, 128, NV), np.float32)

    def pc(v):
        return np.ascontiguousarray(v.reshape(-1, 128).T)
    for l in range(L):
        o = out[l]
        for j in range(3):
            o[:, V_CONV + j * 8:V_CONV + j * 8 + 8] = pc(I['conv_w'][l, j])
        mu = I['rwkv_mu'][l]
        o[:, V_MUR:V_MUR + 8] = pc(mu[0:1024]); o[:, V_MUK:V_MUK + 8] = pc(mu[1024:2048]); o[:, V_MUV:V_MUV + 8] = pc(mu[2048:3072])
        o[:, V_MUWA] = mu[3072:3200]
        o[:, V_MUG] = mu[3200:3328]
        o[0:32, V_MUG + 1] = mu[3328:3360]
        o[:, V_W0:V_W0 + 8] = pc(I['rwkv_w0'][l]); o[:, V_A0:V_A0 + 8] = pc(I['rwkv_a0'][l])
        if l >= 1:
            o[:, V_V0:V_V0 + 8] = pc(I['rwkv_v0'][l - 1])
        o[:, V_KK:V_KK + 8] = pc(I['rwkv_k_k'][l]); o[:, V_KA:V_KA + 8] = pc(I['rwkv_k_a'][l])
        o[:, V_RK:V_RK + 8] = pc(I['rwkv_r_k'][l].reshape(-1))
        o[:, V_LG:V_LG + 8] = pc(I['rwkv_lnx_g'][l]); o[:, V_LB:V_LB + 8] = pc(I['rwkv_lnx_b'][l])
    return out


IN_SHAPES = {'x': [S, D], 'positions': [S], 'ln_in_g': [D], 'ln_in_b': [D], 'w_in': [L, D, NIN], 'rwkv_w2': [L, 64, D], 'rwkv_a2': [L, 64, D],
             'rwkv_g2': [L, 160, D], 'rwkv_v1': [1, D, 32], 'rwkv_v2': [1, 32, D], 'p_a': [L, D, D], 'p_b': [L, D, D], 'p_c': [L, 512, D],
             'w_o': [L, D, D], 'ln1_g': [L, D], 'ln1_b': [L, D], 'w_router': [128, 8, 128], 'router_bias': [128, 16], 'w_gate': [L, 16, D, 512],
             'w_up': [L, 16, D, 512], 'w_down': [L, 16, 512, D], 'ln2_g': [L, D], 'ln2_b': [L, D], 'vecs': [L, 128, NV]}


def build(debug=(), stop_after=None, skip=()):
    net = Net(debug=debug)
    kb = net.kb
    for k_, sh in IN_SHAPES.items():
        net.din(k_, sh, I32 if k_ == 'positions' else F32)
    cfh, cbh = host_consts()
    for k_ in CF_KEYS:
        net.din('c_' + k_, list(cfh[k_].shape), F32)
    for k_ in CB_KEYS:
        net.din('c_' + k_, list(cbh[k_].shape), BF16)
    out = net.nc.dram_tensor('out', [S, D], F32, kind="ExternalOutput")
    out = Buf(kb, out.ap(), dram=True)
    net.dscr('hA', [S, D], F32); net.dscr('hB', [S, D], F32); net.dscr('zT', [NZ, S], BF16)
    for k_ in ('GA', 'GB', 'GC', 'VF'):
        net.dscr(k_, [D, S], BF16)
    net.dscr('YC', [512, S], BF16)
    with Phase(kb) as g:
        C = {}
        for k_ in CF_KEYS:
            C[k_] = g.sb(net.n('k' + k_), list(cfh[k_].shape), F32)
            kb.dma('sp', C[k_][:, :], net.inp['c_' + k_][:, :])
        for k_ in CB_KEYS:
            C[k_] = g.sb(net.n('k' + k_), list(cbh[k_].shape), BF16)
            kb.dma('sp', C[k_][:, :], net.inp['c_' + k_][:, :])
        vec = []
        for l in range(L):
            v_ = g.sb(net.n('vec'), [128, NV], F32)
            kb.dma('sp', v_[:, :], net.inp['vecs'][l])
            kb.ts(v_[:, V_OMKA:V_OMKA + 8], v_[:, V_KA:V_KA + 8], -1.0, ALU.mult, 1.0, ALU.add)
            vec.append(v_)
        CW = g.sb(net.n('CW'), [128, 32, 16], F32)

        def mk_hT(ph):
            return [ph.sb(net.n('hT'), [128, 8, 1024], BF16) for _ in range(4)]
        with Phase(kb) as A:
            hT = mk_hT(A)
            phase0(net, hT, C)
            phase1(net, 0, hT)
        for l in range(L):
            if stop_after == ('p1', l):
                return net
            phase_conv(net, l, vec[l])
            if stop_after == ('conv', l):
                return net
            if 'rwkv' not in skip:
                phase_rwkv(net, l, vec[l], C)
            if stop_after == ('rwkv', l):
                return net
            if 'attn' not in skip:
                phase_attn(net, l, C)
            if stop_after == ('attn', l):
                return net
            with Phase(kb) as B:
                hT = mk_hT(B)
                phase_mix(net, l, vec[l], hT, C, net.dr['hA'], net.dr['hB'], CW)
                if stop_after == ('mix', l):
                    return net
                phase_moe(net, l, hT, C, CW, net.dr['hB'], net.dr['hA'], l == L - 1, out)
                if stop_after == ('moe', l):
                    return net
                if l < L - 1:
                    phase1(net, l + 1, hT)
    return net


def make_inputs(I, b):
    cfh, cbh = host_consts()
    m = {}
    for k_ in IN_SHAPES:
        if k_ == 'vecs':
            continue
        a = I[k_]
        if k_ in ('x', 'positions'):
            a = a[b]
        if k_ == 'w_router':
            a = np.concatenate([a.reshape(8, 128, 16).transpose(1, 0, 2), np.zeros((128, 8, 112), np.float32)], axis=2)
        if k_ == 'router_bias':
            a = np.broadcast_to(a[None, :], (128, 16))
        m[k_] = np.ascontiguousarray(a)
    m['vecs'] = host_vecs(I)
    for k_ in CF_KEYS:
        m['c_' + k_] = cfh[k_]
    for k_ in CB_KEYS:
        m['c_' + k_] = cbh[k_].astype(ml_dtypes.bfloat16)
    return m


def kernel(**inputs):
    I = {k_: np.asarray(v_) for k_, v_ in inputs.items()}
    net = build()
    in_maps = [make_inputs(I, b) for b in range(8)]
    res = run_bass_kernel_spmd(net.nc, in_maps, core_ids=list(range(8)))
    return np.stack([np.asarray(r["out"], dtype=np.float32) for r in res.results], axis=0)
```
